# Optimizing a Trainium2 kernel written in Bass

```python
import math
import jax, jax.numpy as jnp
from jax import lax
import numpy as np

D_MODEL = 2048
BATCH = 8
SEQ = 2048
DEPTH = 4

HEAD_DIM = 64
ATTN_WIDTH = D_MODEL // 2
N_Q_HEADS = ATTN_WIDTH // HEAD_DIM
GQA_RATIO = 8
N_KV_HEADS = N_Q_HEADS // GQA_RATIO
KV_DIM = N_KV_HEADS * HEAD_DIM
WINDOW = 128
ROPE_THETA = 10000.0
LRU_WIDTH = D_MODEL - ATTN_WIDTH
LRU_BLOCKS = 16
LRU_BLOCK = LRU_WIDTH // LRU_BLOCKS
CONV_WIDTH = 4
LRU_C = 8.0
D_IN = ATTN_WIDTH + 2 * KV_DIM + 2 * LRU_WIDTH
N_EXPERTS = 32
TOP_K = 4
D_FF = D_MODEL // 2
SWIGLU_LIMIT = 7.0
SWIGLU_ALPHA = 1.702
EXPERT_BLOCK = 256
DEEPNORM_ALPHA = (2 * DEPTH) ** 0.25
DEEPNORM_BETA = (8 * DEPTH) ** -0.25
LN_EPS = 1e-5
RMS_EPS = 1e-6

kernel_name = "hymba_swa_rglru_moe_deepnorm"


def layer_norm(x, g, b):
    xf = x.astype(jnp.float32)
    mu = jnp.mean(xf, axis=-1, keepdims=True)
    var = jnp.mean(jnp.square(xf - mu), axis=-1, keepdims=True)
    y = (xf - mu) * lax.rsqrt(var + LN_EPS)
    return (y * g.astype(jnp.float32) + b.astype(jnp.float32)).astype(x.dtype)


def rms_norm(x, g):
    xf = x.astype(jnp.float32)
    y = xf * lax.rsqrt(jnp.mean(jnp.square(xf), axis=-1, keepdims=True) + RMS_EPS)
    return (y * g.astype(jnp.float32)).astype(x.dtype)


def rope_tables(seq):
    inv_freq = 1.0 / (ROPE_THETA ** (jnp.arange(0, HEAD_DIM, 2, dtype=jnp.float32) / HEAD_DIM))
    ang = jnp.arange(seq, dtype=jnp.float32)[:, None] * inv_freq[None, :]
    return jnp.cos(ang), jnp.sin(ang)


def apply_rope(t, cos, sin):
    tf = t.astype(jnp.float32)
    t1, t2 = jnp.split(tf, 2, axis=-1)
    c = cos[None, :, None, :]
    s = sin[None, :, None, :]
    return jnp.concatenate([t1 * c - t2 * s, t2 * c + t1 * s], axis=-1).astype(t.dtype)


def sliding_window_attention(q, k, v, sinks):
    B, S, _, Dh = q.shape
    nb = S // WINDOW
    qb = q.reshape(B, nb, WINDOW, N_KV_HEADS, GQA_RATIO, Dh)
    pad = ((0, 0), (WINDOW, 0), (0, 0), (0, 0))
    kp = jnp.pad(k, pad).reshape(B, nb + 1, WINDOW, N_KV_HEADS, Dh)
    vp = jnp.pad(v, pad).reshape(B, nb + 1, WINDOW, N_KV_HEADS, Dh)
    k_band = jnp.concatenate([kp[:, :-1], kp[:, 1:]], axis=2)
    v_band = jnp.concatenate([vp[:, :-1], vp[:, 1:]], axis=2)
    scores = jnp.einsum('bnqhgd,bnkhd->bnhgqk', qb, k_band).astype(jnp.float32) * (Dh ** -0.5)
    i = jnp.arange(WINDOW)[:, None]
    j = jnp.arange(2 * WINDOW)[None, :]
    blk = jnp.arange(nb)[:, None, None]
    valid = (j > i) & (j <= i + WINDOW) & ((blk > 0) | (j >= WINDOW))
    scores = jnp.where(valid[None, :, None, None], scores, -jnp.inf)
    sink = sinks.astype(jnp.float32).reshape(N_KV_HEADS, GQA_RATIO)[None, None, :, :, None, None]
    sink = jnp.broadcast_to(sink, scores.shape[:-1] + (1,))
    probs = jax.nn.softmax(jnp.concatenate([scores, sink], axis=-1), axis=-1)[..., :-1]
    out = jnp.einsum('bnhgqk,bnkhd->bnqhgd', probs.astype(v.dtype), v_band)
    return out.reshape(B, S, N_Q_HEADS * Dh)


def causal_depthwise_conv(u, w, b):
    C = u.shape[-1]
    out = lax.conv_general_dilated(
        u, w[:, None, :].astype(u.dtype), window_strides=(1,),
        padding=[(CONV_WIDTH - 1, 0)], dimension_numbers=('NWC', 'WIO', 'NWC'),
        feature_group_count=C)
    return out + b.astype(u.dtype)


def _linear_recurrence_combine(left, right):
    a1, b1 = left
    a2, b2 = right
    return a1 * a2, a2 * b1 + b2


def rg_lru(u, w_a, b_a, w_x, b_x, lam):
    B, S, C = u.shape
    uh = u.reshape(B, S, LRU_BLOCKS, LRU_BLOCK)
    r = jax.nn.sigmoid((jnp.einsum('bshi,hij->bshj', uh, w_a).reshape(B, S, C) + b_a).astype(jnp.float32))
    ig = jax.nn.sigmoid((jnp.einsum('bshi,hij->bshj', uh, w_x).reshape(B, S, C) + b_x).astype(jnp.float32))
    log_a = -LRU_C * r * jax.nn.softplus(-lam.astype(jnp.float32))
    a = jnp.exp(log_a)
    mult = jnp.sqrt(1.0 - jnp.exp(2.0 * log_a))
    xin = mult * (ig * u.astype(jnp.float32))
    _, h = lax.associative_scan(_linear_recurrence_combine, (a, xin), axis=1)
    return h.astype(u.dtype)


def hybrid_mixer(x, cos, sin, w_in, b_in, attn_sinks, conv_w, conv_b,
                 lru_w_a, lru_b_a, lru_w_x, lru_b_x, lru_lambda, g_attn, g_lru, w_out, b_out):
    B, S, _ = x.shape
    proj = x @ w_in + b_in
    q, k, v, ux, ug = jnp.split(
        proj, [ATTN_WIDTH, ATTN_WIDTH + KV_DIM, ATTN_WIDTH + 2 * KV_DIM,
               ATTN_WIDTH + 2 * KV_DIM + LRU_WIDTH], axis=-1)
    q = apply_rope(q.reshape(B, S, N_Q_HEADS, HEAD_DIM), cos, sin)
    k = apply_rope(k.reshape(B, S, N_KV_HEADS, HEAD_DIM), cos, sin)
    v = v.reshape(B, S, N_KV_HEADS, HEAD_DIM)
    attn = sliding_window_attention(q, k, v, attn_sinks)
    u = causal_depthwise_conv(ux, conv_w, conv_b)
    lru = rg_lru(u, lru_w_a, lru_b_a, lru_w_x, lru_b_x, lru_lambda) * jax.nn.gelu(ug)
    merged = jnp.concatenate([rms_norm(attn, g_attn), rms_norm(lru, g_lru)], axis=-1)
    return merged @ w_out + b_out


def clamped_swiglu(h):
    glu, lin = jnp.split(h, 2, axis=-1)
    glu = jnp.minimum(glu, SWIGLU_LIMIT)
    lin = jnp.clip(lin, -SWIGLU_LIMIT, SWIGLU_LIMIT)
    return glu * jax.nn.sigmoid(SWIGLU_ALPHA * glu) * (lin + 1.0)


def moe_ffn(x, w_router, b_router, w_up, b_up, w_down, b_down):
    B, S, D = x.shape
    T = B * S
    xf = x.reshape(T, D)
    logits = (xf @ w_router + b_router).astype(jnp.float32)
    top_val, top_idx = lax.top_k(logits, TOP_K)
    gates = jax.nn.softmax(top_val, axis=-1)
    n_assign = T * TOP_K
    flat_e = top_idx.reshape(-1)
    flat_tok = jnp.repeat(jnp.arange(T, dtype=jnp.int32), TOP_K)
    flat_g = gates.reshape(-1)
    order = jnp.argsort(flat_e)
    se, stok, sg = flat_e[order], flat_tok[order], flat_g[order]
    counts = jnp.bincount(flat_e, length=N_EXPERTS)
    start = jnp.cumsum(counts) - counts
    padded = (counts + EXPERT_BLOCK - 1) // EXPERT_BLOCK * EXPERT_BLOCK
    pad_end = jnp.cumsum(padded)
    pad_start = pad_end - padded
    dest = pad_start[se] + (jnp.arange(n_assign) - start[se])
    n_blocks = -(-n_assign // EXPERT_BLOCK) + N_EXPERTS
    n_slots = n_blocks * EXPERT_BLOCK
    slot_tok = jnp.full((n_slots,), T, dtype=jnp.int32).at[dest].set(stok)
    slot_gate = jnp.zeros((n_slots,), jnp.float32).at[dest].set(sg)
    block_e = jnp.minimum(
        jnp.searchsorted(pad_end, jnp.arange(n_blocks) * EXPERT_BLOCK, side='right'), N_EXPERTS - 1)
    xs = jnp.take(xf, slot_tok, axis=0, mode='clip').reshape(n_blocks, EXPERT_BLOCK, D)

    def expert_block(args):
        xb, e = args
        h = xb @ w_up[e] + b_up[e]
        return clamped_swiglu(h) @ w_down[e] + b_down[e]

    ys = lax.map(expert_block, (xs, block_e)).reshape(n_slots, D)
    ys = ys * slot_gate[:, None].astype(ys.dtype)
    out = jax.ops.segment_sum(ys, slot_tok, num_segments=T)
    return out.reshape(B, S, D)


def setup_inputs(seed: int = 0) -> dict:
    key = jax.random.key(seed)
    ks = jax.random.split(key, 25)
    L, D, E, F = DEPTH, D_MODEL, N_EXPERTS, D_FF
    nrm = lambda k, shape, s: jax.random.normal(k, shape, jnp.float32) * s
    col_scale = jnp.concatenate([
        jnp.ones((ATTN_WIDTH + KV_DIM,), jnp.float32),
        jnp.full((KV_DIM + LRU_WIDTH,), DEEPNORM_BETA, jnp.float32),
        jnp.ones((LRU_WIDTH,), jnp.float32)])
    a0 = jax.random.uniform(ks[11], (L, LRU_WIDTH), jnp.float32, 0.9, 0.999)
    return {
        "x": nrm(ks[0], (BATCH, SEQ, D), 1.0),
        "w_in": nrm(ks[1], (L, D, D_IN), D ** -0.5) * col_scale,
        "b_in": nrm(ks[2], (L, D_IN), 0.02),
        "attn_sinks": nrm(ks[3], (L, N_Q_HEADS), 0.5),
        "conv_w": nrm(ks[4], (L, CONV_WIDTH, LRU_WIDTH), CONV_WIDTH ** -0.5),
        "conv_b": nrm(ks[5], (L, LRU_WIDTH), 0.02),
        "lru_w_a": nrm(ks[6], (L, LRU_BLOCKS, LRU_BLOCK, LRU_BLOCK), LRU_BLOCK ** -0.5),
        "lru_b_a": nrm(ks[7], (L, LRU_WIDTH), 0.02),
        "lru_w_x": nrm(ks[8], (L, LRU_BLOCKS, LRU_BLOCK, LRU_BLOCK), LRU_BLOCK ** -0.5),
        "lru_b_x": nrm(ks[9], (L, LRU_WIDTH), 0.02),
        "lru_lambda": jnp.log(a0) - jnp.log1p(-a0),
        "g_attn": 1.0 + nrm(ks[10], (L, ATTN_WIDTH), 0.02),
        "g_lru": 1.0 + nrm(ks[12], (L, LRU_WIDTH), 0.02),
        "w_out": nrm(ks[13], (L, D, D), D ** -0.5 * DEEPNORM_BETA),
        "b_out": nrm(ks[14], (L, D), 0.02),
        "ln1_g": 1.0 + nrm(ks[15], (L, D), 0.02),
        "ln1_b": nrm(ks[16], (L, D), 0.02),
        "w_router": nrm(ks[17], (L, D, E), D ** -0.5),
        "b_router": nrm(ks[18], (L, E), 0.01),
        "w_up": nrm(ks[19], (L, E, D, 2 * F), D ** -0.5),
        "b_up": nrm(ks[20], (L, E, 2 * F), 0.02),
        "w_down": nrm(ks[21], (L, E, F, D), F ** -0.5 * DEEPNORM_BETA),
        "b_down": nrm(ks[22], (L, E, D), 0.02),
        "ln2_g": 1.0 + nrm(ks[23], (L, D), 0.02),
        "ln2_b": nrm(ks[24], (L, D), 0.02),
    }


def reference(x, w_in, b_in, attn_sinks, conv_w, conv_b, lru_w_a, lru_b_a, lru_w_x, lru_b_x,
              lru_lambda, g_attn, g_lru, w_out, b_out, ln1_g, ln1_b, w_router, b_router,
              w_up, b_up, w_down, b_down, ln2_g, ln2_b):
    cos, sin = rope_tables(x.shape[1])
    for l in range(DEPTH):
        m = hybrid_mixer(x, cos, sin, w_in[l], b_in[l], attn_sinks[l], conv_w[l], conv_b[l],
                         lru_w_a[l], lru_b_a[l], lru_w_x[l], lru_b_x[l], lru_lambda[l],
                         g_attn[l], g_lru[l], w_out[l], b_out[l])
        x = layer_norm(DEEPNORM_ALPHA * x + m, ln1_g[l], ln1_b[l])
        f = moe_ffn(x, w_router[l], b_router[l], w_up[l], b_up[l], w_down[l], b_down[l])
        x = layer_norm(DEEPNORM_ALPHA * x + f, ln2_g[l], ln2_b[l])
    return x
```

```python
import contextlib
import numpy as np
import concourse.bass as bass
import concourse.mybir as mybir
from concourse.bass_utils import run_bass_kernel_spmd

F32 = mybir.dt.float32
BF16 = mybir.dt.bfloat16
I32 = mybir.dt.int32
U32 = mybir.dt.uint32
AF = mybir.ActivationFunctionType
ALU = mybir.AluOpType
AX = mybir.AxisListType

D = 2048
S = 2048
NT = 16
KC = 16
DIN = 3328
NE = 32
CAP = 384
NSLOT = NE * CAP
DEPTH = 4
ALPHA = float((2 * DEPTH) ** 0.25)
LN_EPS = 1e-5
RMS_EPS = 1e-6
GELU_NATIVE = False

CA_BQ = 0
CA_BK = 8
CA_BUX = 9
CA_BUG = 17
CA_CW = 25
CA_CB = 57
CA_BA = 65
CA_BXG = 73
CA_LAM = 81
CA_GA = 89
CA_GL = 97
NA = 105
RB_BV = 0
RB_BR = 128
RB_SK = 160
NB = 176


class Tk:
    __slots__ = ("w", "r")

    def __init__(self):
        self.w = None
        self.r = {}


class Sched:
    NDS = 24

    def __init__(self, nc, es):
        self.nc = nc
        self.eng = {"pe": nc.tensor, "act": nc.scalar, "dve": nc.vector, "pool": nc.gpsimd, "sp": nc.sync}
        self.semobj = {}
        for k in self.eng:
            self.semobj[k] = es.enter_context(nc.semaphore("s_" + k))
        for i in range(self.NDS):
            self.semobj[("d", i)] = es.enter_context(nc.semaphore("d%d" % i))
        self.cnt = {k: 0 for k in self.eng}
        self.dcnt = [0] * self.NDS
        self.dnext = 0
        self.seen = {k: {} for k in self.eng}

    def _wait(self, e, key, val):
        if self.seen[e].get(key, 0) >= val:
            return
        self.seen[e][key] = val
        self.eng[e].wait_ge(self.semobj[key], val)

    def _deps(self, e, reads, writes, is_dma):
        for t in reads:
            if t.w is not None:
                self._wait(e, t.w[0], t.w[1])
        for t in writes:
            if t.w is not None and (is_dma or t.w[0] != e):
                self._wait(e, t.w[0], t.w[1])
            for key, val in t.r.items():
                if is_dma or key != e:
                    self._wait(e, key, val)

    def _mark(self, ev, reads, writes):
        for t in reads:
            if t.r.get(ev[0], 0) < ev[1]:
                t.r[ev[0]] = ev[1]
        for t in writes:
            t.w = ev
            t.r = {}

    def op(self, e, fn, reads=(), writes=()):
        self._deps(e, reads, writes, False)
        ins = fn()
        self.cnt[e] += 1
        ins.then_inc(self.semobj[e], 1)
        self._mark((e, self.cnt[e]), reads, writes)

    def dma(self, q, fn, reads=(), writes=()):
        i = self.dnext
        self.dnext = (i + 1) % self.NDS
        if self.dcnt[i] > 0:
            self._wait(q, ("d", i), 16 * self.dcnt[i])
        self._deps(q, reads, writes, True)
        ins = fn()
        self.dcnt[i] += 1
        ins.then_inc(self.semobj[("d", i)], 16)
        self._mark((("d", i), 16 * self.dcnt[i]), reads, writes)

    def barrier(self):
        evs = [(e, c) for e, c in self.cnt.items() if c > 0]
        evs += [(("d", i), 16 * c) for i, c in enumerate(self.dcnt) if c > 0]
        for e in self.eng:
            for key, val in evs:
                if key != e:
                    self._wait(e, key, val)


def build(nlayers, dbg=()):
    nc = bass.Bass("TRN2", target_bir_lowering=False)
    L = nlayers

    def din(name, shape, dt=F32):
        return nc.dram_tensor(name, list(shape), dt, kind="ExternalInput").ap()

    def dscr(name, shape, dt=F32):
        kind = "ExternalOutput" if name in dbg else "Internal"
        return nc.dram_tensor(name, list(shape), dt, kind=kind).ap()

    x_in = din("x", [S, D])
    w_in = din("w_in", [L, D, DIN])
    colsA_d = din("colsA", [L, 128, NA])
    rowsB_d = din("rowsB", [L, 128, NB])
    rowsBig_d = din("rowsBig", [L, 128, 5, D])
    wab_d = din("wab", [L, 128, 8, 128])
    wxb_d = din("wxb", [L, 128, 8, 128])
    w_out = din("w_out", [L, D, D])
    w_r = din("w_router", [L, D, NE])
    w_up = din("w_up", [L, NE, D, 2 * 1024])
    bupT_d = din("bupT", [L, 128, NE * 16])
    w_dn = din("w_down", [L, NE, 1024, D])
    b_dn = din("b_down", [L, NE, D])
    c_identf = din("c_identf", [128, 128])
    c_identb = din("c_identb", [128, 128], BF16)
    c_cos = din("c_cos", [128, S])
    c_sin = din("c_sin", [128, S])
    c_permR = din("c_permR", [128, 128])
    c_mprev = din("c_mprev", [128, 1024], BF16)
    c_mcur = din("c_mcur", [128, 1024], BF16)
    c_triu = din("c_triu", [128, 128], BF16)
    c_onesb = din("c_onesb", [128, 128], BF16)
    c_iota = din("c_iota", [128, 64])
    c_tokid = din("c_tokid", [128, NT], I32)
    c_zero = din("c_zero", [128, 96], I32)
    out_d = nc.dram_tensor("out", [S, D], F32, kind="ExternalOutput").ap()

    qT_d = dscr("qT_d", [9, 128, S], BF16)
    V_d = dscr("V_d", [128, NT * 2 * 65], BF16)
    mT_d = dscr("mT_d", [16, 128, S], BF16)
    xa_d = dscr("xa_d", [S, D])
    x1_d = dscr("x1_d", [S, D])
    x1b_d = dscr("x1b_d", [S, D], BF16)
    ys_d = dscr("ys_d", [NSLOT, D])
    stok_d = dscr("stok_d", [NSLOT, 1], I32)
    dbg_d = dscr("dbg_d", [128, 4096])

    es = contextlib.ExitStack()
    S_ = Sched(nc, es)

    uniq = [0]

    def sbt(st, name, shape, dt=F32):
        uniq[0] += 1
        return st.enter_context(nc.sbuf_tensor("sb%d_%s" % (uniq[0], name), list(shape), dt)), Tk()

    def pst(st, name, shape, dt=F32):
        uniq[0] += 1
        return st.enter_context(nc.psum_tensor("ps%d_%s" % (uniq[0], name), list(shape), dt)), Tk()

    PE = lambda fn, r, w: S_.op("pe", fn, r, w)
    ACT = lambda fn, r, w: S_.op("act", fn, r, w)
    DVE = lambda fn, r, w: S_.op("dve", fn, r, w)
    POOL = lambda fn, r, w: S_.op("pool", fn, r, w)
    dramT = {}

    def DT(ap_name):
        if ap_name not in dramT:
            dramT[ap_name] = Tk()
        return dramT[ap_name]

    def dma_sp(out, in_, r, w):
        S_.dma("sp", lambda: nc.sync.dma_start(out=out, in_=in_), r, w)

    def dma_pool(out, in_, r, w):
        S_.dma("pool", lambda: nc.gpsimd.dma_start(out=out, in_=in_), r, w)

    identf, t_identf = sbt(es, "identf", [128, 128])
    identb, t_identb = sbt(es, "identb", [128, 128], BF16)
    onesb, t_onesb = sbt(es, "onesb", [128, 128], BF16)
    triu, t_triu = sbt(es, "triu", [128, 128], BF16)
    iota, t_iota = sbt(es, "iota", [128, 64])
    tokid, t_tokid = sbt(es, "tokid", [128, NT], I32)
    onesf, t_onesf = sbt(es, "onesf", [128, 2])
    colsA, t_colsA = sbt(es, "colsA", [128, NA])
    rowsB, t_rowsB = sbt(es, "rowsB", [128, NB])
    c12, t_c12 = sbt(es, "c12", [128, 16])
    rstdl, t_rstdl = sbt(es, "rstdl", [128, NT])
    gates, t_gates = sbt(es, "gates", [128, NT, 4])
    sloti, t_sloti = sbt(es, "sloti", [128, NT, 4], I32)
    t_slotis = [Tk() for _ in range(NT)]
    GT, t_GT = sbt(es, "GT", [32, NT, 128])
    exps, t_exps = sbt(es, "exps", [128, 16])

    dma_sp(identf[:], c_identf, [], [t_identf])
    dma_sp(identb[:], c_identb, [], [t_identb])
    dma_sp(onesb[:], c_onesb, [], [t_onesb])
    dma_sp(triu[:], c_triu, [], [t_triu])
    dma_sp(iota[:], c_iota, [], [t_iota])
    dma_sp(tokid[:], c_tokid, [], [t_tokid])
    DVE(lambda: nc.vector.memset(onesf[:], 1.0), [], [t_onesf])
    with contextlib.ExitStack() as st0:
        zt, t_zt = sbt(st0, "zt", [128, 96], I32)
        dma_sp(zt[:], c_zero, [], [t_zt])
        dma_sp(stok_d.rearrange("(p j) o -> p (j o)", p=128), zt[:], [t_zt], [DT("stok")])
        S_.barrier()

    def ln_tile(st_small, res, t_res, grow, brow, t_rows, tagname):
        stats, t_stats = st_small["stats"]
        mv, t_mv = st_small["mv"]
        for j in range(4):
            DVE(lambda j=j: nc.vector.bn_stats(out=stats[:, j, :], in_=res[:, j * 512:(j + 1) * 512]),
                [t_res], [t_stats])
        DVE(lambda: nc.vector.bn_aggr(out=mv[:, 0:2], in_=stats[:]), [t_stats], [t_mv])
        ACT(lambda: nc.scalar.activation(out=mv[:, 2:3], in_=mv[:, 1:2], func=AF.Sqrt, bias=LN_EPS), [t_mv], [t_mv])
        DVE(lambda: nc.vector.reciprocal(out=mv[:, 2:3], in_=mv[:, 2:3]), [t_mv], [t_mv])
        DVE(lambda: nc.vector.tensor_scalar(out=res[:], in0=res[:], scalar1=mv[:, 0:1], scalar2=mv[:, 2:3],
                                            op0=ALU.subtract, op1=ALU.mult), [t_res, t_mv], [t_res])
        POOL(lambda: nc.gpsimd.tensor_tensor(out=res[:], in0=res[:], in1=grow, op=ALU.mult),
             [t_res, t_rows], [t_res])
        POOL(lambda: nc.gpsimd.tensor_tensor(out=res[:], in0=res[:], in1=brow, op=ALU.add),
             [t_res, t_rows], [t_res])

    for l in range(L):
        xsrc = x_in if l == 0 else xa_d
        t_xsrc = DT("x_in") if l == 0 else DT("xa")
        last = (l == L - 1)
        dma_sp(colsA[:], colsA_d[l], [], [t_colsA])
        dma_sp(rowsB[:], rowsB_d[l], [], [t_rowsB])
        ACT(lambda: nc.scalar.activation(out=c12[:, 0:8], in_=colsA[:, CA_LAM:CA_LAM + 8], func=AF.Exp, scale=-1.0),
            [t_colsA], [t_c12])
        ACT(lambda: nc.scalar.activation(out=c12[:, 0:8], in_=c12[:, 0:8], func=AF.Ln, bias=1.0),
            [t_c12], [t_c12])
        DVE(lambda: nc.vector.tensor_scalar(out=c12[:, 8:16], in0=c12[:, 0:8], scalar1=-16.0, scalar2=None,
                                            op0=ALU.mult), [t_c12], [t_c12])
        DVE(lambda: nc.vector.tensor_scalar(out=c12[:, 0:8], in0=c12[:, 0:8], scalar1=-8.0, scalar2=None,
                                            op0=ALU.mult), [t_c12], [t_c12])
        ACT(lambda: nc.scalar.activation(out=exps[:], in_=rowsB[:, RB_SK:RB_SK + 16], func=AF.Exp),
            [t_rowsB], [t_exps])

        with contextlib.ExitStack() as sa:
            xT, t_xT = sbt(sa, "xT", [128, KC, S], BF16)
            wring = [sbt(sa, "wr%d" % i, [128, KC, 512], BF16) for i in range(2)]
            wcnt = [0]
            with contextlib.ExitStack() as s1:
                xin = [sbt(s1, "xin%d" % i, [128, D]) for i in range(2)]
                tp = [pst(s1, "tp%d" % i, [128, 4, 128]) for i in range(2)]
                k = 0
                for tt in range(NT):
                    xi, t_xi = xin[tt % 2]
                    dma_sp(xi[:], xsrc[tt * 128:(tt + 1) * 128, :], [t_xsrc], [t_xi])
                    for g in range(4):
                        p_, t_p = tp[k % 2]
                        for j in range(4):
                            kc = g * 4 + j
                            PE(lambda kc=kc, j=j, p_=p_, xi=xi: nc.tensor.transpose(
                                out=p_[:, j, :], in_=xi[:, kc * 128:(kc + 1) * 128], identity=identf[:]),
                               [t_xi, t_identf], [t_p])
                        dst = xT[:, g * 4:(g + 1) * 4, tt * 128:(tt + 1) * 128]
                        if k % 2 == 0:
                            ACT(lambda p_=p_, dst=dst: nc.scalar.copy(out=dst, in_=p_[:]), [t_p], [t_xT])
                        else:
                            DVE(lambda p_=p_, dst=dst: nc.vector.tensor_copy(out=dst, in_=p_[:]), [t_p], [t_xT])
                        k += 1
                S_.barrier()

            def wload(colspecs):
                wt, t_wt = wring[wcnt[0] % 2]
                wcnt[0] += 1
                src = w_in[l].rearrange("(kc kp) n -> kp kc n", kp=128)
                for (d0, s0, n) in colspecs:
                    dma_pool(wt[:, :, d0:d0 + n], src[:, :, s0:s0 + n], [], [t_wt])
                return wt, t_wt

            with contextlib.ExitStack() as s1:
                cosT, t_cos = sbt(s1, "cosT", [128, S])
                sinT, t_sin = sbt(s1, "sinT", [128, S])
                permR, t_permR = sbt(s1, "permR", [128, 128])
                dma_sp(cosT[:], c_cos, [], [t_cos])
                dma_sp(sinT[:], c_sin, [], [t_sin])
                dma_sp(permR[:], c_permR, [], [t_permR])
                qf = [sbt(s1, "qf%d" % i, [128, 512]) for i in range(2)]
                t1 = [sbt(s1, "t1%d" % i, [128, 512]) for i in range(2)]
                qo = [sbt(s1, "qo%d" % i, [128, S], BF16) for i in range(2)]
                Vt, t_V = sbt(s1, "Vt", [128, NT * 2 * 65], BF16)
                pa = [pst(s1, "pa%d" % i, [128, 512]) for i in range(3)]
                pr = [pst(s1, "pr%d" % i, [128, 512]) for i in range(2)]
                pv = [pst(s1, "pv%d" % i, [128, 128]) for i in range(2)]
                DVE(lambda: nc.vector.memset(Vt[:], 1.0), [], [t_V])
                it = 0
                for g in range(3):
                    ncols = 512 if g < 2 else 256
                    wt, t_wt = wload([(0, g * 512, ncols)])
                    nch = 4 if g < 2 else 1
                    for j in range(nch):
                        ch = g * 4 + j
                        bcol = colsA[:, CA_BQ + ch:CA_BQ + ch + 1]
                        qo_, t_qo = qo[ch % 2]
                        for tb in range(4):
                            p_, t_p = pa[it % 3]
                            r_, t_r = pr[it % 2]
                            qf_, t_qf = qf[it % 2]
                            t1_, t_t1 = t1[it % 2]
                            it += 1
                            for kc in range(KC):
                                PE(lambda kc=kc, p_=p_, wt=wt, j=j, tb=tb: nc.tensor.matmul(
                                    p_[:], lhsT=wt[:, kc, j * 128:(j + 1) * 128],
                                    rhs=xT[:, kc, tb * 512:(tb + 1) * 512], start=(kc == 0), stop=(kc == KC - 1)),
                                   [t_wt, t_xT], [t_p])
                            ACT(lambda p_=p_, qf_=qf_, bcol=bcol: nc.scalar.activation(
                                out=qf_[:], in_=p_[:], func=AF.Identity, bias=bcol), [t_p, t_colsA], [t_qf])
                            PE(lambda r_=r_, qf_=qf_: nc.tensor.matmul(r_[:], lhsT=permR[:], rhs=qf_[:],
                                                                       start=True, stop=True),
                               [t_permR, t_qf], [t_r])
                            sl = slice(tb * 512, (tb + 1) * 512)
                            DVE(lambda t1_=t1_, qf_=qf_, sl=sl: nc.vector.tensor_tensor(
                                out=t1_[:], in0=qf_[:], in1=cosT[:, sl], op=ALU.mult), [t_qf, t_cos], [t_t1])
                            DVE(lambda qf_=qf_, r_=r_, sl=sl: nc.vector.tensor_tensor(
                                out=qf_[:], in0=r_[:], in1=sinT[:, sl], op=ALU.mult), [t_r, t_sin], [t_qf])
                            DVE(lambda qo_=qo_, t1_=t1_, qf_=qf_, sl=sl: nc.vector.tensor_tensor(
                                out=qo_[:, sl], in0=t1_[:], in1=qf_[:], op=ALU.add), [t_t1, t_qf], [t_qo])
                        dma_sp(qT_d[ch], qo_[:], [t_qo], [DT("qT")])
                    if g == 2:
                        for tt in range(NT):
                            p_, t_p = pv[tt % 2]
                            for kc in range(KC):
                                PE(lambda kc=kc, p_=p_, tt=tt, wt=wt: nc.tensor.matmul(
                                    p_[:], lhsT=xT[:, kc, tt * 128:(tt + 1) * 128], rhs=wt[:, kc, 128:256],
                                    start=(kc == 0), stop=(kc == KC - 1)), [t_wt, t_xT], [t_p])
                            for hk in range(2):
                                o0 = (tt * 2 + hk) * 65
                                DVE(lambda p_=p_, hk=hk, o0=o0: nc.vector.tensor_tensor(
                                    out=Vt[:, o0:o0 + 64], in0=p_[:, hk * 64:(hk + 1) * 64],
                                    in1=rowsB[:, RB_BV + hk * 64:RB_BV + (hk + 1) * 64], op=ALU.add),
                                    [t_p, t_rowsB], [t_V])
                        dma_sp(V_d, Vt[:], [t_V], [DT("V")])
                S_.barrier()

            with contextlib.ExitStack() as s2:
                wab, t_wab = sbt(s2, "wab", [128, 8, 128], BF16)
                wxb, t_wxb = sbt(s2, "wxb", [128, 8, 128], BF16)
                dma_pool(wab[:], wab_d[l], [], [t_wab])
                dma_pool(wxb[:], wxb_d[l], [], [t_wxb])
                uxp, t_uxp = sbt(s2, "uxp", [128, S + 3])
                u, t_u = sbt(s2, "u", [128, S])
                ub, t_ub = sbt(s2, "ub", [128, S], BF16)
                r_, t_r = sbt(s2, "r", [128, S])
                M_, t_M = sbt(s2, "M", [128, S])
                ig, t_ig = sbt(s2, "ig", [128, S])
                gel, t_gel = sbt(s2, "gel", [128, S])
                h_, t_h = sbt(s2, "hscan", [128, S])
                gtmp, t_gtmp = sbt(s2, "gtmp", [128, S])
                mtl, t_mtl = sbt(s2, "mtl", [128, S], BF16)
                sqacc, t_sq = sbt(s2, "sqacc", [128, S])
                pa = [pst(s2, "pb%d" % i, [128, 512]) for i in range(4)]
                pg = [pst(s2, "pg%d" % i, [128, 512]) for i in range(3)]
                pss, t_pss = pst(s2, "pss", [128, NT])
                DVE(lambda: nc.vector.memset(uxp[:, 0:3], 0.0), [], [t_uxp])
                ia = 0
                igc = 0
                wnext = wload([(0, 1280, 128), (128, 2304, 128)])
                for c in range(8):
                    wt, t_wt = wnext
                    if c + 1 < 8:
                        wnext = wload([(0, 1280 + (c + 1) * 128, 128), (128, 2304 + (c + 1) * 128, 128)])
                    for tb in range(4):
                        p_, t_p = pa[ia % 4]
                        ia += 1
                        for kc in range(KC):
                            PE(lambda kc=kc, p_=p_, tb=tb, wt=wt: nc.tensor.matmul(
                                p_[:], lhsT=wt[:, kc, 0:128], rhs=xT[:, kc, tb * 512:(tb + 1) * 512],
                                start=(kc == 0), stop=(kc == KC - 1)), [t_wt, t_xT], [t_p])
                        ACT(lambda p_=p_, tb=tb, c=c: nc.scalar.activation(
                            out=uxp[:, 3 + tb * 512:3 + (tb + 1) * 512], in_=p_[:], func=AF.Identity,
                            bias=colsA[:, CA_BUX + c:CA_BUX + c + 1]), [t_p, t_colsA], [t_uxp])
                    for tb in range(4):
                        p_, t_p = pa[ia % 4]
                        ia += 1
                        for kc in range(KC):
                            PE(lambda kc=kc, p_=p_, tb=tb, wt=wt: nc.tensor.matmul(
                                p_[:], lhsT=wt[:, kc, 128:256], rhs=xT[:, kc, tb * 512:(tb + 1) * 512],
                                start=(kc == 0), stop=(kc == KC - 1)), [t_wt, t_xT], [t_p])
                        ACT(lambda p_=p_, tb=tb, c=c: nc.scalar.activation(
                            out=gel[:, tb * 512:(tb + 1) * 512], in_=p_[:], func=AF.Identity,
                            bias=colsA[:, CA_BUG + c:CA_BUG + c + 1]), [t_p, t_colsA], [t_gel])
                    cw = lambda k_, c=c: colsA[:, CA_CW + k_ * 8 + c:CA_CW + k_ * 8 + c + 1]
                    DVE(lambda c=c, cw=cw: nc.vector.tensor_scalar(
                        out=u[:], in0=uxp[:, 3:S + 3], scalar1=cw(3), scalar2=colsA[:, CA_CB + c:CA_CB + c + 1],
                        op0=ALU.mult, op1=ALU.add), [t_uxp, t_colsA], [t_u])
                    for k_ in (2, 1, 0):
                        DVE(lambda k_=k_, cw=cw: nc.vector.scalar_tensor_tensor(
                            out=u[:], in0=uxp[:, k_:k_ + S], scalar=cw(k_), in1=u[:], op0=ALU.mult, op1=ALU.add),
                            [t_uxp, t_colsA, t_u], [t_u])
                    DVE(lambda: nc.vector.tensor_copy(out=ub[:], in_=u[:]), [t_u], [t_ub])
                    ACT(lambda: nc.scalar.activation(out=gtmp[:], in_=gel[:], func=AF.Square), [t_gel], [t_gtmp])
                    DVE(lambda: nc.vector.tensor_scalar(out=gtmp[:], in0=gtmp[:], scalar1=0.044715, scalar2=1.0,
                                                        op0=ALU.mult, op1=ALU.add), [t_gtmp], [t_gtmp])
                    DVE(lambda: nc.vector.tensor_tensor(out=gtmp[:], in0=gtmp[:], in1=gel[:], op=ALU.mult),
                        [t_gtmp, t_gel], [t_gtmp])
                    ACT(lambda: nc.scalar.activation(out=gtmp[:], in_=gtmp[:], func=AF.Sigmoid, scale=1.5957691216),
                        [t_gtmp], [t_gtmp])
                    DVE(lambda: nc.vector.tensor_tensor(out=gel[:], in0=gel[:], in1=gtmp[:], op=ALU.mult),
                        [t_gel, t_gtmp], [t_gel])
                    for (wb, t_wb, dst, t_dst, bc) in ((wab, t_wab, r_, t_r, CA_BA), (wxb, t_wxb, ig, t_ig, CA_BXG)):
                        for tb in range(4):
                            p_, t_p = pg[igc % 3]
                            igc += 1
                            PE(lambda p_=p_, wb=wb, tb=tb, c=c: nc.tensor.matmul(
                                p_[:], lhsT=wb[:, c, :], rhs=ub[:, tb * 512:(tb + 1) * 512], start=True, stop=True),
                               [t_wb, t_ub], [t_p])
                            ACT(lambda p_=p_, dst=dst, tb=tb, bc=bc, c=c: nc.scalar.activation(
                                out=dst[:, tb * 512:(tb + 1) * 512], in_=p_[:], func=AF.Sigmoid,
                                bias=colsA[:, bc + c:bc + c + 1]), [t_p, t_colsA], [t_dst])
                    ACT(lambda c=c: nc.scalar.activation(out=M_[:], in_=r_[:], func=AF.Exp,
                                                         scale=c12[:, 8 + c:9 + c]), [t_r, t_c12], [t_M])
                    ACT(lambda c=c: nc.scalar.activation(out=r_[:], in_=r_[:], func=AF.Exp,
                                                         scale=c12[:, c:c + 1]), [t_r, t_c12], [t_r])
                    ACT(lambda: nc.scalar.activation(out=M_[:], in_=M_[:], func=AF.Sqrt, scale=-1.0, bias=1.0),
                        [t_M], [t_M])
                    DVE(lambda: nc.vector.tensor_tensor(out=ig[:], in0=ig[:], in1=u[:], op=ALU.mult),
                        [t_ig, t_u], [t_ig])
                    DVE(lambda: nc.vector.tensor_tensor(out=ig[:], in0=ig[:], in1=M_[:], op=ALU.mult),
                        [t_ig, t_M], [t_ig])
                    DVE(lambda: nc.vector.tensor_tensor_scan(out=h_[:], data0=r_[:], data1=ig[:], initial=0.0,
                                                             op0=ALU.mult, op1=ALU.add), [t_r, t_ig], [t_h])
                    DVE(lambda: nc.vector.tensor_tensor(out=gel[:], in0=gel[:], in1=h_[:], op=ALU.mult),
                        [t_gel, t_h], [t_gel])
                    ACT(lambda c=c: nc.scalar.activation(out=mtl[:], in_=gel[:], func=AF.Identity,
                                                         scale=colsA[:, CA_GL + c:CA_GL + c + 1]),
                        [t_gel, t_colsA], [t_mtl])
                    dma_sp(mT_d[8 + c], mtl[:], [t_mtl], [DT("mT")])
                    if c == 0:
                        ACT(lambda: nc.scalar.activation(out=sqacc[:], in_=gel[:], func=AF.Square), [t_gel], [t_sq])
                    else:
                        ACT(lambda: nc.scalar.activation(out=gtmp[:], in_=gel[:], func=AF.Square),
                            [t_gel], [t_gtmp])
                        POOL(lambda: nc.gpsimd.tensor_tensor(out=sqacc[:], in0=sqacc[:], in1=gtmp[:], op=ALU.add),
                             [t_sq, t_gtmp], [t_sq])
                for tt in range(NT):
                    PE(lambda tt=tt: nc.tensor.matmul(pss[:, tt:tt + 1], lhsT=sqacc[:, tt * 128:(tt + 1) * 128],
                                                      rhs=onesf[:, 0:1], start=True, stop=True),
                       [t_sq, t_onesf], [t_pss])
                ACT(lambda: nc.scalar.activation(out=rstdl[:], in_=pss[:], func=AF.Sqrt, scale=1.0 / 1024,
                                                 bias=RMS_EPS), [t_pss], [t_rstdl])
                DVE(lambda: nc.vector.reciprocal(out=rstdl[:], in_=rstdl[:]), [t_rstdl], [t_rstdl])
                S_.barrier()
        S_.barrier()

        with contextlib.ExitStack() as sbk:
            qT, t_qT = sbt(sbk, "qT", [128, 9, S], BF16)
            Vt, t_V = sbt(sbk, "VtB", [128, NT * 2 * 65], BF16)
            mprev, t_mprev = sbt(sbk, "mprev", [128, 1024], BF16)
            mcur, t_mcur = sbt(sbk, "mcur", [128, 1024], BF16)
            mTa, t_mTa = sbt(sbk, "mTa", [128, 8, S], BF16)
            dma_sp(qT[:], qT_d.rearrange("c p t -> p c t"), [DT("qT")], [t_qT])
            dma_sp(Vt[:], V_d, [DT("V")], [t_V])
            dma_sp(mprev[:], c_mprev, [], [t_mprev])
            dma_sp(mcur[:], c_mcur, [], [t_mcur])
            E = [sbt(sbk, "E%d" % i, [128, 1024], BF16) for i in range(8)]
            at = [sbt(sbk, "at%d" % i, [128, 1024]) for i in range(2)]
            atb = [sbt(sbk, "atb%d" % i, [128, 1024], BF16) for i in range(2)]
            small = [sbt(sbk, "sm%d" % i, [128, 32]) for i in range(2)]
            psc = [pst(sbk, "psc%d" % i, [128, 512]) for i in range(4)]
            po = [pst(sbk, "po%d" % i, [128, 4, 65]) for i in range(2)]
            ptr, t_ptr = pst(sbk, "ptr", [128, 8, 128], BF16)
            cB = {"ie": 0, "isc": 0}
            EsAll = {}

            def stS(qb):
                kbs = ([qb - 1] if qb > 0 else []) + [qb]
                for hk in range(2):
                    Es = []
                    for kb in kbs:
                        E_, t_E = E[cB["ie"] % 8]
                        cB["ie"] += 1
                        for half in range(2):
                            p_, t_p = psc[cB["isc"] % 4]
                            cB["isc"] += 1
                            PE(lambda p_=p_, hk=hk, kb=kb, half=half, qb=qb: nc.tensor.matmul(
                                p_[:], lhsT=qT[hk * 64:(hk + 1) * 64, 8, kb * 128:(kb + 1) * 128],
                                rhs=qT[hk * 64:(hk + 1) * 64, half * 4:(half + 1) * 4, qb * 128:(qb + 1) * 128],
                                start=True, stop=True), [t_qT], [t_p])
                            ACT(lambda p_=p_, E_=E_, half=half: nc.scalar.activation(
                                out=E_[:, half * 512:(half + 1) * 512], in_=p_[:], func=AF.Exp, scale=0.125),
                                [t_p], [t_E])
                        msk, t_msk = (mcur, t_mcur) if kb == qb else (mprev, t_mprev)
                        POOL(lambda E_=E_, msk=msk: nc.gpsimd.tensor_tensor(out=E_[:], in0=E_[:], in1=msk[:],
                                                                            op=ALU.mult), [t_E, t_msk], [t_E])
                        Es.append((E_, t_E, kb))
                    EsAll[(qb, hk)] = Es

            def stP(qb):
                at_, t_at = at[qb % 2]
                atb_, t_atb = atb[qb % 2]
                sm, t_sm = small[qb % 2]
                for hk in range(2):
                    Es = EsAll.pop((qb, hk))
                    for hh in range(2):
                        o_, t_o = po[hh]
                        for g4 in range(4):
                            g = hh * 4 + g4
                            for i_, (E_, t_E, kb) in enumerate(Es):
                                v0 = (kb * 2 + hk) * 65
                                PE(lambda o_=o_, g4=g4, g=g, E_=E_, v0=v0, i_=i_, n=len(Es): nc.tensor.matmul(
                                    o_[:, g4, :], lhsT=E_[:, g * 128:(g + 1) * 128], rhs=Vt[:, v0:v0 + 65],
                                    start=(i_ == 0), stop=(i_ == n - 1)), [t_E, t_V], [t_o])
                        h0 = hk * 8 + hh * 4
                        DVE(lambda o_=o_, sm=sm, h0=h0: nc.vector.tensor_tensor(
                            out=sm[:, h0:h0 + 4], in0=o_[:, :, 64], in1=exps[:, h0:h0 + 4], op=ALU.add),
                            [t_o, t_exps], [t_sm])
                        DVE(lambda sm=sm, h0=h0: nc.vector.reciprocal(out=sm[:, h0:h0 + 4], in_=sm[:, h0:h0 + 4]),
                            [t_sm], [t_sm])
                        for g4 in range(4):
                            h = h0 + g4
                            DVE(lambda o_=o_, g4=g4, h=h, at_=at_, sm=sm: nc.vector.tensor_scalar(
                                out=at_[:, h * 64:(h + 1) * 64], in0=o_[:, g4, 0:64], scalar1=sm[:, h:h + 1],
                                scalar2=None, op0=ALU.mult), [t_o, t_sm], [t_at])
                ACT(lambda atb_=atb_, at_=at_, sm=sm: nc.scalar.activation(
                    out=atb_[:], in_=at_[:], func=AF.Square, accum_out=sm[:, 16:17]), [t_at], [t_atb, t_sm])
                ACT(lambda sm=sm: nc.scalar.activation(out=sm[:, 17:18], in_=sm[:, 16:17], func=AF.Sqrt,
                                                       scale=1.0 / 1024, bias=RMS_EPS), [t_sm], [t_sm])
                DVE(lambda sm=sm: nc.vector.reciprocal(out=sm[:, 17:18], in_=sm[:, 17:18]), [t_sm], [t_sm])
                DVE(lambda atb_=atb_, at_=at_, sm=sm: nc.vector.tensor_scalar(
                    out=atb_[:], in0=at_[:], scalar1=sm[:, 17:18], scalar2=None, op0=ALU.mult),
                    [t_at, t_sm, t_atb], [t_atb])

            def stT(qb):
                atb_, t_atb = atb[qb % 2]
                for c in range(8):
                    PE(lambda c=c, atb_=atb_: nc.tensor.transpose(out=ptr[:, c, :], in_=atb_[:, c * 128:(c + 1) * 128],
                                                                  identity=identb[:]), [t_atb, t_identb], [t_ptr])
                for c in range(8):
                    if c % 2 == 0:
                        ACT(lambda c=c, qb=qb: nc.scalar.activation(
                            out=mTa[:, c, qb * 128:(qb + 1) * 128], in_=ptr[:, c, :], func=AF.Identity,
                            scale=colsA[:, CA_GA + c:CA_GA + c + 1]), [t_ptr, t_colsA], [t_mTa])
                    else:
                        DVE(lambda c=c, qb=qb: nc.vector.tensor_scalar(
                            out=mTa[:, c, qb * 128:(qb + 1) * 128], in0=ptr[:, c, :],
                            scalar1=colsA[:, CA_GA + c:CA_GA + c + 1], scalar2=None, op0=ALU.mult),
                            [t_ptr, t_colsA], [t_mTa])

            stS(0)
            for qb in range(NT):
                if qb + 1 < NT:
                    stS(qb + 1)
                stP(qb)
                if qb > 0:
                    stT(qb - 1)
            stT(NT - 1)
            dma_sp(mT_d[0:8].rearrange("c p t -> p c t"), mTa[:], [t_mTa], [DT("mT")])
            S_.barrier()

        with contextlib.ExitStack() as sc:
            wo, t_wo = sbt(sc, "wo", [128, KC, D], BF16)
            wsrc = w_out[l].rearrange("(kc kp) n -> kp kc n", kp=128)
            for nb in range(4):
                dma_pool(wo[:, :, nb * 512:(nb + 1) * 512], wsrc[:, :, nb * 512:(nb + 1) * 512], [], [t_wo])
            wr32, t_wr = sbt(sc, "wr32", [128, KC, NE])
            dma_sp(wr32[:], w_r[l].rearrange("(kc kp) e -> kp kc e", kp=128), [], [t_wr])
            rows, t_rows = sbt(sc, "rowsC", [128, 3, D])
            dma_sp(rows[:], rowsBig_d[l][:, 0:3, :], [], [t_rows])
            mTr = [sbt(sc, "mTr%d" % i, [128, KC, 512], BF16) for i in range(2)]
            xin = [sbt(sc, "xinC%d" % i, [128, D]) for i in range(2)]
            res = [sbt(sc, "resC%d" % i, [128, D]) for i in range(2)]
            x1b = [sbt(sc, "x1b%d" % i, [128, D], BF16) for i in range(2)]
            x1T, t_x1T = sbt(sc, "x1T", [128, KC, 128])
            lg, t_lg = sbt(sc, "lg", [128, NE])
            maskb, t_maskb = sbt(sc, "maskb", [128, NT, NE], BF16)
            idxf, t_idxf = sbt(sc, "idxf", [128, NT, 4])
            st_small = {"stats": sbt(sc, "stats", [128, 4, 6]), "mv": sbt(sc, "mv", [128, 4])}
            top8, t_top8 = sbt(sc, "top8", [128, 8])
            idx8, t_idx8 = sbt(sc, "idx8", [128, 8], U32)
            sm, t_sm = sbt(sc, "smC", [128, 16])
            G, t_G = sbt(sc, "G", [128, NE])
            oh, t_oh = sbt(sc, "oh", [128, NE])
            posC, t_posC = sbt(sc, "posC", [128, NE])
            slotf, t_slotf = sbt(sc, "slotf", [128, NT, 4])
            pya = [pst(sc, "pya%d" % i, [128, 512]) for i in range(2)]
            pyl = [pst(sc, "pyl%d" % i, [128, 512]) for i in range(2)]
            ptp = [pst(sc, "ptpC%d" % i, [128, 4, 128]) for i in range(2)]
            plg_all, t_plg = pst(sc, "plg", [128, 96])
            plg = plg_all[:, 0:32]
            ppos = plg_all[:, 32:96]
            t_ppos = t_plg
            runc, t_runc = sbt(sc, "runc", [128, NE])
            DVE(lambda: nc.vector.memset(runc[:], 0.0), [], [t_runc])
            pgt, t_pgt = pst(sc, "pgt", [32, 128])
            cc = {"iy": 0, "itp": 0, "mt": None}

            def stage1(tt):
                if tt % 4 == 0:
                    cc["mt"] = mTr[(tt // 4) % 2]
                    dma_sp(cc["mt"][0][:], mT_d.rearrange("c p t -> p c t")[:, :, tt * 128:tt * 128 + 512],
                           [DT("mT")], [cc["mt"][1]])
                mt, t_mt = cc["mt"]
                xi, t_xi = xin[tt % 2]
                rs, t_rs = res[tt % 2]
                xb_, t_xb = x1b[tt % 2]
                dma_sp(xi[:], xsrc[tt * 128:(tt + 1) * 128, :], [t_xsrc], [t_xi])
                DVE(lambda xi=xi: nc.vector.scalar_tensor_tensor(out=xi[:], in0=xi[:], scalar=ALPHA, in1=rows[:, 0, :],
                                                                 op0=ALU.mult, op1=ALU.add), [t_xi, t_rows], [t_xi])
                tl = (tt % 4) * 128
                for nb in range(4):
                    a_, t_a = pya[cc["iy"] % 2]
                    l_, t_l = pyl[cc["iy"] % 2]
                    cc["iy"] += 1
                    for kc in range(8):
                        PE(lambda kc=kc, a_=a_, mt=mt, tl=tl, nb=nb: nc.tensor.matmul(
                            a_[:], lhsT=mt[:, kc, tl:tl + 128], rhs=wo[:, kc, nb * 512:(nb + 1) * 512],
                            start=(kc == 0), stop=(kc == 7)), [t_mt, t_wo], [t_a])
                    for kc in range(8, 16):
                        PE(lambda kc=kc, l_=l_, mt=mt, tl=tl, nb=nb: nc.tensor.matmul(
                            l_[:], lhsT=mt[:, kc, tl:tl + 128], rhs=wo[:, kc, nb * 512:(nb + 1) * 512],
                            start=(kc == 8), stop=(kc == 15)), [t_mt, t_wo], [t_l])
                    sl = slice(nb * 512, (nb + 1) * 512)
                    DVE(lambda a_=a_, rs=rs, xi=xi, sl=sl: nc.vector.tensor_tensor(
                        out=rs[:, sl], in0=a_[:], in1=xi[:, sl], op=ALU.add), [t_a, t_xi], [t_rs])
                    DVE(lambda l_=l_, rs=rs, sl=sl, tt=tt: nc.vector.scalar_tensor_tensor(
                        out=rs[:, sl], in0=l_[:], scalar=rstdl[:, tt:tt + 1], in1=rs[:, sl],
                        op0=ALU.mult, op1=ALU.add), [t_l, t_rstdl, t_rs], [t_rs])

            def stage2(tt):
                rs, t_rs = res[tt % 2]
                xb_, t_xb = x1b[tt % 2]
                ln_tile(st_small, rs, t_rs, rows[:, 1, :], rows[:, 2, :], t_rows, "c")
                dma_sp(x1_d[tt * 128:(tt + 1) * 128, :], rs[:], [t_rs], [DT("x1")])
                ACT(lambda xb_=xb_, rs=rs: nc.scalar.copy(out=xb_[:], in_=rs[:]), [t_rs], [t_xb])
                dma_sp(x1b_d[tt * 128:(tt + 1) * 128, :], xb_[:], [t_xb], [DT("x1b")])

            def stage2b(tt):
                rs, t_rs = res[tt % 2]
                for g in range(4):
                    p_, t_p = ptp[cc["itp"] % 2]
                    cc["itp"] += 1
                    for j in range(4):
                        kc = g * 4 + j
                        PE(lambda kc=kc, j=j, p_=p_, rs=rs: nc.tensor.transpose(
                            out=p_[:, j, :], in_=rs[:, kc * 128:(kc + 1) * 128], identity=identf[:]),
                           [t_rs, t_identf], [t_p])
                    if g % 2 == 0:
                        ACT(lambda p_=p_, g=g: nc.scalar.copy(out=x1T[:, g * 4:(g + 1) * 4, :], in_=p_[:]),
                            [t_p], [t_x1T])
                    else:
                        DVE(lambda p_=p_, g=g: nc.vector.tensor_copy(out=x1T[:, g * 4:(g + 1) * 4, :], in_=p_[:]),
                            [t_p], [t_x1T])
                for kc in range(KC):
                    PE(lambda kc=kc: nc.tensor.matmul(plg, lhsT=x1T[:, kc, :], rhs=wr32[:, kc, :],
                                                      start=(kc == 0), stop=(kc == KC - 1)),
                       [t_x1T, t_wr], [t_plg])
                DVE(lambda: nc.vector.tensor_tensor(out=lg[:], in0=plg, in1=rowsB[:, RB_BR:RB_BR + NE],
                                                    op=ALU.add), [t_plg, t_rowsB], [t_lg])
                DVE(lambda: nc.vector.max(out=top8[:], in_=lg[:]), [t_lg], [t_top8])
                DVE(lambda: nc.vector.max_index(out=idx8[:], in_max=top8[:], in_values=lg[:]),
                    [t_top8, t_lg], [t_idx8])
                DVE(lambda: nc.vector.tensor_scalar(out=sm[:, 0:1], in0=top8[:, 0:1], scalar1=-1.0, scalar2=None,
                                                    op0=ALU.mult), [t_top8], [t_sm])
                ACT(lambda: nc.scalar.activation(out=sm[:, 4:8], in_=top8[:, 0:4], func=AF.Exp, bias=sm[:, 0:1],
                                                 accum_out=sm[:, 1:2]), [t_top8, t_sm], [t_sm])
                DVE(lambda: nc.vector.reciprocal(out=sm[:, 2:3], in_=sm[:, 1:2]), [t_sm], [t_sm])
                DVE(lambda tt=tt: nc.vector.tensor_scalar(out=gates[:, tt, :], in0=sm[:, 4:8], scalar1=sm[:, 2:3],
                                                          scalar2=None, op0=ALU.mult), [t_sm], [t_gates])
                DVE(lambda tt=tt: nc.vector.tensor_scalar(out=maskb[:, tt, :], in0=lg[:], scalar1=top8[:, 3:4],
                                                          scalar2=None, op0=ALU.is_ge), [t_lg, t_top8], [t_maskb])
                DVE(lambda tt=tt: nc.vector.tensor_copy(out=idxf[:, tt, :], in_=idx8[:, 0:4]), [t_idx8], [t_idxf])
                for k_ in range(4):
                    dst, t_dst = (G, t_G) if k_ == 0 else (oh, t_oh)
                    DVE(lambda k_=k_, tt=tt, dst=dst: nc.vector.tensor_scalar(
                        out=dst[:], in0=iota[:, 0:32], scalar1=idxf[:, tt, k_:k_ + 1],
                        scalar2=gates[:, tt, k_:k_ + 1], op0=ALU.is_equal, op1=ALU.mult),
                        [t_iota, t_idxf, t_gates], [t_dst])
                    if k_ > 0:
                        DVE(lambda: nc.vector.tensor_tensor(out=G[:], in0=G[:], in1=oh[:], op=ALU.add),
                            [t_G, t_oh], [t_G])
                PE(lambda: nc.tensor.transpose(out=pgt[:], in_=G[:], identity=identf[:]), [t_G, t_identf], [t_pgt])
                ACT(lambda tt=tt: nc.scalar.copy(out=GT[:, tt, :], in_=pgt[:]), [t_pgt], [t_GT])
                PE(lambda tt=tt: nc.tensor.matmul(ppos[:, 0:32], lhsT=triu[:], rhs=maskb[:, tt, :], start=True,
                                                  stop=True), [t_triu, t_maskb], [t_ppos])
                PE(lambda tt=tt: nc.tensor.matmul(ppos[:, 32:64], lhsT=onesb[:], rhs=maskb[:, tt, :], start=True,
                                                  stop=True), [t_onesb, t_maskb], [t_ppos])
                DVE(lambda: nc.vector.tensor_tensor(out=posC[:], in0=ppos[:, 0:32], in1=runc[:], op=ALU.add),
                    [t_ppos, t_runc], [t_posC])
                DVE(lambda: nc.vector.scalar_tensor_tensor(out=posC[:], in0=posC[:], scalar=float(CAP - 1),
                                                           in1=iota[:, 32:64], op0=ALU.min, op1=ALU.add),
                    [t_posC, t_iota], [t_posC])
                DVE(lambda: nc.vector.tensor_tensor(out=runc[:], in0=runc[:], in1=ppos[:, 32:64], op=ALU.add),
                    [t_ppos, t_runc], [t_runc])
                for k_ in range(4):
                    DVE(lambda k_=k_, tt=tt: nc.vector.scalar_tensor_tensor(
                        out=oh[:], in0=iota[:, 0:32], scalar=idxf[:, tt, k_:k_ + 1], in1=posC[:],
                        op0=ALU.is_equal, op1=ALU.mult), [t_iota, t_idxf, t_posC], [t_oh])
                    DVE(lambda k_=k_, tt=tt: nc.vector.reduce_sum(out=slotf[:, tt, k_:k_ + 1], in_=oh[:], axis=AX.X),
                        [t_oh], [t_slotf])
                DVE(lambda tt=tt: nc.vector.tensor_copy(out=sloti[:, tt, :], in_=slotf[:, tt, :]),
                    [t_slotf], [t_slotis[tt]])
                for k_ in range(4):
                    S_.dma("pool", lambda tt=tt, k_=k_: nc.gpsimd.indirect_dma_start(
                        out=stok_d[:, :], out_offset=bass.IndirectOffsetOnAxis(ap=sloti[:, tt, k_:k_ + 1], axis=0),
                        in_=tokid[:, tt:tt + 1], in_offset=None), [t_slotis[tt], t_tokid], [DT("stok")])

            stage1(0)
            for tt in range(NT):
                stage2(tt)
                if tt + 1 < NT:
                    stage1(tt + 1)
                stage2b(tt)
            S_.barrier()

        with contextlib.ExitStack() as sd:
            bupT, t_bup = sbt(sd, "bupT", [128, NE * 16])
            dma_sp(bupT[:], bupT_d[l], [], [t_bup])
            sidx = [sbt(sd, "sidx%d" % i, [128, 3], I32) for i in range(2)]
            xs = [[sbt(sd, "xs%d_%d" % (i, j), [128, D], BF16) for j in range(3)] for i in range(2)]
            xsT = [sbt(sd, "xsT%d" % i, [128, KC, CAP], BF16) for i in range(2)]
            NWU = 4
            NWD = 4
            wu = [sbt(sd, "wu%d" % i, [128, KC, 512], BF16) for i in range(NWU)]
            wd = [sbt(sd, "wd%d" % i, [128, 8, 512], BF16) for i in range(NWD)]
            gs = [sbt(sd, "gs%d" % i, [128, 4, CAP]) for i in range(2)]
            sg = [sbt(sd, "sg%d" % i, [128, CAP]) for i in range(2)]
            ln_ = [sbt(sd, "ln%d" % i, [128, CAP]) for i in range(2)]
            hT = [sbt(sd, "hT%d" % i, [128, 8, CAP], BF16) for i in range(1)]
            yo = [sbt(sd, "yo%d" % i, [128, 512]) for i in range(4)]
            ptr_ = [pst(sd, "ptrD%d" % i, [128, 8, 128], BF16) for i in range(2)]
            pu = [pst(sd, "pu%d" % i, [128, CAP]) for i in range(3)]
            pd = [pst(sd, "pd%d" % i, [128, 512]) for i in range(3)]
            cnt = {"wu": 0, "wd": 0, "tr": 0, "pu": 0, "pd": 0, "gs": 0, "sg": 0, "yo": 0}

            def gather(e):
                si, t_si = sidx[e % 2]
                dma_sp(si[:], stok_d[e * CAP:(e + 1) * CAP, :].rearrange("(p j) o -> p (j o)", p=128),
                       [DT("stok")], [t_si])
                for j in range(3):
                    xs_, t_xs = xs[e % 2][j]
                    S_.dma("pool", lambda xs_=xs_, si=si, j=j: nc.gpsimd.indirect_dma_start(
                        out=xs_[:, :], out_offset=None, in_=x1b_d[:, :],
                        in_offset=bass.IndirectOffsetOnAxis(ap=si[:, j:j + 1], axis=0)),
                        [t_si, DT("x1b")], [t_xs])

            def transp(e):
                xsT_, t_xsT = xsT[e % 2]
                for j in range(3):
                    xs_, t_xs = xs[e % 2][j]
                    for g in range(2):
                        p_, t_p = ptr_[cnt["tr"] % 2]
                        cnt["tr"] += 1
                        for c in range(8):
                            kc = g * 8 + c
                            PE(lambda p_=p_, c=c, kc=kc, xs_=xs_: nc.tensor.transpose(
                                out=p_[:, c, :], in_=xs_[:, kc * 128:(kc + 1) * 128], identity=identb[:]),
                               [t_xs, t_identb], [t_p])
                        dst = xsT_[:, g * 8:(g + 1) * 8, j * 128:(j + 1) * 128]
                        if cnt["tr"] % 2 == 0:
                            ACT(lambda p_=p_, dst=dst: nc.scalar.copy(out=dst, in_=p_[:]), [t_p], [t_xsT])
                        else:
                            DVE(lambda p_=p_, dst=dst: nc.vector.tensor_copy(out=dst, in_=p_[:]), [t_p], [t_xsT])

            gather(0)
            transp(0)
            for e in range(NE):
                xsT_, t_xsT = xsT[e % 2]
                hT_, t_hT = hT[0]
                if e + 1 < NE:
                    gather(e + 1)
                usrc = w_up[l, e].rearrange("(kc kp) n -> kp kc n", kp=128)
                for hf in range(2):
                    gs_, t_gs = gs[cnt["gs"] % 2]
                    cnt["gs"] += 1
                    for part in range(2):
                        w_, t_w = wu[cnt["wu"] % NWU]
                        cnt["wu"] += 1
                        c0 = part * 1024 + hf * 512
                        dma_pool(w_[:], usrc[:, :, c0:c0 + 512], [], [t_w])
                        for j4 in range(4):
                            fc = hf * 4 + j4
                            p_, t_p = pu[cnt["pu"] % 3]
                            cnt["pu"] += 1
                            for kc in range(KC):
                                PE(lambda p_=p_, w_=w_, j4=j4, kc=kc, xsT_=xsT_: nc.tensor.matmul(
                                    p_[:], lhsT=w_[:, kc, j4 * 128:(j4 + 1) * 128], rhs=xsT_[:, kc, :],
                                    start=(kc == 0), stop=(kc == KC - 1)), [t_w, t_xsT], [t_p])
                            bcol = bupT[:, e * 16 + part * 8 + fc:e * 16 + part * 8 + fc + 1]
                            if part == 0:
                                sg_, t_sg = sg[cnt["sg"] % 2]
                                cnt["sg"] += 1
                                DVE(lambda p_=p_, gs_=gs_, j4=j4, bcol=bcol: nc.vector.tensor_scalar(
                                    out=gs_[:, j4, :], in0=p_[:], scalar1=bcol, scalar2=7.0, op0=ALU.add,
                                    op1=ALU.min), [t_p, t_bup], [t_gs])
                                ACT(lambda sg_=sg_, gs_=gs_, j4=j4: nc.scalar.activation(
                                    out=sg_[:], in_=gs_[:, j4, :], func=AF.Sigmoid, scale=1.702), [t_gs], [t_sg])
                                DVE(lambda sg_=sg_, gs_=gs_, j4=j4: nc.vector.tensor_tensor(
                                    out=gs_[:, j4, :], in0=gs_[:, j4, :], in1=sg_[:], op=ALU.mult),
                                    [t_gs, t_sg], [t_gs])
                            else:
                                l_, t_l = ln_[cnt["sg"] % 2]
                                cnt["sg"] += 1
                                DVE(lambda p_=p_, l_=l_, bcol=bcol: nc.vector.tensor_scalar(
                                    out=l_[:], in0=p_[:], scalar1=bcol, scalar2=7.0, op0=ALU.add, op1=ALU.min),
                                    [t_p, t_bup], [t_l])
                                DVE(lambda l_=l_: nc.vector.tensor_scalar(
                                    out=l_[:], in0=l_[:], scalar1=-7.0, scalar2=1.0, op0=ALU.max, op1=ALU.add),
                                    [t_l], [t_l])
                                DVE(lambda l_=l_, gs_=gs_, j4=j4, fc=fc, hT_=hT_: nc.vector.tensor_tensor(
                                    out=hT_[:, fc, :], in0=l_[:], in1=gs_[:, j4, :], op=ALU.mult),
                                    [t_l, t_gs], [t_hT])
                if e + 1 < NE:
                    transp(e + 1)
                dsrc = w_dn[l, e].rearrange("(fc fp) n -> fp fc n", fp=128)
                for nb in range(4):
                    w_, t_w = wd[cnt["wd"] % NWD]
                    cnt["wd"] += 1
                    dma_pool(w_[:], dsrc[:, :, nb * 512:(nb + 1) * 512], [], [t_w])
                    for j in range(3):
                        p_, t_p = pd[cnt["pd"] % 3]
                        cnt["pd"] += 1
                        for fc in range(8):
                            PE(lambda p_=p_, fc=fc, j=j, w_=w_, hT_=hT_: nc.tensor.matmul(
                                p_[:], lhsT=hT_[:, fc, j * 128:(j + 1) * 128], rhs=w_[:, fc, :],
                                start=(fc == 0), stop=(fc == 7)), [t_hT, t_w], [t_p])
                        yt, t_yt = yo[cnt["yo"] % 4]
                        cnt["yo"] += 1
                        if cnt["yo"] % 2 == 0:
                            ACT(lambda p_=p_, yt=yt: nc.scalar.copy(out=yt[:], in_=p_[:]), [t_p], [t_yt])
                        else:
                            DVE(lambda p_=p_, yt=yt: nc.vector.tensor_copy(out=yt[:], in_=p_[:]), [t_p], [t_yt])
                        dma_sp(ys_d[e * CAP:(e + 1) * CAP, :].rearrange("(p j) n -> p j n", j=3)[:, j, nb * 512:(nb + 1) * 512], yt[:],
                               [t_yt], [DT("ys")])
            S_.barrier()

        with contextlib.ExitStack() as se:
            rows, t_rows = sbt(se, "rowsE", [128, 2, D])
            dma_sp(rows[:], rowsBig_d[l][:, 3:5, :], [], [t_rows])
            bd, t_bd = sbt(se, "bd", [32, D])
            dma_sp(bd[:], b_dn[l], [], [t_bd])
            yg = [[sbt(se, "yg%d_%d" % (i, k_), [128, D]) for k_ in range(4)] for i in range(2)]
            xi2 = [sbt(se, "xi2%d" % i, [128, D]) for i in range(2)]
            st_small = {"stats": sbt(se, "statsE", [128, 4, 6]), "mv": sbt(se, "mvE", [128, 4])}
            pb = [pst(se, "pbE%d" % i, [128, 512]) for i in range(4)]
            ipb = 0
            dst_d = out_d if last else xa_d
            t_dst = DT("out") if last else DT("xa")
            def fetchE(tt):
                xi, t_xi = xi2[tt % 2]
                dma_sp(xi[:], x1_d[tt * 128:(tt + 1) * 128, :], [DT("x1")], [t_xi])
                for k_ in range(4):
                    y_, t_y = yg[tt % 2][k_]
                    S_.dma("pool", lambda y_=y_, tt=tt, k_=k_: nc.gpsimd.indirect_dma_start(
                        out=y_[:, :], out_offset=None, in_=ys_d[:, :],
                        in_offset=bass.IndirectOffsetOnAxis(ap=sloti[:, tt, k_:k_ + 1], axis=0)),
                        [t_slotis[tt], DT("ys")], [t_y])

            fetchE(0)
            for tt in range(NT):
                xi, t_xi = xi2[tt % 2]
                if tt + 1 < NT:
                    fetchE(tt + 1)
                for nb in range(4):
                    p_, t_p = pb[ipb % 4]
                    ipb += 1
                    PE(lambda p_=p_, tt=tt, nb=nb: nc.tensor.matmul(p_[:], lhsT=GT[:, tt, :],
                                                                    rhs=bd[:, nb * 512:(nb + 1) * 512],
                                                                    start=True, stop=True), [t_GT, t_bd], [t_p])
                    sl = slice(nb * 512, (nb + 1) * 512)
                    DVE(lambda p_=p_, xi=xi, sl=sl: nc.vector.scalar_tensor_tensor(
                        out=xi[:, sl], in0=xi[:, sl], scalar=ALPHA, in1=p_[:], op0=ALU.mult, op1=ALU.add),
                        [t_xi, t_p], [t_xi])
                for k_ in range(4):
                    y_, t_y = yg[tt % 2][k_]
                    DVE(lambda y_=y_, xi=xi, tt=tt, k_=k_: nc.vector.scalar_tensor_tensor(
                        out=xi[:], in0=y_[:], scalar=gates[:, tt, k_:k_ + 1], in1=xi[:], op0=ALU.mult,
                        op1=ALU.add), [t_y, t_gates, t_xi], [t_xi])
                ln_tile(st_small, xi, t_xi, rows[:, 0, :], rows[:, 1, :], t_rows, "e")
                dma_sp(dst_d[tt * 128:(tt + 1) * 128, :], xi[:], [t_xi], [t_dst])
            S_.barrier()
    S_.barrier()
    es.close()
    return nc


def _consts():
    import ml_dtypes
    bf = ml_dtypes.bfloat16
    p = np.arange(128)
    d = p % 64
    inv_freq = (1.0 / (np.float32(10000.0) ** (np.arange(0, 64, 2, dtype=np.float32) / np.float32(64)))).astype(np.float32)
    ang = (np.arange(S, dtype=np.float32)[:, None] * inv_freq[None, :]).astype(np.float32)
    cos = np.cos(ang).astype(np.float32)
    sin = np.sin(ang).astype(np.float32)
    cosT = np.ascontiguousarray(cos[:, d % 32].T)
    sgn = np.where(d < 32, -1.0, 1.0).astype(np.float32)
    sinT = np.ascontiguousarray(sin[:, d % 32].T * sgn[:, None]).astype(np.float32)
    partner = 64 * (p // 64) + ((p % 64) + 32) % 64
    permR = np.zeros((128, 128), np.float32)
    permR[partner, p] = 1.0
    kk = np.arange(128)[:, None]
    qq = np.arange(128)[None, :]
    mprev = np.tile((kk > qq).astype(np.float32), (1, 8)).astype(bf)
    mcur = np.tile((kk <= qq).astype(np.float32), (1, 8)).astype(bf)
    triu = (kk < qq).astype(np.float32).astype(bf)
    iota = np.zeros((128, 64), np.float32)
    iota[:, 0:32] = np.arange(32)[None, :]
    iota[:, 32:64] = np.arange(32)[None, :] * CAP
    tokid = (np.arange(NT)[None, :] * 128 + p[:, None]).astype(np.int32)
    return {
        "c_identf": np.eye(128, dtype=np.float32), "c_identb": np.eye(128, dtype=np.float32).astype(bf),
        "c_cos": cosT, "c_sin": sinT, "c_permR": permR, "c_mprev": mprev, "c_mcur": mcur, "c_triu": triu,
        "c_onesb": np.ones((128, 128), np.float32).astype(bf), "c_iota": iota, "c_tokid": tokid,
        "c_zero": np.zeros((128, 96), np.int32),
    }


def _prep(inp, layers):
    ls = list(layers)
    f = lambda k: np.asarray(inp[k])
    pq = np.arange(1024)
    cq, pp = pq // 128, pq % 128
    perm_q = (np.where(pp < 64, cq, 8 + cq) * 64 + (pp % 64))
    perm = np.concatenate([perm_q, np.arange(1024, DIN)])
    w_in = np.ascontiguousarray(f("w_in")[ls][:, :, perm])
    b_in = f("b_in")[ls][:, perm]
    Ln = len(ls)
    colsA = np.zeros((Ln, 128, NA), np.float32)
    col = lambda v: v.reshape(Ln, -1, 128).transpose(0, 2, 1)
    colsA[:, :, CA_BQ:CA_BQ + 8] = col(b_in[:, 0:1024])
    colsA[:, :, CA_BK:CA_BK + 1] = col(b_in[:, 1024:1152])
    colsA[:, :, CA_BUX:CA_BUX + 8] = col(b_in[:, 1280:2304])
    colsA[:, :, CA_BUG:CA_BUG + 8] = col(b_in[:, 2304:3328])
    cw = f("conv_w")[ls]
    for k in range(4):
        colsA[:, :, CA_CW + k * 8:CA_CW + (k + 1) * 8] = col(cw[:, k, :])
    colsA[:, :, CA_CB:CA_CB + 8] = col(f("conv_b")[ls])
    colsA[:, :, CA_BA:CA_BA + 8] = col(f("lru_b_a")[ls])
    colsA[:, :, CA_BXG:CA_BXG + 8] = col(f("lru_b_x")[ls])
    colsA[:, :, CA_LAM:CA_LAM + 8] = col(f("lru_lambda")[ls])
    colsA[:, :, CA_GA:CA_GA + 8] = col(f("g_attn")[ls])
    colsA[:, :, CA_GL:CA_GL + 8] = col(f("g_lru")[ls])
    rowsB = np.zeros((Ln, 128, NB), np.float32)
    rowsB[:, :, RB_BV:RB_BV + 128] = b_in[:, None, 1152:1280]
    rowsB[:, :, RB_BR:RB_BR + 32] = f("b_router")[ls][:, None, :]
    rowsB[:, :, RB_SK:RB_SK + 16] = f("attn_sinks")[ls][:, None, :]
    rowsBig = np.zeros((Ln, 128, 5, D), np.float32)
    for i, k in enumerate(["b_out", "ln1_g", "ln1_b", "ln2_g", "ln2_b"]):
        rowsBig[:, :, i, :] = f(k)[ls][:, None, :]

    def bdiag(w):
        w = w[ls]
        o = np.zeros((Ln, 128, 8, 128), np.float32)
        for c in range(8):
            o[:, 0:64, c, 0:64] = w[:, 2 * c]
            o[:, 64:128, c, 64:128] = w[:, 2 * c + 1]
        return o
    bup = f("b_up")[ls]
    bupT = np.ascontiguousarray(bup.reshape(Ln, NE, 16, 128).transpose(0, 3, 1, 2).reshape(Ln, 128, NE * 16))
    m = {
        "w_in": w_in, "colsA": colsA, "rowsB": rowsB, "rowsBig": rowsBig,
        "wab": bdiag(f("lru_w_a")), "wxb": bdiag(f("lru_w_x")),
        "w_out": np.ascontiguousarray(f("w_out")[ls]), "w_router": np.ascontiguousarray(f("w_router")[ls]),
        "w_up": np.ascontiguousarray(f("w_up")[ls]), "bupT": bupT,
        "w_down": np.ascontiguousarray(f("w_down")[ls]), "b_down": np.ascontiguousarray(f("b_down")[ls]),
    }
    m.update(_consts())
    return m


_NC_CACHE = {}


def _get_nc(nl):
    if nl not in _NC_CACHE:
        _NC_CACHE[nl] = build(nl)
    return _NC_CACHE[nl]


FUSED = True


def kernel(**inputs):
    x = np.asarray(inputs["x"], dtype=np.float32)
    B = x.shape[0]
    if FUSED:
        nc = _get_nc(DEPTH)
        shared = _prep(inputs, range(DEPTH))
        in_maps = [dict(shared, x=np.ascontiguousarray(x[b])) for b in range(B)]
        res = run_bass_kernel_spmd(nc, in_maps, core_ids=list(range(B)))
        return np.stack([np.asarray(r["out"]) for r in res.results], axis=0).astype(np.float32)
    cur = [np.ascontiguousarray(x[b]) for b in range(B)]
    for l in range(DEPTH):
        nc = _get_nc(1)
        shared = _prep(inputs, [l])
        in_maps = [dict(shared, x=cur[b]) for b in range(B)]
        res = run_bass_kernel_spmd(nc, in_maps, core_ids=list(range(B)))
        cur = [np.ascontiguousarray(np.asarray(r["out"], dtype=np.float32)) for r in res.results]
    return np.stack(cur, axis=0).astype(np.float32)
```

```python
import contextlib
import numpy as np
import concourse.bass as bass
import concourse.mybir as mybir
from concourse.bass_utils import run_bass_kernel_spmd

F32 = mybir.dt.float32
BF16 = mybir.dt.bfloat16
I32 = mybir.dt.int32
U32 = mybir.dt.uint32
AF = mybir.ActivationFunctionType
ALU = mybir.AluOpType
AX = mybir.AxisListType

D = 2048
S = 2048
NT = 16
KC = 16
DIN = 3328
NE = 32
CAP = 384
NSLOT = NE * CAP
DEPTH = 4
ALPHA = float((2 * DEPTH) ** 0.25)
LN_EPS = 1e-5
RMS_EPS = 1e-6
GELU_NATIVE = False

CA_BQ = 0
CA_BK = 8
CA_BUX = 9
CA_BUG = 17
CA_CW = 25
CA_CB = 57
CA_BA = 65
CA_BXG = 73
CA_LAM = 81
CA_GA = 89
CA_GL = 97
NA = 105
RB_BV = 0
RB_BR = 128
RB_SK = 160
NB = 176


class Tk:
    __slots__ = ("w", "r")

    def __init__(self):
        self.w = None
        self.r = {}


class Sched:
    NDS = 24

    def __init__(self, nc, es):
        self.nc = nc
        self.eng = {"pe": nc.tensor, "act": nc.scalar, "dve": nc.vector, "pool": nc.gpsimd, "sp": nc.sync}
        self.semobj = {}
        for k in self.eng:
            self.semobj[k] = es.enter_context(nc.semaphore("s_" + k))
        self.nds = {"sp": 24, "pool": 32, "act": 4}
        self.dcnt = {}
        self.dnext = {}
        for q, n in self.nds.items():
            for i in range(n):
                self.semobj[("d", q, i)] = es.enter_context(nc.semaphore("d%s%d" % (q, i)))
                self.dcnt[(q, i)] = 0
            self.dnext[q] = 0
        self.cnt = {k: 0 for k in self.eng}
        self.seen = {k: {} for k in self.eng}

    def _wait(self, e, key, val):
        if self.seen[e].get(key, 0) >= val:
            return
        self.seen[e][key] = val
        self.eng[e].wait_ge(self.semobj[key], val)

    def _deps(self, e, reads, writes, is_dma):
        for t in reads:
            if t.w is not None:
                self._wait(e, t.w[0], t.w[1])
        for t in writes:
            if t.w is not None and (is_dma or t.w[0] != e):
                self._wait(e, t.w[0], t.w[1])
            for key, val in t.r.items():
                if is_dma or key != e:
                    self._wait(e, key, val)

    def _mark(self, ev, reads, writes):
        for t in reads:
            if t.r.get(ev[0], 0) < ev[1]:
                t.r[ev[0]] = ev[1]
        for t in writes:
            t.w = ev
            t.r = {}

    def op(self, e, fn, reads=(), writes=()):
        self._deps(e, reads, writes, False)
        ins = fn()
        self.cnt[e] += 1
        ins.then_inc(self.semobj[e], 1)
        self._mark((e, self.cnt[e]), reads, writes)

    def dma(self, q, fn, reads=(), writes=()):
        i = self.dnext[q]
        self.dnext[q] = (i + 1) % self.nds[q]
        key = ("d", q, i)
        if self.dcnt[(q, i)] > 0:
            self._wait(q, key, 16 * self.dcnt[(q, i)])
        self._deps(q, reads, writes, True)
        ins = fn()
        self.dcnt[(q, i)] += 1
        ins.then_inc(self.semobj[key], 16)
        self._mark((key, 16 * self.dcnt[(q, i)]), reads, writes)

    def barrier(self):
        evs = [(e, c) for e, c in self.cnt.items() if c > 0]
        evs += [(("d", q, i), 16 * c) for (q, i), c in self.dcnt.items() if c > 0]
        for e in self.eng:
            for key, val in evs:
                if key != e:
                    self._wait(e, key, val)


def build(nlayers, dbg=()):
    nc = bass.Bass("TRN2", target_bir_lowering=False)
    L = nlayers

    def din(name, shape, dt=F32):
        return nc.dram_tensor(name, list(shape), dt, kind="ExternalInput").ap()

    def dscr(name, shape, dt=F32):
        kind = "ExternalOutput" if name in dbg else "Internal"
        return nc.dram_tensor(name, list(shape), dt, kind=kind).ap()

    x_in = din("x", [S, D])
    w_in = din("w_in", [L, D, DIN])
    colsA_d = din("colsA", [L, 128, NA])
    rowsB_d = din("rowsB", [L, 128, NB])
    rowsBig_d = din("rowsBig", [L, 128, 5, D])
    wab_d = din("wab", [L, 128, 8, 128])
    wxb_d = din("wxb", [L, 128, 8, 128])
    w_out = din("w_out", [L, D, D])
    w_r = din("w_router", [L, D, NE])
    w_up = din("w_up", [L, NE, D, 2 * 1024])
    bupT_d = din("bupT", [L, 128, NE * 16])
    w_dn = din("w_down", [L, NE, 1024, D])
    b_dn = din("b_down", [L, NE, D])
    c_identf = din("c_identf", [128, 128])
    c_identb = din("c_identb", [128, 128], BF16)
    c_cos = din("c_cos", [128, S])
    c_sin = din("c_sin", [128, S])
    c_permR = din("c_permR", [128, 128])
    c_mprev = din("c_mprev", [128, 1024], BF16)
    c_mcur = din("c_mcur", [128, 1024], BF16)
    c_triu = din("c_triu", [128, 128], BF16)
    c_onesb = din("c_onesb", [128, 128], BF16)
    c_iota = din("c_iota", [128, 64])
    c_tokid = din("c_tokid", [128, NT], I32)
    c_zero = din("c_zero", [128, 96], I32)
    out_d = nc.dram_tensor("out", [S, D], F32, kind="ExternalOutput").ap()

    qT_d = dscr("qT_d", [9, 128, S], BF16)
    V_d = dscr("V_d", [128, NT * 2 * 65], BF16)
    mT_d = dscr("mT_d", [16, 128, S], BF16)
    xa_d = dscr("xa_d", [S, D])
    x1_d = dscr("x1_d", [S, D])
    x1b_d = dscr("x1b_d", [S, D], BF16)
    ys_d = dscr("ys_d", [NSLOT, D])
    stok_d = dscr("stok_d", [NSLOT, 1], I32)
    dbg_d = dscr("dbg_d", [128, 4096])

    es = contextlib.ExitStack()
    S_ = Sched(nc, es)

    uniq = [0]

    def sbt(st, name, shape, dt=F32):
        uniq[0] += 1
        return st.enter_context(nc.sbuf_tensor("sb%d_%s" % (uniq[0], name), list(shape), dt)), Tk()

    def pst(st, name, shape, dt=F32):
        uniq[0] += 1
        return st.enter_context(nc.psum_tensor("ps%d_%s" % (uniq[0], name), list(shape), dt)), Tk()

    PE = lambda fn, r, w: S_.op("pe", fn, r, w)
    ACT = lambda fn, r, w: S_.op("act", fn, r, w)
    DVE = lambda fn, r, w: S_.op("dve", fn, r, w)
    POOL = lambda fn, r, w: S_.op("pool", fn, r, w)
    dramT = {}

    def DT(ap_name):
        if ap_name not in dramT:
            dramT[ap_name] = Tk()
        return dramT[ap_name]

    def dma_sp(out, in_, r, w):
        S_.dma("sp", lambda: nc.sync.dma_start(out=out, in_=in_), r, w)

    def dma_pool(out, in_, r, w):
        S_.dma("pool", lambda: nc.gpsimd.dma_start(out=out, in_=in_), r, w)

    identf, t_identf = sbt(es, "identf", [128, 128])
    identb, t_identb = sbt(es, "identb", [128, 128], BF16)
    onesb, t_onesb = sbt(es, "onesb", [128, 128], BF16)
    triu, t_triu = sbt(es, "triu", [128, 128], BF16)
    iota, t_iota = sbt(es, "iota", [128, 64])
    tokid, t_tokid = sbt(es, "tokid", [128, NT], I32)
    onesf, t_onesf = sbt(es, "onesf", [128, 2])
    colsA, t_colsA = sbt(es, "colsA", [128, NA])
    rowsB, t_rowsB = sbt(es, "rowsB", [128, NB])
    c12, t_c12 = sbt(es, "c12", [128, 16])
    rstdl, t_rstdl = sbt(es, "rstdl", [128, NT])
    gates, t_gates = sbt(es, "gates", [128, NT, 4])
    sloti, t_sloti = sbt(es, "sloti", [128, NT, 4], I32)
    t_slotis = [Tk() for _ in range(NT)]
    GT, t_GT = sbt(es, "GT", [32, NT, 128])
    exps, t_exps = sbt(es, "exps", [128, 16])

    dma_sp(identf[:], c_identf, [], [t_identf])
    dma_sp(identb[:], c_identb, [], [t_identb])
    dma_sp(onesb[:], c_onesb, [], [t_onesb])
    dma_sp(triu[:], c_triu, [], [t_triu])
    dma_sp(iota[:], c_iota, [], [t_iota])
    dma_sp(tokid[:], c_tokid, [], [t_tokid])
    DVE(lambda: nc.vector.memset(onesf[:], 1.0), [], [t_onesf])
    with contextlib.ExitStack() as st0:
        zt, t_zt = sbt(st0, "zt", [128, 96], I32)
        dma_sp(zt[:], c_zero, [], [t_zt])
        dma_sp(stok_d.rearrange("(p j) o -> p (j o)", p=128), zt[:], [t_zt], [DT("stok")])
        S_.barrier()

    def ln_tile(st_small, res, t_res, grow, brow, t_rows, tagname):
        stats, t_stats = st_small["stats"]
        mv, t_mv = st_small["mv"]
        for j in range(4):
            DVE(lambda j=j: nc.vector.bn_stats(out=stats[:, j, :], in_=res[:, j * 512:(j + 1) * 512]),
                [t_res], [t_stats])
        DVE(lambda: nc.vector.bn_aggr(out=mv[:, 0:2], in_=stats[:]), [t_stats], [t_mv])
        ACT(lambda: nc.scalar.activation(out=mv[:, 2:3], in_=mv[:, 1:2], func=AF.Sqrt, bias=LN_EPS), [t_mv], [t_mv])
        DVE(lambda: nc.vector.reciprocal(out=mv[:, 2:3], in_=mv[:, 2:3]), [t_mv], [t_mv])
        DVE(lambda: nc.vector.scalar_tensor_tensor(out=mv[:, 3:4], in0=mv[:, 0:1], scalar=-1.0, in1=mv[:, 2:3],
                                                   op0=ALU.mult, op1=ALU.mult), [t_mv], [t_mv])
        ACT(lambda: nc.scalar.activation(out=res[:], in_=res[:], func=AF.Identity, scale=mv[:, 2:3],
                                         bias=mv[:, 3:4]), [t_res, t_mv], [t_res])
        POOL(lambda: nc.gpsimd.tensor_tensor(out=res[:], in0=res[:], in1=grow, op=ALU.mult),
             [t_res, t_rows], [t_res])
        POOL(lambda: nc.gpsimd.tensor_tensor(out=res[:], in0=res[:], in1=brow, op=ALU.add),
             [t_res, t_rows], [t_res])

    for l in range(L):
        xsrc = x_in if l == 0 else xa_d
        t_xsrc = DT("x_in") if l == 0 else DT("xa")
        last = (l == L - 1)
        dma_sp(colsA[:], colsA_d[l], [], [t_colsA])
        dma_sp(rowsB[:], rowsB_d[l], [], [t_rowsB])
        ACT(lambda: nc.scalar.activation(out=c12[:, 0:8], in_=colsA[:, CA_LAM:CA_LAM + 8], func=AF.Exp, scale=-1.0),
            [t_colsA], [t_c12])
        ACT(lambda: nc.scalar.activation(out=c12[:, 0:8], in_=c12[:, 0:8], func=AF.Ln, bias=1.0),
            [t_c12], [t_c12])
        DVE(lambda: nc.vector.tensor_scalar(out=c12[:, 8:16], in0=c12[:, 0:8], scalar1=-16.0, scalar2=None,
                                            op0=ALU.mult), [t_c12], [t_c12])
        DVE(lambda: nc.vector.tensor_scalar(out=c12[:, 0:8], in0=c12[:, 0:8], scalar1=-8.0, scalar2=None,
                                            op0=ALU.mult), [t_c12], [t_c12])
        ACT(lambda: nc.scalar.activation(out=exps[:], in_=rowsB[:, RB_SK:RB_SK + 16], func=AF.Exp),
            [t_rowsB], [t_exps])

        with contextlib.ExitStack() as sa:
            xT, t_xT = sbt(sa, "xT", [128, KC, S], BF16)
            wring = [sbt(sa, "wr%d" % i, [128, KC, 512], BF16) for i in range(2)]
            wcnt = [0]
            with contextlib.ExitStack() as s1:
                xin = [sbt(s1, "xin%d" % i, [128, D]) for i in range(2)]
                tp = [pst(s1, "tp%d" % i, [128, 4, 128]) for i in range(2)]
                k = 0
                for tt in range(NT):
                    xi, t_xi = xin[tt % 2]
                    dma_sp(xi[:], xsrc[tt * 128:(tt + 1) * 128, :], [t_xsrc], [t_xi])
                    for g in range(4):
                        p_, t_p = tp[k % 2]
                        for j in range(4):
                            kc = g * 4 + j
                            PE(lambda kc=kc, j=j, p_=p_, xi=xi: nc.tensor.transpose(
                                out=p_[:, j, :], in_=xi[:, kc * 128:(kc + 1) * 128], identity=identf[:]),
                               [t_xi, t_identf], [t_p])
                        dst = xT[:, g * 4:(g + 1) * 4, tt * 128:(tt + 1) * 128]
                        if k % 2 == 0:
                            ACT(lambda p_=p_, dst=dst: nc.scalar.copy(out=dst, in_=p_[:]), [t_p], [t_xT])
                        else:
                            DVE(lambda p_=p_, dst=dst: nc.vector.tensor_copy(out=dst, in_=p_[:]), [t_p], [t_xT])
                        k += 1
                S_.barrier()

            def wload(colspecs):
                wt, t_wt = wring[wcnt[0] % 2]
                wcnt[0] += 1
                src = w_in[l].rearrange("(kc kp) n -> kp kc n", kp=128)
                for (d0, s0, n) in colspecs:
                    dma_pool(wt[:, :, d0:d0 + n], src[:, :, s0:s0 + n], [], [t_wt])
                return wt, t_wt

            with contextlib.ExitStack() as s1:
                cosT, t_cos = sbt(s1, "cosT", [128, S])
                sinT, t_sin = sbt(s1, "sinT", [128, S])
                permR, t_permR = sbt(s1, "permR", [128, 128])
                dma_sp(cosT[:], c_cos, [], [t_cos])
                dma_sp(sinT[:], c_sin, [], [t_sin])
                dma_sp(permR[:], c_permR, [], [t_permR])
                qf = [sbt(s1, "qf%d" % i, [128, 512]) for i in range(2)]
                t1 = [sbt(s1, "t1%d" % i, [128, 512]) for i in range(2)]
                qo = [sbt(s1, "qo%d" % i, [128, S], BF16) for i in range(2)]
                Vt, t_V = sbt(s1, "Vt", [128, NT * 2 * 65], BF16)
                pa = [pst(s1, "pa%d" % i, [128, 512]) for i in range(3)]
                pr = [pst(s1, "pr%d" % i, [128, 512]) for i in range(2)]
                pv = [pst(s1, "pv%d" % i, [128, 128]) for i in range(2)]
                DVE(lambda: nc.vector.memset(Vt[:], 1.0), [], [t_V])
                it = 0
                for g in range(3):
                    ncols = 512 if g < 2 else 256
                    wt, t_wt = wload([(0, g * 512, ncols)])
                    nch = 4 if g < 2 else 1
                    for j in range(nch):
                        ch = g * 4 + j
                        bcol = colsA[:, CA_BQ + ch:CA_BQ + ch + 1]
                        qo_, t_qo = qo[ch % 2]
                        for tb in range(4):
                            p_, t_p = pa[it % 3]
                            r_, t_r = pr[it % 2]
                            qf_, t_qf = qf[it % 2]
                            t1_, t_t1 = t1[it % 2]
                            it += 1
                            for kc in range(KC):
                                PE(lambda kc=kc, p_=p_, wt=wt, j=j, tb=tb: nc.tensor.matmul(
                                    p_[:], lhsT=wt[:, kc, j * 128:(j + 1) * 128],
                                    rhs=xT[:, kc, tb * 512:(tb + 1) * 512], start=(kc == 0), stop=(kc == KC - 1)),
                                   [t_wt, t_xT], [t_p])
                            ACT(lambda p_=p_, qf_=qf_, bcol=bcol: nc.scalar.activation(
                                out=qf_[:], in_=p_[:], func=AF.Identity, bias=bcol), [t_p, t_colsA], [t_qf])
                            PE(lambda r_=r_, qf_=qf_: nc.tensor.matmul(r_[:], lhsT=permR[:], rhs=qf_[:],
                                                                       start=True, stop=True),
                               [t_permR, t_qf], [t_r])
                            sl = slice(tb * 512, (tb + 1) * 512)
                            DVE(lambda t1_=t1_, qf_=qf_, sl=sl: nc.vector.tensor_tensor(
                                out=t1_[:], in0=qf_[:], in1=cosT[:, sl], op=ALU.mult), [t_qf, t_cos], [t_t1])
                            DVE(lambda qf_=qf_, r_=r_, sl=sl: nc.vector.tensor_tensor(
                                out=qf_[:], in0=r_[:], in1=sinT[:, sl], op=ALU.mult), [t_r, t_sin], [t_qf])
                            DVE(lambda qo_=qo_, t1_=t1_, qf_=qf_, sl=sl: nc.vector.tensor_tensor(
                                out=qo_[:, sl], in0=t1_[:], in1=qf_[:], op=ALU.add), [t_t1, t_qf], [t_qo])
                        dma_sp(qT_d[ch], qo_[:], [t_qo], [DT("qT")])
                    if g == 2:
                        for tt in range(NT):
                            p_, t_p = pv[tt % 2]
                            for kc in range(KC):
                                PE(lambda kc=kc, p_=p_, tt=tt, wt=wt: nc.tensor.matmul(
                                    p_[:], lhsT=xT[:, kc, tt * 128:(tt + 1) * 128], rhs=wt[:, kc, 128:256],
                                    start=(kc == 0), stop=(kc == KC - 1)), [t_wt, t_xT], [t_p])
                            for hk in range(2):
                                o0 = (tt * 2 + hk) * 65
                                DVE(lambda p_=p_, hk=hk, o0=o0: nc.vector.tensor_tensor(
                                    out=Vt[:, o0:o0 + 64], in0=p_[:, hk * 64:(hk + 1) * 64],
                                    in1=rowsB[:, RB_BV + hk * 64:RB_BV + (hk + 1) * 64], op=ALU.add),
                                    [t_p, t_rowsB], [t_V])
                        dma_sp(V_d, Vt[:], [t_V], [DT("V")])
                S_.barrier()

            with contextlib.ExitStack() as s2:
                wab, t_wab = sbt(s2, "wab", [128, 8, 128], BF16)
                wxb, t_wxb = sbt(s2, "wxb", [128, 8, 128], BF16)
                dma_pool(wab[:], wab_d[l], [], [t_wab])
                dma_pool(wxb[:], wxb_d[l], [], [t_wxb])
                uxp, t_uxp = sbt(s2, "uxp", [128, S + 3])
                u, t_u = sbt(s2, "u", [128, S])
                ub, t_ub = sbt(s2, "ub", [128, S], BF16)
                r_, t_r = sbt(s2, "r", [128, S])
                M_, t_M = sbt(s2, "M", [128, S])
                ig, t_ig = sbt(s2, "ig", [128, S])
                gel, t_gel = sbt(s2, "gel", [128, S])
                h_, t_h = sbt(s2, "hscan", [128, S])
                gtmp, t_gtmp = sbt(s2, "gtmp", [128, S])
                mtl, t_mtl = sbt(s2, "mtl", [128, S], BF16)
                sqacc, t_sq = sbt(s2, "sqacc", [128, S])
                pa = [pst(s2, "pb%d" % i, [128, 512]) for i in range(4)]
                pg = [pst(s2, "pg%d" % i, [128, 512]) for i in range(3)]
                pss, t_pss = pst(s2, "pss", [128, NT])
                DVE(lambda: nc.vector.memset(uxp[:, 0:3], 0.0), [], [t_uxp])
                ia = 0
                igc = 0
                wnext = wload([(0, 1280, 128), (128, 2304, 128)])
                for c in range(8):
                    wt, t_wt = wnext
                    if c + 1 < 8:
                        wnext = wload([(0, 1280 + (c + 1) * 128, 128), (128, 2304 + (c + 1) * 128, 128)])
                    for tb in range(4):
                        p_, t_p = pa[ia % 4]
                        ia += 1
                        for kc in range(KC):
                            PE(lambda kc=kc, p_=p_, tb=tb, wt=wt: nc.tensor.matmul(
                                p_[:], lhsT=wt[:, kc, 0:128], rhs=xT[:, kc, tb * 512:(tb + 1) * 512],
                                start=(kc == 0), stop=(kc == KC - 1)), [t_wt, t_xT], [t_p])
                        ACT(lambda p_=p_, tb=tb, c=c: nc.scalar.activation(
                            out=uxp[:, 3 + tb * 512:3 + (tb + 1) * 512], in_=p_[:], func=AF.Identity,
                            bias=colsA[:, CA_BUX + c:CA_BUX + c + 1]), [t_p, t_colsA], [t_uxp])
                    for tb in range(4):
                        p_, t_p = pa[ia % 4]
                        ia += 1
                        for kc in range(KC):
                            PE(lambda kc=kc, p_=p_, tb=tb, wt=wt: nc.tensor.matmul(
                                p_[:], lhsT=wt[:, kc, 128:256], rhs=xT[:, kc, tb * 512:(tb + 1) * 512],
                                start=(kc == 0), stop=(kc == KC - 1)), [t_wt, t_xT], [t_p])
                        ACT(lambda p_=p_, tb=tb, c=c: nc.scalar.activation(
                            out=gel[:, tb * 512:(tb + 1) * 512], in_=p_[:], func=AF.Identity,
                            bias=colsA[:, CA_BUG + c:CA_BUG + c + 1]), [t_p, t_colsA], [t_gel])
                    cw = lambda k_, c=c: colsA[:, CA_CW + k_ * 8 + c:CA_CW + k_ * 8 + c + 1]
                    DVE(lambda c=c, cw=cw: nc.vector.tensor_scalar(
                        out=u[:], in0=uxp[:, 3:S + 3], scalar1=cw(3), scalar2=colsA[:, CA_CB + c:CA_CB + c + 1],
                        op0=ALU.mult, op1=ALU.add), [t_uxp, t_colsA], [t_u])
                    for k_ in (2, 1, 0):
                        DVE(lambda k_=k_, cw=cw: nc.vector.scalar_tensor_tensor(
                            out=u[:], in0=uxp[:, k_:k_ + S], scalar=cw(k_), in1=u[:], op0=ALU.mult, op1=ALU.add),
                            [t_uxp, t_colsA, t_u], [t_u])
                    DVE(lambda: nc.vector.tensor_copy(out=ub[:], in_=u[:]), [t_u], [t_ub])
                    ACT(lambda: nc.scalar.activation(out=gtmp[:], in_=gel[:], func=AF.Square), [t_gel], [t_gtmp])
                    DVE(lambda: nc.vector.tensor_scalar(out=gtmp[:], in0=gtmp[:], scalar1=0.044715, scalar2=1.0,
                                                        op0=ALU.mult, op1=ALU.add), [t_gtmp], [t_gtmp])
                    DVE(lambda: nc.vector.tensor_tensor(out=gtmp[:], in0=gtmp[:], in1=gel[:], op=ALU.mult),
                        [t_gtmp, t_gel], [t_gtmp])
                    ACT(lambda: nc.scalar.activation(out=gtmp[:], in_=gtmp[:], func=AF.Sigmoid, scale=1.5957691216),
                        [t_gtmp], [t_gtmp])
                    DVE(lambda: nc.vector.tensor_tensor(out=gel[:], in0=gel[:], in1=gtmp[:], op=ALU.mult),
                        [t_gel, t_gtmp], [t_gel])
                    for (wb, t_wb, dst, t_dst, bc) in ((wab, t_wab, r_, t_r, CA_BA), (wxb, t_wxb, ig, t_ig, CA_BXG)):
                        for tb in range(4):
                            p_, t_p = pg[igc % 3]
                            igc += 1
                            PE(lambda p_=p_, wb=wb, tb=tb, c=c: nc.tensor.matmul(
                                p_[:], lhsT=wb[:, c, :], rhs=ub[:, tb * 512:(tb + 1) * 512], start=True, stop=True),
                               [t_wb, t_ub], [t_p])
                            ACT(lambda p_=p_, dst=dst, tb=tb, bc=bc, c=c: nc.scalar.activation(
                                out=dst[:, tb * 512:(tb + 1) * 512], in_=p_[:], func=AF.Sigmoid,
                                bias=colsA[:, bc + c:bc + c + 1]), [t_p, t_colsA], [t_dst])
                    ACT(lambda c=c: nc.scalar.activation(out=M_[:], in_=r_[:], func=AF.Exp,
                                                         scale=c12[:, 8 + c:9 + c]), [t_r, t_c12], [t_M])
                    ACT(lambda c=c: nc.scalar.activation(out=r_[:], in_=r_[:], func=AF.Exp,
                                                         scale=c12[:, c:c + 1]), [t_r, t_c12], [t_r])
                    ACT(lambda: nc.scalar.activation(out=M_[:], in_=M_[:], func=AF.Sqrt, scale=-1.0, bias=1.0),
                        [t_M], [t_M])
                    DVE(lambda: nc.vector.tensor_tensor(out=ig[:], in0=ig[:], in1=u[:], op=ALU.mult),
                        [t_ig, t_u], [t_ig])
                    DVE(lambda: nc.vector.tensor_tensor(out=ig[:], in0=ig[:], in1=M_[:], op=ALU.mult),
                        [t_ig, t_M], [t_ig])
                    DVE(lambda: nc.vector.tensor_tensor_scan(out=h_[:], data0=r_[:], data1=ig[:], initial=0.0,
                                                             op0=ALU.mult, op1=ALU.add), [t_r, t_ig], [t_h])
                    DVE(lambda: nc.vector.tensor_tensor(out=gel[:], in0=gel[:], in1=h_[:], op=ALU.mult),
                        [t_gel, t_h], [t_gel])
                    ACT(lambda c=c: nc.scalar.activation(out=mtl[:], in_=gel[:], func=AF.Identity,
                                                         scale=colsA[:, CA_GL + c:CA_GL + c + 1]),
                        [t_gel, t_colsA], [t_mtl])
                    dma_sp(mT_d[8 + c], mtl[:], [t_mtl], [DT("mT")])
                    if c == 0:
                        ACT(lambda: nc.scalar.activation(out=sqacc[:], in_=gel[:], func=AF.Square), [t_gel], [t_sq])
                    else:
                        ACT(lambda: nc.scalar.activation(out=gtmp[:], in_=gel[:], func=AF.Square),
                            [t_gel], [t_gtmp])
                        POOL(lambda: nc.gpsimd.tensor_tensor(out=sqacc[:], in0=sqacc[:], in1=gtmp[:], op=ALU.add),
                             [t_sq, t_gtmp], [t_sq])
                for tt in range(NT):
                    PE(lambda tt=tt: nc.tensor.matmul(pss[:, tt:tt + 1], lhsT=sqacc[:, tt * 128:(tt + 1) * 128],
                                                      rhs=onesf[:, 0:1], start=True, stop=True),
                       [t_sq, t_onesf], [t_pss])
                ACT(lambda: nc.scalar.activation(out=rstdl[:], in_=pss[:], func=AF.Sqrt, scale=1.0 / 1024,
                                                 bias=RMS_EPS), [t_pss], [t_rstdl])
                DVE(lambda: nc.vector.reciprocal(out=rstdl[:], in_=rstdl[:]), [t_rstdl], [t_rstdl])
                S_.barrier()
        S_.barrier()

        with contextlib.ExitStack() as sbk:
            qT, t_qT = sbt(sbk, "qT", [128, 9, S], BF16)
            Vt, t_V = sbt(sbk, "VtB", [128, NT * 2 * 65], BF16)
            mprev, t_mprev = sbt(sbk, "mprev", [128, 1024], BF16)
            mcur, t_mcur = sbt(sbk, "mcur", [128, 1024], BF16)
            mTa, t_mTa = sbt(sbk, "mTa", [128, 8, S], BF16)
            dma_sp(qT[:], qT_d.rearrange("c p t -> p c t"), [DT("qT")], [t_qT])
            dma_sp(Vt[:], V_d, [DT("V")], [t_V])
            dma_sp(mprev[:], c_mprev, [], [t_mprev])
            dma_sp(mcur[:], c_mcur, [], [t_mcur])
            E = [sbt(sbk, "E%d" % i, [128, 1024], BF16) for i in range(8)]
            at = [sbt(sbk, "at%d" % i, [128, 1024]) for i in range(2)]
            atb = [sbt(sbk, "atb%d" % i, [128, 1024], BF16) for i in range(2)]
            small = [sbt(sbk, "sm%d" % i, [128, 32]) for i in range(2)]
            psc = [pst(sbk, "psc%d" % i, [128, 512]) for i in range(4)]
            po = [pst(sbk, "po%d" % i, [128, 4, 65]) for i in range(2)]
            ptr, t_ptr = pst(sbk, "ptr", [128, 8, 128], BF16)
            cB = {"ie": 0, "isc": 0}
            EsAll = {}

            def stS(qb):
                kbs = ([qb - 1] if qb > 0 else []) + [qb]
                for hk in range(2):
                    Es = []
                    for kb in kbs:
                        E_, t_E = E[cB["ie"] % 8]
                        cB["ie"] += 1
                        for half in range(2):
                            p_, t_p = psc[cB["isc"] % 4]
                            cB["isc"] += 1
                            PE(lambda p_=p_, hk=hk, kb=kb, half=half, qb=qb: nc.tensor.matmul(
                                p_[:], lhsT=qT[hk * 64:(hk + 1) * 64, 8, kb * 128:(kb + 1) * 128],
                                rhs=qT[hk * 64:(hk + 1) * 64, half * 4:(half + 1) * 4, qb * 128:(qb + 1) * 128],
                                start=True, stop=True), [t_qT], [t_p])
                            ACT(lambda p_=p_, E_=E_, half=half: nc.scalar.activation(
                                out=E_[:, half * 512:(half + 1) * 512], in_=p_[:], func=AF.Exp, scale=0.125),
                                [t_p], [t_E])
                        msk, t_msk = (mcur, t_mcur) if kb == qb else (mprev, t_mprev)
                        POOL(lambda E_=E_, msk=msk: nc.gpsimd.tensor_tensor(out=E_[:], in0=E_[:], in1=msk[:],
                                                                            op=ALU.mult), [t_E, t_msk], [t_E])
                        Es.append((E_, t_E, kb))
                    EsAll[(qb, hk)] = Es

            def stP(qb):
                at_, t_at = at[qb % 2]
                atb_, t_atb = atb[qb % 2]
                sm, t_sm = small[qb % 2]
                for hk in range(2):
                    Es = EsAll.pop((qb, hk))
                    for hh in range(2):
                        o_, t_o = po[hh]
                        for g4 in range(4):
                            g = hh * 4 + g4
                            for i_, (E_, t_E, kb) in enumerate(Es):
                                v0 = (kb * 2 + hk) * 65
                                PE(lambda o_=o_, g4=g4, g=g, E_=E_, v0=v0, i_=i_, n=len(Es): nc.tensor.matmul(
                                    o_[:, g4, :], lhsT=E_[:, g * 128:(g + 1) * 128], rhs=Vt[:, v0:v0 + 65],
                                    start=(i_ == 0), stop=(i_ == n - 1)), [t_E, t_V], [t_o])
                        h0 = hk * 8 + hh * 4
                        DVE(lambda o_=o_, sm=sm, h0=h0: nc.vector.tensor_tensor(
                            out=sm[:, h0:h0 + 4], in0=o_[:, :, 64], in1=exps[:, h0:h0 + 4], op=ALU.add),
                            [t_o, t_exps], [t_sm])
                        DVE(lambda sm=sm, h0=h0: nc.vector.reciprocal(out=sm[:, h0:h0 + 4], in_=sm[:, h0:h0 + 4]),
                            [t_sm], [t_sm])
                        for g4 in range(4):
                            h = h0 + g4
                            DVE(lambda o_=o_, g4=g4, h=h, at_=at_, sm=sm: nc.vector.tensor_scalar(
                                out=at_[:, h * 64:(h + 1) * 64], in0=o_[:, g4, 0:64], scalar1=sm[:, h:h + 1],
                                scalar2=None, op0=ALU.mult), [t_o, t_sm], [t_at])
                ACT(lambda atb_=atb_, at_=at_, sm=sm: nc.scalar.activation(
                    out=atb_[:], in_=at_[:], func=AF.Square, accum_out=sm[:, 16:17]), [t_at], [t_atb, t_sm])
                ACT(lambda sm=sm: nc.scalar.activation(out=sm[:, 17:18], in_=sm[:, 16:17], func=AF.Sqrt,
                                                       scale=1.0 / 1024, bias=RMS_EPS), [t_sm], [t_sm])
                DVE(lambda sm=sm: nc.vector.reciprocal(out=sm[:, 17:18], in_=sm[:, 17:18]), [t_sm], [t_sm])
                DVE(lambda atb_=atb_, at_=at_, sm=sm: nc.vector.tensor_scalar(
                    out=atb_[:], in0=at_[:], scalar1=sm[:, 17:18], scalar2=None, op0=ALU.mult),
                    [t_at, t_sm, t_atb], [t_atb])

            def stT(qb):
                atb_, t_atb = atb[qb % 2]
                for c in range(8):
                    PE(lambda c=c, atb_=atb_: nc.tensor.transpose(out=ptr[:, c, :], in_=atb_[:, c * 128:(c + 1) * 128],
                                                                  identity=identb[:]), [t_atb, t_identb], [t_ptr])
                for c in range(8):
                    if c % 2 == 0:
                        ACT(lambda c=c, qb=qb: nc.scalar.activation(
                            out=mTa[:, c, qb * 128:(qb + 1) * 128], in_=ptr[:, c, :], func=AF.Identity,
                            scale=colsA[:, CA_GA + c:CA_GA + c + 1]), [t_ptr, t_colsA], [t_mTa])
                    else:
                        DVE(lambda c=c, qb=qb: nc.vector.tensor_scalar(
                            out=mTa[:, c, qb * 128:(qb + 1) * 128], in0=ptr[:, c, :],
                            scalar1=colsA[:, CA_GA + c:CA_GA + c + 1], scalar2=None, op0=ALU.mult),
                            [t_ptr, t_colsA], [t_mTa])

            stS(0)
            for qb in range(NT):
                if qb + 1 < NT:
                    stS(qb + 1)
                stP(qb)
                if qb > 0:
                    stT(qb - 1)
            stT(NT - 1)
            dma_sp(mT_d[0:8].rearrange("c p t -> p c t"), mTa[:], [t_mTa], [DT("mT")])
            S_.barrier()

        with contextlib.ExitStack() as sc:
            wo, t_wo = sbt(sc, "wo", [128, KC, D], BF16)
            wsrc = w_out[l].rearrange("(kc kp) n -> kp kc n", kp=128)
            for nb in range(4):
                dma_pool(wo[:, :, nb * 512:(nb + 1) * 512], wsrc[:, :, nb * 512:(nb + 1) * 512], [], [t_wo])
            wr32, t_wr = sbt(sc, "wr32", [128, KC, NE])
            dma_sp(wr32[:], w_r[l].rearrange("(kc kp) e -> kp kc e", kp=128), [], [t_wr])
            rows, t_rows = sbt(sc, "rowsC", [128, 3, D])
            dma_sp(rows[:], rowsBig_d[l][:, 0:3, :], [], [t_rows])
            mTr = [sbt(sc, "mTr%d" % i, [128, KC, 512], BF16) for i in range(2)]
            xin = [sbt(sc, "xinC%d" % i, [128, D]) for i in range(2)]
            res = [sbt(sc, "resC%d" % i, [128, D]) for i in range(2)]
            x1b = [sbt(sc, "x1b%d" % i, [128, D], BF16) for i in range(2)]
            x1T, t_x1T = sbt(sc, "x1T", [128, KC, 128])
            lg, t_lg = sbt(sc, "lg", [128, NE])
            maskb, t_maskb = sbt(sc, "maskb", [128, NT, NE], BF16)
            idxf, t_idxf = sbt(sc, "idxf", [128, NT, 4])
            st_small = {"stats": sbt(sc, "stats", [128, 4, 6]), "mv": sbt(sc, "mv", [128, 4])}
            top8, t_top8 = sbt(sc, "top8", [128, 8])
            idx8, t_idx8 = sbt(sc, "idx8", [128, 8], U32)
            sm, t_sm = sbt(sc, "smC", [128, 16])
            G, t_G = sbt(sc, "G", [128, NE])
            oh, t_oh = sbt(sc, "oh", [128, NE])
            posC, t_posC = sbt(sc, "posC", [128, NE])
            slotf, t_slotf = sbt(sc, "slotf", [128, NT, 4])
            pya = [pst(sc, "pya%d" % i, [128, 512]) for i in range(2)]
            pyl = [pst(sc, "pyl%d" % i, [128, 512]) for i in range(2)]
            ptp = [pst(sc, "ptpC%d" % i, [128, 4, 128]) for i in range(2)]
            plg_all, t_plg = pst(sc, "plg", [128, 96])
            plg = plg_all[:, 0:32]
            ppos = plg_all[:, 32:96]
            t_ppos = t_plg
            runc, t_runc = sbt(sc, "runc", [128, NE])
            DVE(lambda: nc.vector.memset(runc[:], 0.0), [], [t_runc])
            pgt, t_pgt = pst(sc, "pgt", [32, 128])
            cc = {"iy": 0, "itp": 0, "mt": None}

            def stage1(tt):
                if tt % 4 == 0:
                    cc["mt"] = mTr[(tt // 4) % 2]
                    dma_sp(cc["mt"][0][:], mT_d.rearrange("c p t -> p c t")[:, :, tt * 128:tt * 128 + 512],
                           [DT("mT")], [cc["mt"][1]])
                mt, t_mt = cc["mt"]
                xi, t_xi = xin[tt % 2]
                rs, t_rs = res[tt % 2]
                xb_, t_xb = x1b[tt % 2]
                dma_sp(xi[:], xsrc[tt * 128:(tt + 1) * 128, :], [t_xsrc], [t_xi])
                DVE(lambda xi=xi: nc.vector.scalar_tensor_tensor(out=xi[:], in0=xi[:], scalar=ALPHA, in1=rows[:, 0, :],
                                                                 op0=ALU.mult, op1=ALU.add), [t_xi, t_rows], [t_xi])
                tl = (tt % 4) * 128
                for nb in range(4):
                    a_, t_a = pya[cc["iy"] % 2]
                    l_, t_l = pyl[cc["iy"] % 2]
                    cc["iy"] += 1
                    for kc in range(8):
                        PE(lambda kc=kc, a_=a_, mt=mt, tl=tl, nb=nb: nc.tensor.matmul(
                            a_[:], lhsT=mt[:, kc, tl:tl + 128], rhs=wo[:, kc, nb * 512:(nb + 1) * 512],
                            start=(kc == 0), stop=(kc == 7)), [t_mt, t_wo], [t_a])
                    for kc in range(8, 16):
                        PE(lambda kc=kc, l_=l_, mt=mt, tl=tl, nb=nb: nc.tensor.matmul(
                            l_[:], lhsT=mt[:, kc, tl:tl + 128], rhs=wo[:, kc, nb * 512:(nb + 1) * 512],
                            start=(kc == 8), stop=(kc == 15)), [t_mt, t_wo], [t_l])
                    sl = slice(nb * 512, (nb + 1) * 512)
                    DVE(lambda a_=a_, rs=rs, xi=xi, sl=sl: nc.vector.tensor_tensor(
                        out=rs[:, sl], in0=a_[:], in1=xi[:, sl], op=ALU.add), [t_a, t_xi], [t_rs])
                    DVE(lambda l_=l_, rs=rs, sl=sl, tt=tt: nc.vector.scalar_tensor_tensor(
                        out=rs[:, sl], in0=l_[:], scalar=rstdl[:, tt:tt + 1], in1=rs[:, sl],
                        op0=ALU.mult, op1=ALU.add), [t_l, t_rstdl, t_rs], [t_rs])

            def stage2(tt):
                rs, t_rs = res[tt % 2]
                xb_, t_xb = x1b[tt % 2]
                ln_tile(st_small, rs, t_rs, rows[:, 1, :], rows[:, 2, :], t_rows, "c")
                dma_sp(x1_d[tt * 128:(tt + 1) * 128, :], rs[:], [t_rs], [DT("x1")])
                ACT(lambda xb_=xb_, rs=rs: nc.scalar.copy(out=xb_[:], in_=rs[:]), [t_rs], [t_xb])
                dma_sp(x1b_d[tt * 128:(tt + 1) * 128, :], xb_[:], [t_xb], [DT("x1b")])

            def stage2b(tt):
                rs, t_rs = res[tt % 2]
                for g in range(4):
                    p_, t_p = ptp[cc["itp"] % 2]
                    cc["itp"] += 1
                    for j in range(4):
                        kc = g * 4 + j
                        PE(lambda kc=kc, j=j, p_=p_, rs=rs: nc.tensor.transpose(
                            out=p_[:, j, :], in_=rs[:, kc * 128:(kc + 1) * 128], identity=identf[:]),
                           [t_rs, t_identf], [t_p])
                    if g % 2 == 0:
                        ACT(lambda p_=p_, g=g: nc.scalar.copy(out=x1T[:, g * 4:(g + 1) * 4, :], in_=p_[:]),
                            [t_p], [t_x1T])
                    else:
                        DVE(lambda p_=p_, g=g: nc.vector.tensor_copy(out=x1T[:, g * 4:(g + 1) * 4, :], in_=p_[:]),
                            [t_p], [t_x1T])
                for kc in range(KC):
                    PE(lambda kc=kc: nc.tensor.matmul(plg, lhsT=x1T[:, kc, :], rhs=wr32[:, kc, :],
                                                      start=(kc == 0), stop=(kc == KC - 1)),
                       [t_x1T, t_wr], [t_plg])
                DVE(lambda: nc.vector.tensor_tensor(out=lg[:], in0=plg, in1=rowsB[:, RB_BR:RB_BR + NE],
                                                    op=ALU.add), [t_plg, t_rowsB], [t_lg])
                DVE(lambda: nc.vector.max(out=top8[:], in_=lg[:]), [t_lg], [t_top8])
                DVE(lambda: nc.vector.max_index(out=idx8[:], in_max=top8[:], in_values=lg[:]),
                    [t_top8, t_lg], [t_idx8])
                DVE(lambda: nc.vector.tensor_scalar(out=sm[:, 0:1], in0=top8[:, 0:1], scalar1=-1.0, scalar2=None,
                                                    op0=ALU.mult), [t_top8], [t_sm])
                ACT(lambda: nc.scalar.activation(out=sm[:, 4:8], in_=top8[:, 0:4], func=AF.Exp, bias=sm[:, 0:1],
                                                 accum_out=sm[:, 1:2]), [t_top8, t_sm], [t_sm])
                DVE(lambda: nc.vector.reciprocal(out=sm[:, 2:3], in_=sm[:, 1:2]), [t_sm], [t_sm])
                DVE(lambda tt=tt: nc.vector.tensor_scalar(out=gates[:, tt, :], in0=sm[:, 4:8], scalar1=sm[:, 2:3],
                                                          scalar2=None, op0=ALU.mult), [t_sm], [t_gates])
                DVE(lambda tt=tt: nc.vector.tensor_scalar(out=maskb[:, tt, :], in0=lg[:], scalar1=top8[:, 3:4],
                                                          scalar2=None, op0=ALU.is_ge), [t_lg, t_top8], [t_maskb])
                DVE(lambda tt=tt: nc.vector.tensor_copy(out=idxf[:, tt, :], in_=idx8[:, 0:4]), [t_idx8], [t_idxf])
                for k_ in range(4):
                    dst, t_dst = (G, t_G) if k_ == 0 else (oh, t_oh)
                    DVE(lambda k_=k_, tt=tt, dst=dst: nc.vector.tensor_scalar(
                        out=dst[:], in0=iota[:, 0:32], scalar1=idxf[:, tt, k_:k_ + 1],
                        scalar2=gates[:, tt, k_:k_ + 1], op0=ALU.is_equal, op1=ALU.mult),
                        [t_iota, t_idxf, t_gates], [t_dst])
                    if k_ > 0:
                        DVE(lambda: nc.vector.tensor_tensor(out=G[:], in0=G[:], in1=oh[:], op=ALU.add),
                            [t_G, t_oh], [t_G])
                PE(lambda: nc.tensor.transpose(out=pgt[:], in_=G[:], identity=identf[:]), [t_G, t_identf], [t_pgt])
                ACT(lambda tt=tt: nc.scalar.copy(out=GT[:, tt, :], in_=pgt[:]), [t_pgt], [t_GT])
                PE(lambda tt=tt: nc.tensor.matmul(ppos[:, 0:32], lhsT=triu[:], rhs=maskb[:, tt, :], start=True,
                                                  stop=True), [t_triu, t_maskb], [t_ppos])
                PE(lambda tt=tt: nc.tensor.matmul(ppos[:, 32:64], lhsT=onesb[:], rhs=maskb[:, tt, :], start=True,
                                                  stop=True), [t_onesb, t_maskb], [t_ppos])
                DVE(lambda: nc.vector.tensor_tensor(out=posC[:], in0=ppos[:, 0:32], in1=runc[:], op=ALU.add),
                    [t_ppos, t_runc], [t_posC])
                DVE(lambda: nc.vector.scalar_tensor_tensor(out=posC[:], in0=posC[:], scalar=float(CAP - 1),
                                                           in1=iota[:, 32:64], op0=ALU.min, op1=ALU.add),
                    [t_posC, t_iota], [t_posC])
                DVE(lambda: nc.vector.tensor_tensor(out=runc[:], in0=runc[:], in1=ppos[:, 32:64], op=ALU.add),
                    [t_ppos, t_runc], [t_runc])
                for k_ in range(4):
                    DVE(lambda k_=k_, tt=tt: nc.vector.scalar_tensor_tensor(
                        out=oh[:], in0=iota[:, 0:32], scalar=idxf[:, tt, k_:k_ + 1], in1=posC[:],
                        op0=ALU.is_equal, op1=ALU.mult), [t_iota, t_idxf, t_posC], [t_oh])
                    DVE(lambda k_=k_, tt=tt: nc.vector.reduce_sum(out=slotf[:, tt, k_:k_ + 1], in_=oh[:], axis=AX.X),
                        [t_oh], [t_slotf])
                DVE(lambda tt=tt: nc.vector.tensor_copy(out=sloti[:, tt, :], in_=slotf[:, tt, :]),
                    [t_slotf], [t_slotis[tt]])
                for k_ in range(4):
                    S_.dma("pool", lambda tt=tt, k_=k_: nc.gpsimd.indirect_dma_start(
                        out=stok_d[:, :], out_offset=bass.IndirectOffsetOnAxis(ap=sloti[:, tt, k_:k_ + 1], axis=0),
                        in_=tokid[:, tt:tt + 1], in_offset=None), [t_slotis[tt], t_tokid], [DT("stok")])

            stage1(0)
            for tt in range(NT):
                stage2(tt)
                if tt + 1 < NT:
                    stage1(tt + 1)
                stage2b(tt)
            S_.barrier()

        with contextlib.ExitStack() as sd:
            bupT, t_bup = sbt(sd, "bupT", [128, NE * 16])
            dma_sp(bupT[:], bupT_d[l], [], [t_bup])
            sidx = [sbt(sd, "sidx%d" % i, [128, 3], I32) for i in range(2)]
            xs = [[sbt(sd, "xs%d_%d" % (i, j), [128, D], BF16) for j in range(3)] for i in range(2)]
            xsT = [sbt(sd, "xsT%d" % i, [128, KC, CAP], BF16) for i in range(2)]
            NWU = 4
            NWD = 4
            wu = [sbt(sd, "wu%d" % i, [128, KC, 512], BF16) for i in range(NWU)]
            wd = [sbt(sd, "wd%d" % i, [128, 8, 512], BF16) for i in range(NWD)]
            gs = [sbt(sd, "gs%d" % i, [128, 4, CAP]) for i in range(2)]
            sg = [sbt(sd, "sg%d" % i, [128, CAP]) for i in range(2)]
            ln_ = [sbt(sd, "ln%d" % i, [128, CAP]) for i in range(2)]
            hT = [sbt(sd, "hT%d" % i, [128, 8, CAP], BF16) for i in range(1)]
            yo = [sbt(sd, "yo%d" % i, [128, 512]) for i in range(4)]
            ptr_ = [pst(sd, "ptrD%d" % i, [128, 8, 128], BF16) for i in range(2)]
            pu = [pst(sd, "pu%d" % i, [128, CAP]) for i in range(3)]
            pd = [pst(sd, "pd%d" % i, [128, 512]) for i in range(3)]
            cnt = {"wu": 0, "wd": 0, "tr": 0, "pu": 0, "pd": 0, "gs": 0, "sg": 0, "yo": 0}

            def gather(e):
                si, t_si = sidx[e % 2]
                dma_sp(si[:], stok_d[e * CAP:(e + 1) * CAP, :].rearrange("(p j) o -> p (j o)", p=128),
                       [DT("stok")], [t_si])
                for j in range(3):
                    xs_, t_xs = xs[e % 2][j]
                    S_.dma("pool", lambda xs_=xs_, si=si, j=j: nc.gpsimd.indirect_dma_start(
                        out=xs_[:, :], out_offset=None, in_=x1b_d[:, :],
                        in_offset=bass.IndirectOffsetOnAxis(ap=si[:, j:j + 1], axis=0)),
                        [t_si, DT("x1b")], [t_xs])

            def transp(e):
                xsT_, t_xsT = xsT[e % 2]
                for j in range(3):
                    xs_, t_xs = xs[e % 2][j]
                    for g in range(2):
                        p_, t_p = ptr_[cnt["tr"] % 2]
                        cnt["tr"] += 1
                        for c in range(8):
                            kc = g * 8 + c
                            PE(lambda p_=p_, c=c, kc=kc, xs_=xs_: nc.tensor.transpose(
                                out=p_[:, c, :], in_=xs_[:, kc * 128:(kc + 1) * 128], identity=identb[:]),
                               [t_xs, t_identb], [t_p])
                        dst = xsT_[:, g * 8:(g + 1) * 8, j * 128:(j + 1) * 128]
                        if cnt["tr"] % 2 == 0:
                            ACT(lambda p_=p_, dst=dst: nc.scalar.copy(out=dst, in_=p_[:]), [t_p], [t_xsT])
                        else:
                            DVE(lambda p_=p_, dst=dst: nc.vector.tensor_copy(out=dst, in_=p_[:]), [t_p], [t_xsT])

            gather(0)
            transp(0)
            for e in range(NE):
                xsT_, t_xsT = xsT[e % 2]
                hT_, t_hT = hT[0]
                if e + 1 < NE:
                    gather(e + 1)
                usrc = w_up[l, e].rearrange("(kc kp) n -> kp kc n", kp=128)
                for hf in range(2):
                    gs_, t_gs = gs[cnt["gs"] % 2]
                    cnt["gs"] += 1
                    for part in range(2):
                        w_, t_w = wu[cnt["wu"] % NWU]
                        cnt["wu"] += 1
                        c0 = part * 1024 + hf * 512
                        dma_pool(w_[:], usrc[:, :, c0:c0 + 512], [], [t_w])
                        for j4 in range(4):
                            fc = hf * 4 + j4
                            p_, t_p = pu[cnt["pu"] % 3]
                            cnt["pu"] += 1
                            for kc in range(KC):
                                PE(lambda p_=p_, w_=w_, j4=j4, kc=kc, xsT_=xsT_: nc.tensor.matmul(
                                    p_[:], lhsT=w_[:, kc, j4 * 128:(j4 + 1) * 128], rhs=xsT_[:, kc, :],
                                    start=(kc == 0), stop=(kc == KC - 1)), [t_w, t_xsT], [t_p])
                            bcol = bupT[:, e * 16 + part * 8 + fc:e * 16 + part * 8 + fc + 1]
                            if part == 0:
                                sg_, t_sg = sg[cnt["sg"] % 2]
                                cnt["sg"] += 1
                                DVE(lambda p_=p_, gs_=gs_, j4=j4, bcol=bcol: nc.vector.tensor_scalar(
                                    out=gs_[:, j4, :], in0=p_[:], scalar1=bcol, scalar2=7.0, op0=ALU.add,
                                    op1=ALU.min), [t_p, t_bup], [t_gs])
                                ACT(lambda sg_=sg_, gs_=gs_, j4=j4: nc.scalar.activation(
                                    out=sg_[:], in_=gs_[:, j4, :], func=AF.Sigmoid, scale=1.702), [t_gs], [t_sg])
                                DVE(lambda sg_=sg_, gs_=gs_, j4=j4: nc.vector.tensor_tensor(
                                    out=gs_[:, j4, :], in0=gs_[:, j4, :], in1=sg_[:], op=ALU.mult),
                                    [t_gs, t_sg], [t_gs])
                            else:
                                l_, t_l = ln_[cnt["sg"] % 2]
                                cnt["sg"] += 1
                                DVE(lambda p_=p_, l_=l_, bcol=bcol: nc.vector.tensor_scalar(
                                    out=l_[:], in0=p_[:], scalar1=bcol, scalar2=7.0, op0=ALU.add, op1=ALU.min),
                                    [t_p, t_bup], [t_l])
                                DVE(lambda l_=l_: nc.vector.tensor_scalar(
                                    out=l_[:], in0=l_[:], scalar1=-7.0, scalar2=1.0, op0=ALU.max, op1=ALU.add),
                                    [t_l], [t_l])
                                DVE(lambda l_=l_, gs_=gs_, j4=j4, fc=fc, hT_=hT_: nc.vector.tensor_tensor(
                                    out=hT_[:, fc, :], in0=l_[:], in1=gs_[:, j4, :], op=ALU.mult),
                                    [t_l, t_gs], [t_hT])
                if e + 1 < NE:
                    transp(e + 1)
                dsrc = w_dn[l, e].rearrange("(fc fp) n -> fp fc n", fp=128)
                for nb in range(4):
                    w_, t_w = wd[cnt["wd"] % NWD]
                    cnt["wd"] += 1
                    dma_pool(w_[:], dsrc[:, :, nb * 512:(nb + 1) * 512], [], [t_w])
                    for j in range(3):
                        p_, t_p = pd[cnt["pd"] % 3]
                        cnt["pd"] += 1
                        for fc in range(8):
                            PE(lambda p_=p_, fc=fc, j=j, w_=w_, hT_=hT_: nc.tensor.matmul(
                                p_[:], lhsT=hT_[:, fc, j * 128:(j + 1) * 128], rhs=w_[:, fc, :],
                                start=(fc == 0), stop=(fc == 7)), [t_hT, t_w], [t_p])
                        yt, t_yt = yo[cnt["yo"] % 4]
                        cnt["yo"] += 1
                        if cnt["yo"] % 2 == 0:
                            ACT(lambda p_=p_, yt=yt: nc.scalar.copy(out=yt[:], in_=p_[:]), [t_p], [t_yt])
                        else:
                            DVE(lambda p_=p_, yt=yt: nc.vector.tensor_copy(out=yt[:], in_=p_[:]), [t_p], [t_yt])
                        dma_sp(ys_d[e * CAP:(e + 1) * CAP, :].rearrange("(p j) n -> p j n", j=3)[:, j, nb * 512:(nb + 1) * 512], yt[:],
                               [t_yt], [DT("ys")])
            S_.barrier()

        with contextlib.ExitStack() as se:
            rows, t_rows = sbt(se, "rowsE", [128, 2, D])
            dma_sp(rows[:], rowsBig_d[l][:, 3:5, :], [], [t_rows])
            bd, t_bd = sbt(se, "bd", [32, D])
            dma_sp(bd[:], b_dn[l], [], [t_bd])
            yg = [[sbt(se, "yg%d_%d" % (i, k_), [128, D]) for k_ in range(4)] for i in range(2)]
            xi2 = [sbt(se, "xi2%d" % i, [128, D]) for i in range(2)]
            st_small = {"stats": sbt(se, "statsE", [128, 4, 6]), "mv": sbt(se, "mvE", [128, 4])}
            pb = [pst(se, "pbE%d" % i, [128, 512]) for i in range(4)]
            ipb = 0
            dst_d = out_d if last else xa_d
            t_dst = DT("out") if last else DT("xa")
            def fetchE(tt):
                xi, t_xi = xi2[tt % 2]
                dma_sp(xi[:], x1_d[tt * 128:(tt + 1) * 128, :], [DT("x1")], [t_xi])
                for k_ in range(4):
                    y_, t_y = yg[tt % 2][k_]
                    S_.dma("pool", lambda y_=y_, tt=tt, k_=k_: nc.gpsimd.indirect_dma_start(
                        out=y_[:, :], out_offset=None, in_=ys_d[:, :],
                        in_offset=bass.IndirectOffsetOnAxis(ap=sloti[:, tt, k_:k_ + 1], axis=0)),
                        [t_slotis[tt], DT("ys")], [t_y])

            fetchE(0)
            for tt in range(NT):
                xi, t_xi = xi2[tt % 2]
                if tt + 1 < NT:
                    fetchE(tt + 1)
                for nb in range(4):
                    p_, t_p = pb[ipb % 4]
                    ipb += 1
                    PE(lambda p_=p_, tt=tt, nb=nb: nc.tensor.matmul(p_[:], lhsT=GT[:, tt, :],
                                                                    rhs=bd[:, nb * 512:(nb + 1) * 512],
                                                                    start=True, stop=True), [t_GT, t_bd], [t_p])
                    sl = slice(nb * 512, (nb + 1) * 512)
                    DVE(lambda p_=p_, xi=xi, sl=sl: nc.vector.scalar_tensor_tensor(
                        out=xi[:, sl], in0=xi[:, sl], scalar=ALPHA, in1=p_[:], op0=ALU.mult, op1=ALU.add),
                        [t_xi, t_p], [t_xi])
                for k_ in range(4):
                    y_, t_y = yg[tt % 2][k_]
                    DVE(lambda y_=y_, xi=xi, tt=tt, k_=k_: nc.vector.scalar_tensor_tensor(
                        out=xi[:], in0=y_[:], scalar=gates[:, tt, k_:k_ + 1], in1=xi[:], op0=ALU.mult,
                        op1=ALU.add), [t_y, t_gates, t_xi], [t_xi])
                ln_tile(st_small, xi, t_xi, rows[:, 0, :], rows[:, 1, :], t_rows, "e")
                dma_sp(dst_d[tt * 128:(tt + 1) * 128, :], xi[:], [t_xi], [t_dst])
            S_.barrier()
    S_.barrier()
    es.close()
    return nc


def _consts():
    import ml_dtypes
    bf = ml_dtypes.bfloat16
    p = np.arange(128)
    d = p % 64
    inv_freq = (1.0 / (np.float32(10000.0) ** (np.arange(0, 64, 2, dtype=np.float32) / np.float32(64)))).astype(np.float32)
    ang = (np.arange(S, dtype=np.float32)[:, None] * inv_freq[None, :]).astype(np.float32)
    cos = np.cos(ang).astype(np.float32)
    sin = np.sin(ang).astype(np.float32)
    cosT = np.ascontiguousarray(cos[:, d % 32].T)
    sgn = np.where(d < 32, -1.0, 1.0).astype(np.float32)
    sinT = np.ascontiguousarray(sin[:, d % 32].T * sgn[:, None]).astype(np.float32)
    partner = 64 * (p // 64) + ((p % 64) + 32) % 64
    permR = np.zeros((128, 128), np.float32)
    permR[partner, p] = 1.0
    kk = np.arange(128)[:, None]
    qq = np.arange(128)[None, :]
    mprev = np.tile((kk > qq).astype(np.float32), (1, 8)).astype(bf)
    mcur = np.tile((kk <= qq).astype(np.float32), (1, 8)).astype(bf)
    triu = (kk < qq).astype(np.float32).astype(bf)
    iota = np.zeros((128, 64), np.float32)
    iota[:, 0:32] = np.arange(32)[None, :]
    iota[:, 32:64] = np.arange(32)[None, :] * CAP
    tokid = (np.arange(NT)[None, :] * 128 + p[:, None]).astype(np.int32)
    return {
        "c_identf": np.eye(128, dtype=np.float32), "c_identb": np.eye(128, dtype=np.float32).astype(bf),
        "c_cos": cosT, "c_sin": sinT, "c_permR": permR, "c_mprev": mprev, "c_mcur": mcur, "c_triu": triu,
        "c_onesb": np.ones((128, 128), np.float32).astype(bf), "c_iota": iota, "c_tokid": tokid,
        "c_zero": np.zeros((128, 96), np.int32),
    }


def _prep(inp, layers):
    ls = list(layers)
    f = lambda k: np.asarray(inp[k])
    pq = np.arange(1024)
    cq, pp = pq // 128, pq % 128
    perm_q = (np.where(pp < 64, cq, 8 + cq) * 64 + (pp % 64))
    perm = np.concatenate([perm_q, np.arange(1024, DIN)])
    w_in = np.ascontiguousarray(f("w_in")[ls][:, :, perm])
    b_in = f("b_in")[ls][:, perm]
    Ln = len(ls)
    colsA = np.zeros((Ln, 128, NA), np.float32)
    col = lambda v: v.reshape(Ln, -1, 128).transpose(0, 2, 1)
    colsA[:, :, CA_BQ:CA_BQ + 8] = col(b_in[:, 0:1024])
    colsA[:, :, CA_BK:CA_BK + 1] = col(b_in[:, 1024:1152])
    colsA[:, :, CA_BUX:CA_BUX + 8] = col(b_in[:, 1280:2304])
    colsA[:, :, CA_BUG:CA_BUG + 8] = col(b_in[:, 2304:3328])
    cw = f("conv_w")[ls]
    for k in range(4):
        colsA[:, :, CA_CW + k * 8:CA_CW + (k + 1) * 8] = col(cw[:, k, :])
    colsA[:, :, CA_CB:CA_CB + 8] = col(f("conv_b")[ls])
    colsA[:, :, CA_BA:CA_BA + 8] = col(f("lru_b_a")[ls])
    colsA[:, :, CA_BXG:CA_BXG + 8] = col(f("lru_b_x")[ls])
    colsA[:, :, CA_LAM:CA_LAM + 8] = col(f("lru_lambda")[ls])
    colsA[:, :, CA_GA:CA_GA + 8] = col(f("g_attn")[ls])
    colsA[:, :, CA_GL:CA_GL + 8] = col(f("g_lru")[ls])
    rowsB = np.zeros((Ln, 128, NB), np.float32)
    rowsB[:, :, RB_BV:RB_BV + 128] = b_in[:, None, 1152:1280]
    rowsB[:, :, RB_BR:RB_BR + 32] = f("b_router")[ls][:, None, :]
    rowsB[:, :, RB_SK:RB_SK + 16] = f("attn_sinks")[ls][:, None, :]
    rowsBig = np.zeros((Ln, 128, 5, D), np.float32)
    for i, k in enumerate(["b_out", "ln1_g", "ln1_b", "ln2_g", "ln2_b"]):
        rowsBig[:, :, i, :] = f(k)[ls][:, None, :]

    def bdiag(w):
        w = w[ls]
        o = np.zeros((Ln, 128, 8, 128), np.float32)
        for c in range(8):
            o[:, 0:64, c, 0:64] = w[:, 2 * c]
            o[:, 64:128, c, 64:128] = w[:, 2 * c + 1]
        return o
    bup = f("b_up")[ls]
    bupT = np.ascontiguousarray(bup.reshape(Ln, NE, 16, 128).transpose(0, 3, 1, 2).reshape(Ln, 128, NE * 16))
    m = {
        "w_in": w_in, "colsA": colsA, "rowsB": rowsB, "rowsBig": rowsBig,
        "wab": bdiag(f("lru_w_a")), "wxb": bdiag(f("lru_w_x")),
        "w_out": np.ascontiguousarray(f("w_out")[ls]), "w_router": np.ascontiguousarray(f("w_router")[ls]),
        "w_up": np.ascontiguousarray(f("w_up")[ls]), "bupT": bupT,
        "w_down": np.ascontiguousarray(f("w_down")[ls]), "b_down": np.ascontiguousarray(f("b_down")[ls]),
    }
    m.update(_consts())
    return m


_NC_CACHE = {}


def _get_nc(nl):
    if nl not in _NC_CACHE:
        _NC_CACHE[nl] = build(nl)
    return _NC_CACHE[nl]


FUSED = True


def kernel(**inputs):
    x = np.asarray(inputs["x"], dtype=np.float32)
    B = x.shape[0]
    if FUSED:
        nc = _get_nc(DEPTH)
        shared = _prep(inputs, range(DEPTH))
        in_maps = [dict(shared, x=np.ascontiguousarray(x[b])) for b in range(B)]
        res = run_bass_kernel_spmd(nc, in_maps, core_ids=list(range(B)))
        return np.stack([np.asarray(r["out"]) for r in res.results], axis=0).astype(np.float32)
    cur = [np.ascontiguousarray(x[b]) for b in range(B)]
    for l in range(DEPTH):
        nc = _get_nc(1)
        shared = _prep(inputs, [l])
        in_maps = [dict(shared, x=cur[b]) for b in range(B)]
        res = run_bass_kernel_spmd(nc, in_maps, core_ids=list(range(B)))
        cur = [np.ascontiguousarray(np.asarray(r["out"], dtype=np.float32)) for r in res.results]
    return np.stack(cur, axis=0).astype(np.float32)
```

```python
import contextlib
import numpy as np
import concourse.bass as bass
import concourse.mybir as mybir
from concourse.bass_utils import run_bass_kernel_spmd

F32 = mybir.dt.float32
BF16 = mybir.dt.bfloat16
I32 = mybir.dt.int32
U32 = mybir.dt.uint32
AF = mybir.ActivationFunctionType
ALU = mybir.AluOpType
AX = mybir.AxisListType

D = 2048
S = 2048
NT = 16
KC = 16
DIN = 3328
NE = 32
CAP = 384
NSLOT = NE * CAP
DEPTH = 4
ALPHA = float((2 * DEPTH) ** 0.25)
LN_EPS = 1e-5
RMS_EPS = 1e-6
GELU_NATIVE = False

CA_BQ = 0
CA_BK = 8
CA_BUX = 9
CA_BUG = 17
CA_CW = 25
CA_CB = 57
CA_BA = 65
CA_BXG = 73
CA_LAM = 81
CA_GA = 89
CA_GL = 97
NA = 105
RB_BV = 0
RB_BR = 128
RB_SK = 160
NB = 176


class Tk:
    __slots__ = ("w", "r")

    def __init__(self):
        self.w = None
        self.r = {}


class Sched:
    NDS = 24

    def __init__(self, nc, es):
        self.nc = nc
        self.eng = {"pe": nc.tensor, "act": nc.scalar, "dve": nc.vector, "pool": nc.gpsimd, "sp": nc.sync}
        self.semobj = {}
        for k in self.eng:
            self.semobj[k] = es.enter_context(nc.semaphore("s_" + k))
        self.nds = {"sp": 24, "pool": 32, "act": 4}
        self.dcnt = {}
        self.dnext = {}
        for q, n in self.nds.items():
            for i in range(n):
                self.semobj[("d", q, i)] = es.enter_context(nc.semaphore("d%s%d" % (q, i)))
                self.dcnt[(q, i)] = 0
            self.dnext[q] = 0
        self.cnt = {k: 0 for k in self.eng}
        self.seen = {k: {} for k in self.eng}

    def _wait(self, e, key, val):
        if self.seen[e].get(key, 0) >= val:
            return
        self.seen[e][key] = val
        self.eng[e].wait_ge(self.semobj[key], val)

    def _deps(self, e, reads, writes, is_dma):
        for t in reads:
            if t.w is not None:
                self._wait(e, t.w[0], t.w[1])
        for t in writes:
            if t.w is not None and (is_dma or t.w[0] != e):
                self._wait(e, t.w[0], t.w[1])
            for key, val in t.r.items():
                if is_dma or key != e:
                    self._wait(e, key, val)

    def _mark(self, ev, reads, writes):
        for t in reads:
            if t.r.get(ev[0], 0) < ev[1]:
                t.r[ev[0]] = ev[1]
        for t in writes:
            t.w = ev
            t.r = {}

    def op(self, e, fn, reads=(), writes=()):
        self._deps(e, reads, writes, False)
        ins = fn()
        self.cnt[e] += 1
        ins.then_inc(self.semobj[e], 1)
        self._mark((e, self.cnt[e]), reads, writes)

    def dma(self, q, fn, reads=(), writes=()):
        i = self.dnext[q]
        self.dnext[q] = (i + 1) % self.nds[q]
        key = ("d", q, i)
        if self.dcnt[(q, i)] > 0:
            self._wait(q, key, 16 * self.dcnt[(q, i)])
        self._deps(q, reads, writes, True)
        ins = fn()
        self.dcnt[(q, i)] += 1
        ins.then_inc(self.semobj[key], 16)
        self._mark((key, 16 * self.dcnt[(q, i)]), reads, writes)

    def barrier(self):
        evs = [(e, c) for e, c in self.cnt.items() if c > 0]
        evs += [(("d", q, i), 16 * c) for (q, i), c in self.dcnt.items() if c > 0]
        for e in self.eng:
            for key, val in evs:
                if key != e:
                    self._wait(e, key, val)


def build(nlayers, dbg=()):
    nc = bass.Bass("TRN2", target_bir_lowering=False)
    L = nlayers

    def din(name, shape, dt=F32):
        return nc.dram_tensor(name, list(shape), dt, kind="ExternalInput").ap()

    def dscr(name, shape, dt=F32):
        kind = "ExternalOutput" if name in dbg else "Internal"
        return nc.dram_tensor(name, list(shape), dt, kind=kind).ap()

    x_in = din("x", [S, D])
    w_in = din("w_in", [L, D, DIN])
    colsA_d = din("colsA", [L, 128, NA])
    rowsB_d = din("rowsB", [L, 128, NB])
    rowsBig_d = din("rowsBig", [L, 128, 5, D])
    wab_d = din("wab", [L, 128, 8, 128])
    wxb_d = din("wxb", [L, 128, 8, 128])
    w_out = din("w_out", [L, D, D])
    w_r = din("w_router", [L, D, NE])
    w_up = din("w_up", [L, NE, D, 2 * 1024])
    bupT_d = din("bupT", [L, 128, NE * 16])
    w_dn = din("w_down", [L, NE, 1024, D])
    b_dn = din("b_down", [L, NE, D])
    c_identf = din("c_identf", [128, 128])
    c_identb = din("c_identb", [128, 128], BF16)
    c_cos = din("c_cos", [128, S])
    c_sin = din("c_sin", [128, S])
    c_permR = din("c_permR", [128, 128])
    c_mprev = din("c_mprev", [128, 1024], BF16)
    c_mcur = din("c_mcur", [128, 1024], BF16)
    c_triu = din("c_triu", [128, 128], BF16)
    c_onesb = din("c_onesb", [128, 128], BF16)
    c_iota = din("c_iota", [128, 64])
    c_tokid = din("c_tokid", [128, NT], I32)
    c_zero = din("c_zero", [128, 96], I32)
    out_d = nc.dram_tensor("out", [S, D], F32, kind="ExternalOutput").ap()

    qT_d = dscr("qT_d", [9, 128, S], BF16)
    V_d = dscr("V_d", [128, NT * 2 * 65], BF16)
    mT_d = dscr("mT_d", [16, 128, S], BF16)
    xa_d = dscr("xa_d", [S, D])
    x1_d = dscr("x1_d", [S, D])
    x1b_d = dscr("x1b_d", [S, D], BF16)
    ys_d = dscr("ys_d", [NSLOT, D])
    stok_d = dscr("stok_d", [NSLOT, 1], I32)
    dbg_d = dscr("dbg_d", [128, 4096])

    es = contextlib.ExitStack()
    S_ = Sched(nc, es)

    uniq = [0]

    def sbt(st, name, shape, dt=F32):
        uniq[0] += 1
        return st.enter_context(nc.sbuf_tensor("sb%d_%s" % (uniq[0], name), list(shape), dt)), Tk()

    def pst(st, name, shape, dt=F32):
        uniq[0] += 1
        return st.enter_context(nc.psum_tensor("ps%d_%s" % (uniq[0], name), list(shape), dt)), Tk()

    PE = lambda fn, r, w: S_.op("pe", fn, r, w)
    ACT = lambda fn, r, w: S_.op("act", fn, r, w)
    DVE = lambda fn, r, w: S_.op("dve", fn, r, w)
    POOL = lambda fn, r, w: S_.op("pool", fn, r, w)
    dramT = {}

    def DT(ap_name):
        if ap_name not in dramT:
            dramT[ap_name] = Tk()
        return dramT[ap_name]

    def dma_sp(out, in_, r, w):
        S_.dma("sp", lambda: nc.sync.dma_start(out=out, in_=in_), r, w)

    def dma_pool(out, in_, r, w):
        S_.dma("pool", lambda: nc.gpsimd.dma_start(out=out, in_=in_), r, w)

    identf, t_identf = sbt(es, "identf", [128, 128])
    identb, t_identb = sbt(es, "identb", [128, 128], BF16)
    onesb, t_onesb = sbt(es, "onesb", [128, 128], BF16)
    triu, t_triu = sbt(es, "triu", [128, 128], BF16)
    iota, t_iota = sbt(es, "iota", [128, 64])
    tokid, t_tokid = sbt(es, "tokid", [128, NT], I32)
    onesf, t_onesf = sbt(es, "onesf", [128, 2])
    colsA, t_colsA = sbt(es, "colsA", [128, NA])
    rowsB, t_rowsB = sbt(es, "rowsB", [128, NB])
    c12, t_c12 = sbt(es, "c12", [128, 16])
    rstdl, t_rstdl = sbt(es, "rstdl", [128, NT])
    gates, t_gates = sbt(es, "gates", [128, NT, 4])
    sloti, t_sloti = sbt(es, "sloti", [128, NT, 4], I32)
    t_slotis = [Tk() for _ in range(NT)]
    GT, t_GT = sbt(es, "GT", [32, NT, 128])
    exps, t_exps = sbt(es, "exps", [128, 16])

    dma_sp(identf[:], c_identf, [], [t_identf])
    dma_sp(identb[:], c_identb, [], [t_identb])
    dma_sp(onesb[:], c_onesb, [], [t_onesb])
    dma_sp(triu[:], c_triu, [], [t_triu])
    dma_sp(iota[:], c_iota, [], [t_iota])
    dma_sp(tokid[:], c_tokid, [], [t_tokid])
    DVE(lambda: nc.vector.memset(onesf[:], 1.0), [], [t_onesf])
    with contextlib.ExitStack() as st0:
        zt, t_zt = sbt(st0, "zt", [128, 96], I32)
        dma_sp(zt[:], c_zero, [], [t_zt])
        dma_sp(stok_d.rearrange("(p j) o -> p (j o)", p=128), zt[:], [t_zt], [DT("stok")])
        S_.barrier()

    def ln_tile(st_small, res, t_res, grow, brow, t_rows, tagname):
        stats, t_stats = st_small["stats"]
        mv, t_mv = st_small["mv"]
        for j in range(4):
            DVE(lambda j=j: nc.vector.bn_stats(out=stats[:, j, :], in_=res[:, j * 512:(j + 1) * 512]),
                [t_res], [t_stats])
        DVE(lambda: nc.vector.bn_aggr(out=mv[:, 0:2], in_=stats[:]), [t_stats], [t_mv])
        ACT(lambda: nc.scalar.activation(out=mv[:, 2:3], in_=mv[:, 1:2], func=AF.Sqrt, bias=LN_EPS), [t_mv], [t_mv])
        DVE(lambda: nc.vector.reciprocal(out=mv[:, 2:3], in_=mv[:, 2:3]), [t_mv], [t_mv])
        DVE(lambda: nc.vector.scalar_tensor_tensor(out=mv[:, 3:4], in0=mv[:, 0:1], scalar=-1.0, in1=mv[:, 2:3],
                                                   op0=ALU.mult, op1=ALU.mult), [t_mv], [t_mv])
        ACT(lambda: nc.scalar.activation(out=res[:], in_=res[:], func=AF.Identity, scale=mv[:, 2:3],
                                         bias=mv[:, 3:4]), [t_res, t_mv], [t_res])
        POOL(lambda: nc.gpsimd.tensor_tensor(out=res[:], in0=res[:], in1=grow, op=ALU.mult),
             [t_res, t_rows], [t_res])
        POOL(lambda: nc.gpsimd.tensor_tensor(out=res[:], in0=res[:], in1=brow, op=ALU.add),
             [t_res, t_rows], [t_res])

    for l in range(L):
        xsrc = x_in if l == 0 else xa_d
        t_xsrc = DT("x_in") if l == 0 else DT("xa")
        last = (l == L - 1)
        dma_sp(colsA[:], colsA_d[l], [], [t_colsA])
        dma_sp(rowsB[:], rowsB_d[l], [], [t_rowsB])
        ACT(lambda: nc.scalar.activation(out=c12[:, 0:8], in_=colsA[:, CA_LAM:CA_LAM + 8], func=AF.Exp, scale=-1.0),
            [t_colsA], [t_c12])
        ACT(lambda: nc.scalar.activation(out=c12[:, 0:8], in_=c12[:, 0:8], func=AF.Ln, bias=1.0),
            [t_c12], [t_c12])
        DVE(lambda: nc.vector.tensor_scalar(out=c12[:, 8:16], in0=c12[:, 0:8], scalar1=-16.0, scalar2=None,
                                            op0=ALU.mult), [t_c12], [t_c12])
        DVE(lambda: nc.vector.tensor_scalar(out=c12[:, 0:8], in0=c12[:, 0:8], scalar1=-8.0, scalar2=None,
                                            op0=ALU.mult), [t_c12], [t_c12])
        ACT(lambda: nc.scalar.activation(out=exps[:], in_=rowsB[:, RB_SK:RB_SK + 16], func=AF.Exp),
            [t_rowsB], [t_exps])

        with contextlib.ExitStack() as sa:
            xT, t_xT = sbt(sa, "xT", [128, KC, S], BF16)
            wring = [sbt(sa, "wr%d" % i, [128, KC, 512], BF16) for i in range(2)]
            wcnt = [0]
            with contextlib.ExitStack() as s1:
                xin = [sbt(s1, "xin%d" % i, [128, D]) for i in range(2)]
                tp = [pst(s1, "tp%d" % i, [128, 4, 128]) for i in range(2)]
                k = 0
                for tt in range(NT):
                    xi, t_xi = xin[tt % 2]
                    dma_sp(xi[:], xsrc[tt * 128:(tt + 1) * 128, :], [t_xsrc], [t_xi])
                    for g in range(4):
                        p_, t_p = tp[k % 2]
                        for j in range(4):
                            kc = g * 4 + j
                            PE(lambda kc=kc, j=j, p_=p_, xi=xi: nc.tensor.transpose(
                                out=p_[:, j, :], in_=xi[:, kc * 128:(kc + 1) * 128], identity=identf[:]),
                               [t_xi, t_identf], [t_p])
                        dst = xT[:, g * 4:(g + 1) * 4, tt * 128:(tt + 1) * 128]
                        if k % 2 == 0:
                            ACT(lambda p_=p_, dst=dst: nc.scalar.copy(out=dst, in_=p_[:]), [t_p], [t_xT])
                        else:
                            DVE(lambda p_=p_, dst=dst: nc.vector.tensor_copy(out=dst, in_=p_[:]), [t_p], [t_xT])
                        k += 1
                S_.barrier()

            def wload(colspecs):
                wt, t_wt = wring[wcnt[0] % 2]
                wcnt[0] += 1
                src = w_in[l].rearrange("(kc kp) n -> kp kc n", kp=128)
                for (d0, s0, n) in colspecs:
                    dma_pool(wt[:, :, d0:d0 + n], src[:, :, s0:s0 + n], [], [t_wt])
                return wt, t_wt

            with contextlib.ExitStack() as s1:
                cosT, t_cos = sbt(s1, "cosT", [128, S])
                sinT, t_sin = sbt(s1, "sinT", [128, S])
                permR, t_permR = sbt(s1, "permR", [128, 128])
                dma_sp(cosT[:], c_cos, [], [t_cos])
                dma_sp(sinT[:], c_sin, [], [t_sin])
                dma_sp(permR[:], c_permR, [], [t_permR])
                qf = [sbt(s1, "qf%d" % i, [128, 512]) for i in range(2)]
                t1 = [sbt(s1, "t1%d" % i, [128, 512]) for i in range(2)]
                qo = [sbt(s1, "qo%d" % i, [128, S], BF16) for i in range(2)]
                Vt, t_V = sbt(s1, "Vt", [128, NT * 2 * 65], BF16)
                pa = [pst(s1, "pa%d" % i, [128, 512]) for i in range(3)]
                pr = [pst(s1, "pr%d" % i, [128, 512]) for i in range(2)]
                pv = [pst(s1, "pv%d" % i, [128, 128]) for i in range(2)]
                DVE(lambda: nc.vector.memset(Vt[:], 1.0), [], [t_V])
                it = 0
                for g in range(3):
                    ncols = 512 if g < 2 else 256
                    wt, t_wt = wload([(0, g * 512, ncols)])
                    nch = 4 if g < 2 else 1
                    for j in range(nch):
                        ch = g * 4 + j
                        bcol = colsA[:, CA_BQ + ch:CA_BQ + ch + 1]
                        qo_, t_qo = qo[ch % 2]
                        for tb in range(4):
                            p_, t_p = pa[it % 3]
                            r_, t_r = pr[it % 2]
                            qf_, t_qf = qf[it % 2]
                            t1_, t_t1 = t1[it % 2]
                            it += 1
                            for kc in range(KC):
                                PE(lambda kc=kc, p_=p_, wt=wt, j=j, tb=tb: nc.tensor.matmul(
                                    p_[:], lhsT=wt[:, kc, j * 128:(j + 1) * 128],
                                    rhs=xT[:, kc, tb * 512:(tb + 1) * 512], start=(kc == 0), stop=(kc == KC - 1)),
                                   [t_wt, t_xT], [t_p])
                            ACT(lambda p_=p_, qf_=qf_, bcol=bcol: nc.scalar.activation(
                                out=qf_[:], in_=p_[:], func=AF.Identity, bias=bcol), [t_p, t_colsA], [t_qf])
                            PE(lambda r_=r_, qf_=qf_: nc.tensor.matmul(r_[:], lhsT=permR[:], rhs=qf_[:],
                                                                       start=True, stop=True),
                               [t_permR, t_qf], [t_r])
                            sl = slice(tb * 512, (tb + 1) * 512)
                            DVE(lambda t1_=t1_, qf_=qf_, sl=sl: nc.vector.tensor_tensor(
                                out=t1_[:], in0=qf_[:], in1=cosT[:, sl], op=ALU.mult), [t_qf, t_cos], [t_t1])
                            DVE(lambda qf_=qf_, r_=r_, sl=sl: nc.vector.tensor_tensor(
                                out=qf_[:], in0=r_[:], in1=sinT[:, sl], op=ALU.mult), [t_r, t_sin], [t_qf])
                            DVE(lambda qo_=qo_, t1_=t1_, qf_=qf_, sl=sl: nc.vector.tensor_tensor(
                                out=qo_[:, sl], in0=t1_[:], in1=qf_[:], op=ALU.add), [t_t1, t_qf], [t_qo])
                        dma_sp(qT_d[ch], qo_[:], [t_qo], [DT("qT")])
                    if g == 2:
                        for tt in range(NT):
                            p_, t_p = pv[tt % 2]
                            for kc in range(KC):
                                PE(lambda kc=kc, p_=p_, tt=tt, wt=wt: nc.tensor.matmul(
                                    p_[:], lhsT=xT[:, kc, tt * 128:(tt + 1) * 128], rhs=wt[:, kc, 128:256],
                                    start=(kc == 0), stop=(kc == KC - 1)), [t_wt, t_xT], [t_p])
                            for hk in range(2):
                                o0 = (tt * 2 + hk) * 65
                                DVE(lambda p_=p_, hk=hk, o0=o0: nc.vector.tensor_tensor(
                                    out=Vt[:, o0:o0 + 64], in0=p_[:, hk * 64:(hk + 1) * 64],
                                    in1=rowsB[:, RB_BV + hk * 64:RB_BV + (hk + 1) * 64], op=ALU.add),
                                    [t_p, t_rowsB], [t_V])
                        dma_sp(V_d, Vt[:], [t_V], [DT("V")])
                S_.barrier()

            with contextlib.ExitStack() as s2:
                wab, t_wab = sbt(s2, "wab", [128, 8, 128], BF16)
                wxb, t_wxb = sbt(s2, "wxb", [128, 8, 128], BF16)
                dma_pool(wab[:], wab_d[l], [], [t_wab])
                dma_pool(wxb[:], wxb_d[l], [], [t_wxb])
                uxp, t_uxp = sbt(s2, "uxp", [128, S + 3])
                u, t_u = sbt(s2, "u", [128, S])
                ub, t_ub = sbt(s2, "ub", [128, S], BF16)
                r_, t_r = sbt(s2, "r", [128, S])
                M_, t_M = sbt(s2, "M", [128, S])
                ig, t_ig = sbt(s2, "ig", [128, S])
                gel, t_gel = sbt(s2, "gel", [128, S])
                h_, t_h = sbt(s2, "hscan", [128, S])
                gtmp, t_gtmp = sbt(s2, "gtmp", [128, S])
                mtl, t_mtl = sbt(s2, "mtl", [128, S], BF16)
                sqacc, t_sq = sbt(s2, "sqacc", [128, S])
                pa = [pst(s2, "pb%d" % i, [128, 512]) for i in range(4)]
                pg = [pst(s2, "pg%d" % i, [128, 512]) for i in range(3)]
                pss, t_pss = pst(s2, "pss", [128, NT])
                DVE(lambda: nc.vector.memset(uxp[:, 0:3], 0.0), [], [t_uxp])
                ia = 0
                igc = 0
                wnext = wload([(0, 1280, 128), (128, 2304, 128)])
                for c in range(8):
                    wt, t_wt = wnext
                    if c + 1 < 8:
                        wnext = wload([(0, 1280 + (c + 1) * 128, 128), (128, 2304 + (c + 1) * 128, 128)])
                    for tb in range(4):
                        p_, t_p = pa[ia % 4]
                        ia += 1
                        for kc in range(KC):
                            PE(lambda kc=kc, p_=p_, tb=tb, wt=wt: nc.tensor.matmul(
                                p_[:], lhsT=wt[:, kc, 0:128], rhs=xT[:, kc, tb * 512:(tb + 1) * 512],
                                start=(kc == 0), stop=(kc == KC - 1)), [t_wt, t_xT], [t_p])
                        ACT(lambda p_=p_, tb=tb, c=c: nc.scalar.activation(
                            out=uxp[:, 3 + tb * 512:3 + (tb + 1) * 512], in_=p_[:], func=AF.Identity,
                            bias=colsA[:, CA_BUX + c:CA_BUX + c + 1]), [t_p, t_colsA], [t_uxp])
                    for tb in range(4):
                        p_, t_p = pa[ia % 4]
                        ia += 1
                        for kc in range(KC):
                            PE(lambda kc=kc, p_=p_, tb=tb, wt=wt: nc.tensor.matmul(
                                p_[:], lhsT=wt[:, kc, 128:256], rhs=xT[:, kc, tb * 512:(tb + 1) * 512],
                                start=(kc == 0), stop=(kc == KC - 1)), [t_wt, t_xT], [t_p])
                        ACT(lambda p_=p_, tb=tb, c=c: nc.scalar.activation(
                            out=gel[:, tb * 512:(tb + 1) * 512], in_=p_[:], func=AF.Identity,
                            bias=colsA[:, CA_BUG + c:CA_BUG + c + 1]), [t_p, t_colsA], [t_gel])
                    cw = lambda k_, c=c: colsA[:, CA_CW + k_ * 8 + c:CA_CW + k_ * 8 + c + 1]
                    DVE(lambda c=c, cw=cw: nc.vector.tensor_scalar(
                        out=u[:], in0=uxp[:, 3:S + 3], scalar1=cw(3), scalar2=colsA[:, CA_CB + c:CA_CB + c + 1],
                        op0=ALU.mult, op1=ALU.add), [t_uxp, t_colsA], [t_u])
                    for k_ in (2, 1, 0):
                        DVE(lambda k_=k_, cw=cw: nc.vector.scalar_tensor_tensor(
                            out=u[:], in0=uxp[:, k_:k_ + S], scalar=cw(k_), in1=u[:], op0=ALU.mult, op1=ALU.add),
                            [t_uxp, t_colsA, t_u], [t_u])
                    DVE(lambda: nc.vector.tensor_copy(out=ub[:], in_=u[:]), [t_u], [t_ub])
                    ACT(lambda: nc.scalar.activation(out=gtmp[:], in_=gel[:], func=AF.Square), [t_gel], [t_gtmp])
                    DVE(lambda: nc.vector.tensor_scalar(out=gtmp[:], in0=gtmp[:], scalar1=0.044715, scalar2=1.0,
                                                        op0=ALU.mult, op1=ALU.add), [t_gtmp], [t_gtmp])
                    DVE(lambda: nc.vector.tensor_tensor(out=gtmp[:], in0=gtmp[:], in1=gel[:], op=ALU.mult),
                        [t_gtmp, t_gel], [t_gtmp])
                    ACT(lambda: nc.scalar.activation(out=gtmp[:], in_=gtmp[:], func=AF.Sigmoid, scale=1.5957691216),
                        [t_gtmp], [t_gtmp])
                    DVE(lambda: nc.vector.tensor_tensor(out=gel[:], in0=gel[:], in1=gtmp[:], op=ALU.mult),
                        [t_gel, t_gtmp], [t_gel])
                    for (wb, t_wb, dst, t_dst, bc) in ((wab, t_wab, r_, t_r, CA_BA), (wxb, t_wxb, ig, t_ig, CA_BXG)):
                        for tb in range(4):
                            p_, t_p = pg[igc % 3]
                            igc += 1
                            PE(lambda p_=p_, wb=wb, tb=tb, c=c: nc.tensor.matmul(
                                p_[:], lhsT=wb[:, c, :], rhs=ub[:, tb * 512:(tb + 1) * 512], start=True, stop=True),
                               [t_wb, t_ub], [t_p])
                            ACT(lambda p_=p_, dst=dst, tb=tb, bc=bc, c=c: nc.scalar.activation(
                                out=dst[:, tb * 512:(tb + 1) * 512], in_=p_[:], func=AF.Sigmoid,
                                bias=colsA[:, bc + c:bc + c + 1]), [t_p, t_colsA], [t_dst])
                    ACT(lambda c=c: nc.scalar.activation(out=M_[:], in_=r_[:], func=AF.Exp,
                                                         scale=c12[:, 8 + c:9 + c]), [t_r, t_c12], [t_M])
                    ACT(lambda c=c: nc.scalar.activation(out=r_[:], in_=r_[:], func=AF.Exp,
                                                         scale=c12[:, c:c + 1]), [t_r, t_c12], [t_r])
                    ACT(lambda: nc.scalar.activation(out=M_[:], in_=M_[:], func=AF.Sqrt, scale=-1.0, bias=1.0),
                        [t_M], [t_M])
                    DVE(lambda: nc.vector.tensor_tensor(out=ig[:], in0=ig[:], in1=u[:], op=ALU.mult),
                        [t_ig, t_u], [t_ig])
                    DVE(lambda: nc.vector.tensor_tensor(out=ig[:], in0=ig[:], in1=M_[:], op=ALU.mult),
                        [t_ig, t_M], [t_ig])
                    DVE(lambda: nc.vector.tensor_tensor_scan(out=h_[:], data0=r_[:], data1=ig[:], initial=0.0,
                                                             op0=ALU.mult, op1=ALU.add), [t_r, t_ig], [t_h])
                    DVE(lambda: nc.vector.tensor_tensor(out=gel[:], in0=gel[:], in1=h_[:], op=ALU.mult),
                        [t_gel, t_h], [t_gel])
                    ACT(lambda c=c: nc.scalar.activation(out=mtl[:], in_=gel[:], func=AF.Identity,
                                                         scale=colsA[:, CA_GL + c:CA_GL + c + 1]),
                        [t_gel, t_colsA], [t_mtl])
                    dma_sp(mT_d[8 + c], mtl[:], [t_mtl], [DT("mT")])
                    if c == 0:
                        ACT(lambda: nc.scalar.activation(out=sqacc[:], in_=gel[:], func=AF.Square), [t_gel], [t_sq])
                    else:
                        ACT(lambda: nc.scalar.activation(out=gtmp[:], in_=gel[:], func=AF.Square),
                            [t_gel], [t_gtmp])
                        POOL(lambda: nc.gpsimd.tensor_tensor(out=sqacc[:], in0=sqacc[:], in1=gtmp[:], op=ALU.add),
                             [t_sq, t_gtmp], [t_sq])
                for tt in range(NT):
                    PE(lambda tt=tt: nc.tensor.matmul(pss[:, tt:tt + 1], lhsT=sqacc[:, tt * 128:(tt + 1) * 128],
                                                      rhs=onesf[:, 0:1], start=True, stop=True),
                       [t_sq, t_onesf], [t_pss])
                ACT(lambda: nc.scalar.activation(out=rstdl[:], in_=pss[:], func=AF.Sqrt, scale=1.0 / 1024,
                                                 bias=RMS_EPS), [t_pss], [t_rstdl])
                DVE(lambda: nc.vector.reciprocal(out=rstdl[:], in_=rstdl[:]), [t_rstdl], [t_rstdl])
                S_.barrier()
        S_.barrier()

        with contextlib.ExitStack() as sbk:
            qT, t_qT = sbt(sbk, "qT", [128, 9, S], BF16)
            Vt, t_V = sbt(sbk, "VtB", [128, NT * 2 * 65], BF16)
            mprev, t_mprev = sbt(sbk, "mprev", [128, 1024], BF16)
            mcur, t_mcur = sbt(sbk, "mcur", [128, 1024], BF16)
            mTa, t_mTa = sbt(sbk, "mTa", [128, 8, S], BF16)
            dma_sp(qT[:], qT_d.rearrange("c p t -> p c t"), [DT("qT")], [t_qT])
            dma_sp(Vt[:], V_d, [DT("V")], [t_V])
            dma_sp(mprev[:], c_mprev, [], [t_mprev])
            dma_sp(mcur[:], c_mcur, [], [t_mcur])
            E = [sbt(sbk, "E%d" % i, [128, 1024], BF16) for i in range(8)]
            at = [sbt(sbk, "at%d" % i, [128, 1024]) for i in range(2)]
            atb = [sbt(sbk, "atb%d" % i, [128, 1024], BF16) for i in range(2)]
            small = [sbt(sbk, "sm%d" % i, [128, 32]) for i in range(2)]
            psc = [pst(sbk, "psc%d" % i, [128, 512]) for i in range(4)]
            po = [pst(sbk, "po%d" % i, [128, 4, 65]) for i in range(2)]
            ptr, t_ptr = pst(sbk, "ptr", [128, 8, 128], BF16)
            cB = {"ie": 0, "isc": 0}
            EsAll = {}

            def stS(qb):
                kbs = ([qb - 1] if qb > 0 else []) + [qb]
                for hk in range(2):
                    Es = []
                    for kb in kbs:
                        E_, t_E = E[cB["ie"] % 8]
                        cB["ie"] += 1
                        for half in range(2):
                            p_, t_p = psc[cB["isc"] % 4]
                            cB["isc"] += 1
                            PE(lambda p_=p_, hk=hk, kb=kb, half=half, qb=qb: nc.tensor.matmul(
                                p_[:], lhsT=qT[hk * 64:(hk + 1) * 64, 8, kb * 128:(kb + 1) * 128],
                                rhs=qT[hk * 64:(hk + 1) * 64, half * 4:(half + 1) * 4, qb * 128:(qb + 1) * 128],
                                start=True, stop=True), [t_qT], [t_p])
                            ACT(lambda p_=p_, E_=E_, half=half: nc.scalar.activation(
                                out=E_[:, half * 512:(half + 1) * 512], in_=p_[:], func=AF.Exp, scale=0.125),
                                [t_p], [t_E])
                        msk, t_msk = (mcur, t_mcur) if kb == qb else (mprev, t_mprev)
                        POOL(lambda E_=E_, msk=msk: nc.gpsimd.tensor_tensor(out=E_[:], in0=E_[:], in1=msk[:],
                                                                            op=ALU.mult), [t_E, t_msk], [t_E])
                        Es.append((E_, t_E, kb))
                    EsAll[(qb, hk)] = Es

            def stP(qb):
                at_, t_at = at[qb % 2]
                atb_, t_atb = atb[qb % 2]
                sm, t_sm = small[qb % 2]
                for hk in range(2):
                    Es = EsAll.pop((qb, hk))
                    for hh in range(2):
                        o_, t_o = po[hh]
                        for g4 in range(4):
                            g = hh * 4 + g4
                            for i_, (E_, t_E, kb) in enumerate(Es):
                                v0 = (kb * 2 + hk) * 65
                                PE(lambda o_=o_, g4=g4, g=g, E_=E_, v0=v0, i_=i_, n=len(Es): nc.tensor.matmul(
                                    o_[:, g4, :], lhsT=E_[:, g * 128:(g + 1) * 128], rhs=Vt[:, v0:v0 + 65],
                                    start=(i_ == 0), stop=(i_ == n - 1)), [t_E, t_V], [t_o])
                        h0 = hk * 8 + hh * 4
                        DVE(lambda o_=o_, sm=sm, h0=h0: nc.vector.tensor_tensor(
                            out=sm[:, h0:h0 + 4], in0=o_[:, :, 64], in1=exps[:, h0:h0 + 4], op=ALU.add),
                            [t_o, t_exps], [t_sm])
                        DVE(lambda sm=sm, h0=h0: nc.vector.reciprocal(out=sm[:, h0:h0 + 4], in_=sm[:, h0:h0 + 4]),
                            [t_sm], [t_sm])
                        for g4 in range(4):
                            h = h0 + g4
                            DVE(lambda o_=o_, g4=g4, h=h, at_=at_, sm=sm: nc.vector.tensor_scalar(
                                out=at_[:, h * 64:(h + 1) * 64], in0=o_[:, g4, 0:64], scalar1=sm[:, h:h + 1],
                                scalar2=None, op0=ALU.mult), [t_o, t_sm], [t_at])
                ACT(lambda atb_=atb_, at_=at_, sm=sm: nc.scalar.activation(
                    out=atb_[:], in_=at_[:], func=AF.Square, accum_out=sm[:, 16:17]), [t_at], [t_atb, t_sm])
                ACT(lambda sm=sm: nc.scalar.activation(out=sm[:, 17:18], in_=sm[:, 16:17], func=AF.Sqrt,
                                                       scale=1.0 / 1024, bias=RMS_EPS), [t_sm], [t_sm])
                DVE(lambda sm=sm: nc.vector.reciprocal(out=sm[:, 17:18], in_=sm[:, 17:18]), [t_sm], [t_sm])
                DVE(lambda atb_=atb_, at_=at_, sm=sm: nc.vector.tensor_scalar(
                    out=atb_[:], in0=at_[:], scalar1=sm[:, 17:18], scalar2=None, op0=ALU.mult),
                    [t_at, t_sm, t_atb], [t_atb])

            def stT(qb):
                atb_, t_atb = atb[qb % 2]
                for c in range(8):
                    PE(lambda c=c, atb_=atb_: nc.tensor.transpose(out=ptr[:, c, :], in_=atb_[:, c * 128:(c + 1) * 128],
                                                                  identity=identb[:]), [t_atb, t_identb], [t_ptr])
                for c in range(8):
                    if c % 2 == 0:
                        ACT(lambda c=c, qb=qb: nc.scalar.activation(
                            out=mTa[:, c, qb * 128:(qb + 1) * 128], in_=ptr[:, c, :], func=AF.Identity,
                            scale=colsA[:, CA_GA + c:CA_GA + c + 1]), [t_ptr, t_colsA], [t_mTa])
                    else:
                        DVE(lambda c=c, qb=qb: nc.vector.tensor_scalar(
                            out=mTa[:, c, qb * 128:(qb + 1) * 128], in0=ptr[:, c, :],
                            scalar1=colsA[:, CA_GA + c:CA_GA + c + 1], scalar2=None, op0=ALU.mult),
                            [t_ptr, t_colsA], [t_mTa])

            stS(0)
            for qb in range(NT):
                if qb + 1 < NT:
                    stS(qb + 1)
                stP(qb)
                if qb > 0:
                    stT(qb - 1)
            stT(NT - 1)
            dma_sp(mT_d[0:8].rearrange("c p t -> p c t"), mTa[:], [t_mTa], [DT("mT")])
            S_.barrier()

        with contextlib.ExitStack() as sc:
            wo, t_wo = sbt(sc, "wo", [128, KC, D], BF16)
            wsrc = w_out[l].rearrange("(kc kp) n -> kp kc n", kp=128)
            for nb in range(4):
                dma_pool(wo[:, :, nb * 512:(nb + 1) * 512], wsrc[:, :, nb * 512:(nb + 1) * 512], [], [t_wo])
            wr32, t_wr = sbt(sc, "wr32", [128, KC, NE])
            dma_sp(wr32[:], w_r[l].rearrange("(kc kp) e -> kp kc e", kp=128), [], [t_wr])
            rows, t_rows = sbt(sc, "rowsC", [128, 3, D])
            dma_sp(rows[:], rowsBig_d[l][:, 0:3, :], [], [t_rows])
            mTr = [sbt(sc, "mTr%d" % i, [128, KC, 512], BF16) for i in range(2)]
            xin = [sbt(sc, "xinC%d" % i, [128, D]) for i in range(2)]
            res = [sbt(sc, "resC%d" % i, [128, D]) for i in range(2)]
            x1b = [sbt(sc, "x1b%d" % i, [128, D], BF16) for i in range(2)]
            x1T, t_x1T = sbt(sc, "x1T", [128, KC, 128])
            lg, t_lg = sbt(sc, "lg", [128, NE])
            maskb, t_maskb = sbt(sc, "maskb", [128, NT, NE], BF16)
            idxf, t_idxf = sbt(sc, "idxf", [128, NT, 4])
            st_small = {"stats": sbt(sc, "stats", [128, 4, 6]), "mv": sbt(sc, "mv", [128, 4])}
            top8, t_top8 = sbt(sc, "top8", [128, 8])
            idx8, t_idx8 = sbt(sc, "idx8", [128, 8], U32)
            sm, t_sm = sbt(sc, "smC", [128, 16])
            G, t_G = sbt(sc, "G", [128, NE])
            oh, t_oh = sbt(sc, "oh", [128, NE])
            posC, t_posC = sbt(sc, "posC", [128, NE])
            slotf, t_slotf = sbt(sc, "slotf", [128, NT, 4])
            pya = [pst(sc, "pya%d" % i, [128, 512]) for i in range(2)]
            pyl = [pst(sc, "pyl%d" % i, [128, 512]) for i in range(2)]
            ptp = [pst(sc, "ptpC%d" % i, [128, 4, 128]) for i in range(2)]
            plg_all, t_plg = pst(sc, "plg", [128, 96])
            plg = plg_all[:, 0:32]
            ppos = plg_all[:, 32:96]
            t_ppos = t_plg
            runc, t_runc = sbt(sc, "runc", [128, NE])
            DVE(lambda: nc.vector.memset(runc[:], 0.0), [], [t_runc])
            pgt, t_pgt = pst(sc, "pgt", [32, 128])
            cc = {"iy": 0, "itp": 0, "mt": None}

            def ldC(tt):
                xi, t_xi = xin[tt % 2]
                dma_sp(xi[:], xsrc[tt * 128:(tt + 1) * 128, :], [t_xsrc], [t_xi])
                if tt % 4 == 0:
                    mt_, t_mt_ = mTr[(tt // 4) % 2]
                    dma_sp(mt_[:], mT_d.rearrange("c p t -> p c t")[:, :, tt * 128:tt * 128 + 512],
                           [DT("mT")], [t_mt_])

            def stage1(tt):
                if tt % 4 == 0:
                    cc["mt"] = mTr[(tt // 4) % 2]
                mt, t_mt = cc["mt"]
                xi, t_xi = xin[tt % 2]
                rs, t_rs = res[tt % 2]
                xb_, t_xb = x1b[tt % 2]
                DVE(lambda xi=xi: nc.vector.scalar_tensor_tensor(out=xi[:], in0=xi[:], scalar=ALPHA, in1=rows[:, 0, :],
                                                                 op0=ALU.mult, op1=ALU.add), [t_xi, t_rows], [t_xi])
                tl = (tt % 4) * 128
                for nb in range(4):
                    a_, t_a = pya[cc["iy"] % 2]
                    l_, t_l = pyl[cc["iy"] % 2]
                    cc["iy"] += 1
                    for kc in range(8):
                        PE(lambda kc=kc, a_=a_, mt=mt, tl=tl, nb=nb: nc.tensor.matmul(
                            a_[:], lhsT=mt[:, kc, tl:tl + 128], rhs=wo[:, kc, nb * 512:(nb + 1) * 512],
                            start=(kc == 0), stop=(kc == 7)), [t_mt, t_wo], [t_a])
                    for kc in range(8, 16):
                        PE(lambda kc=kc, l_=l_, mt=mt, tl=tl, nb=nb: nc.tensor.matmul(
                            l_[:], lhsT=mt[:, kc, tl:tl + 128], rhs=wo[:, kc, nb * 512:(nb + 1) * 512],
                            start=(kc == 8), stop=(kc == 15)), [t_mt, t_wo], [t_l])
                    sl = slice(nb * 512, (nb + 1) * 512)
                    DVE(lambda a_=a_, rs=rs, xi=xi, sl=sl: nc.vector.tensor_tensor(
                        out=rs[:, sl], in0=a_[:], in1=xi[:, sl], op=ALU.add), [t_a, t_xi], [t_rs])
                    DVE(lambda l_=l_, rs=rs, sl=sl, tt=tt: nc.vector.scalar_tensor_tensor(
                        out=rs[:, sl], in0=l_[:], scalar=rstdl[:, tt:tt + 1], in1=rs[:, sl],
                        op0=ALU.mult, op1=ALU.add), [t_l, t_rstdl, t_rs], [t_rs])

            def stage2(tt):
                rs, t_rs = res[tt % 2]
                xb_, t_xb = x1b[tt % 2]
                ln_tile(st_small, rs, t_rs, rows[:, 1, :], rows[:, 2, :], t_rows, "c")
                dma_sp(x1_d[tt * 128:(tt + 1) * 128, :], rs[:], [t_rs], [DT("x1")])
                ACT(lambda xb_=xb_, rs=rs: nc.scalar.copy(out=xb_[:], in_=rs[:]), [t_rs], [t_xb])
                dma_sp(x1b_d[tt * 128:(tt + 1) * 128, :], xb_[:], [t_xb], [DT("x1b")])

            def stage2b(tt):
                rs, t_rs = res[tt % 2]
                for g in range(4):
                    p_, t_p = ptp[cc["itp"] % 2]
                    cc["itp"] += 1
                    for j in range(4):
                        kc = g * 4 + j
                        PE(lambda kc=kc, j=j, p_=p_, rs=rs: nc.tensor.transpose(
                            out=p_[:, j, :], in_=rs[:, kc * 128:(kc + 1) * 128], identity=identf[:]),
                           [t_rs, t_identf], [t_p])
                    if g % 2 == 0:
                        ACT(lambda p_=p_, g=g: nc.scalar.copy(out=x1T[:, g * 4:(g + 1) * 4, :], in_=p_[:]),
                            [t_p], [t_x1T])
                    else:
                        DVE(lambda p_=p_, g=g: nc.vector.tensor_copy(out=x1T[:, g * 4:(g + 1) * 4, :], in_=p_[:]),
                            [t_p], [t_x1T])
                for kc in range(KC):
                    PE(lambda kc=kc: nc.tensor.matmul(plg, lhsT=x1T[:, kc, :], rhs=wr32[:, kc, :],
                                                      start=(kc == 0), stop=(kc == KC - 1)),
                       [t_x1T, t_wr], [t_plg])
                DVE(lambda: nc.vector.tensor_tensor(out=lg[:], in0=plg, in1=rowsB[:, RB_BR:RB_BR + NE],
                                                    op=ALU.add), [t_plg, t_rowsB], [t_lg])
                DVE(lambda: nc.vector.max(out=top8[:], in_=lg[:]), [t_lg], [t_top8])
                DVE(lambda: nc.vector.max_index(out=idx8[:], in_max=top8[:], in_values=lg[:]),
                    [t_top8, t_lg], [t_idx8])
                DVE(lambda: nc.vector.tensor_scalar(out=sm[:, 0:1], in0=top8[:, 0:1], scalar1=-1.0, scalar2=None,
                                                    op0=ALU.mult), [t_top8], [t_sm])
                ACT(lambda: nc.scalar.activation(out=sm[:, 4:8], in_=top8[:, 0:4], func=AF.Exp, bias=sm[:, 0:1],
                                                 accum_out=sm[:, 1:2]), [t_top8, t_sm], [t_sm])
                DVE(lambda: nc.vector.reciprocal(out=sm[:, 2:3], in_=sm[:, 1:2]), [t_sm], [t_sm])
                DVE(lambda tt=tt: nc.vector.tensor_scalar(out=gates[:, tt, :], in0=sm[:, 4:8], scalar1=sm[:, 2:3],
                                                          scalar2=None, op0=ALU.mult), [t_sm], [t_gates])
                DVE(lambda tt=tt: nc.vector.tensor_scalar(out=maskb[:, tt, :], in0=lg[:], scalar1=top8[:, 3:4],
                                                          scalar2=None, op0=ALU.is_ge), [t_lg, t_top8], [t_maskb])
                DVE(lambda tt=tt: nc.vector.tensor_copy(out=idxf[:, tt, :], in_=idx8[:, 0:4]), [t_idx8], [t_idxf])
                for k_ in range(4):
                    dst, t_dst = (G, t_G) if k_ == 0 else (oh, t_oh)
                    DVE(lambda k_=k_, tt=tt, dst=dst: nc.vector.tensor_scalar(
                        out=dst[:], in0=iota[:, 0:32], scalar1=idxf[:, tt, k_:k_ + 1],
                        scalar2=gates[:, tt, k_:k_ + 1], op0=ALU.is_equal, op1=ALU.mult),
                        [t_iota, t_idxf, t_gates], [t_dst])
                    if k_ > 0:
                        DVE(lambda: nc.vector.tensor_tensor(out=G[:], in0=G[:], in1=oh[:], op=ALU.add),
                            [t_G, t_oh], [t_G])
                PE(lambda: nc.tensor.transpose(out=pgt[:], in_=G[:], identity=identf[:]), [t_G, t_identf], [t_pgt])
                ACT(lambda tt=tt: nc.scalar.copy(out=GT[:, tt, :], in_=pgt[:]), [t_pgt], [t_GT])
                PE(lambda tt=tt: nc.tensor.matmul(ppos[:, 0:32], lhsT=triu[:], rhs=maskb[:, tt, :], start=True,
                                                  stop=True), [t_triu, t_maskb], [t_ppos])
                PE(lambda tt=tt: nc.tensor.matmul(ppos[:, 32:64], lhsT=onesb[:], rhs=maskb[:, tt, :], start=True,
                                                  stop=True), [t_onesb, t_maskb], [t_ppos])
                DVE(lambda: nc.vector.tensor_tensor(out=posC[:], in0=ppos[:, 0:32], in1=runc[:], op=ALU.add),
                    [t_ppos, t_runc], [t_posC])
                DVE(lambda: nc.vector.scalar_tensor_tensor(out=posC[:], in0=posC[:], scalar=float(CAP - 1),
                                                           in1=iota[:, 32:64], op0=ALU.min, op1=ALU.add),
                    [t_posC, t_iota], [t_posC])
                DVE(lambda: nc.vector.tensor_tensor(out=runc[:], in0=runc[:], in1=ppos[:, 32:64], op=ALU.add),
                    [t_ppos, t_runc], [t_runc])
                for k_ in range(4):
                    DVE(lambda k_=k_, tt=tt: nc.vector.scalar_tensor_tensor(
                        out=oh[:], in0=iota[:, 0:32], scalar=idxf[:, tt, k_:k_ + 1], in1=posC[:],
                        op0=ALU.is_equal, op1=ALU.mult), [t_iota, t_idxf, t_posC], [t_oh])
                    DVE(lambda k_=k_, tt=tt: nc.vector.reduce_sum(out=slotf[:, tt, k_:k_ + 1], in_=oh[:], axis=AX.X),
                        [t_oh], [t_slotf])
                DVE(lambda tt=tt: nc.vector.tensor_copy(out=sloti[:, tt, :], in_=slotf[:, tt, :]),
                    [t_slotf], [t_slotis[tt]])
                for k_ in range(4):
                    S_.dma("pool", lambda tt=tt, k_=k_: nc.gpsimd.indirect_dma_start(
                        out=stok_d[:, :], out_offset=bass.IndirectOffsetOnAxis(ap=sloti[:, tt, k_:k_ + 1], axis=0),
                        in_=tokid[:, tt:tt + 1], in_offset=None), [t_slotis[tt], t_tokid], [DT("stok")])

            ldC(0)
            stage1(0)
            for tt in range(NT):
                if tt + 1 < NT:
                    ldC(tt + 1)
                stage2(tt)
                if tt + 1 < NT:
                    stage1(tt + 1)
                stage2b(tt)
            S_.barrier()

        with contextlib.ExitStack() as sd:
            bupT, t_bup = sbt(sd, "bupT", [128, NE * 16])
            dma_sp(bupT[:], bupT_d[l], [], [t_bup])
            sidx_all, _ = sbt(sd, "sidx", [128, NE, 3], I32)
            t_sidx = [Tk() for _ in range(NE)]
            for e in range(NE):
                dma_sp(sidx_all[:, e, :], stok_d[e * CAP:(e + 1) * CAP, :].rearrange("(p j) o -> p (j o)", p=128),
                       [DT("stok")], [t_sidx[e]])
            xs = [[sbt(sd, "xs%d_%d" % (i, j), [128, D], BF16) for j in range(3)] for i in range(2)]
            xsT = [sbt(sd, "xsT%d" % i, [128, KC, CAP], BF16) for i in range(2)]
            NWU = 4
            NWD = 4
            wu = [sbt(sd, "wu%d" % i, [128, KC, 512], BF16) for i in range(NWU)]
            wd = [sbt(sd, "wd%d" % i, [128, 8, 512], BF16) for i in range(NWD)]
            gs = [sbt(sd, "gs%d" % i, [128, 4, CAP]) for i in range(2)]
            sg = [sbt(sd, "sg%d" % i, [128, CAP]) for i in range(2)]
            ln_ = [sbt(sd, "ln%d" % i, [128, CAP]) for i in range(2)]
            hT = [sbt(sd, "hT%d" % i, [128, 8, CAP], BF16) for i in range(1)]
            yo = [sbt(sd, "yo%d" % i, [128, 512]) for i in range(4)]
            ptr_ = [pst(sd, "ptrD%d" % i, [128, 8, 128], BF16) for i in range(2)]
            pu = [pst(sd, "pu%d" % i, [128, CAP]) for i in range(3)]
            pd = [pst(sd, "pd%d" % i, [128, 512]) for i in range(3)]
            cnt = {"wu": 0, "wd": 0, "tr": 0, "pu": 0, "pd": 0, "gs": 0, "sg": 0, "yo": 0}

            def gather(e):
                t_si = t_sidx[e]
                for j in range(3):
                    xs_, t_xs = xs[e % 2][j]
                    S_.dma("pool", lambda xs_=xs_, e=e, j=j: nc.gpsimd.indirect_dma_start(
                        out=xs_[:, :], out_offset=None, in_=x1b_d[:, :],
                        in_offset=bass.IndirectOffsetOnAxis(ap=sidx_all[:, e, j:j + 1], axis=0)),
                        [t_si, DT("x1b")], [t_xs])

            def transp(e):
                xsT_, t_xsT = xsT[e % 2]
                for j in range(3):
                    xs_, t_xs = xs[e % 2][j]
                    for g in range(2):
                        p_, t_p = ptr_[cnt["tr"] % 2]
                        cnt["tr"] += 1
                        for c in range(8):
                            kc = g * 8 + c
                            PE(lambda p_=p_, c=c, kc=kc, xs_=xs_: nc.tensor.transpose(
                                out=p_[:, c, :], in_=xs_[:, kc * 128:(kc + 1) * 128], identity=identb[:]),
                               [t_xs, t_identb], [t_p])
                        dst = xsT_[:, g * 8:(g + 1) * 8, j * 128:(j + 1) * 128]
                        if cnt["tr"] % 2 == 0:
                            ACT(lambda p_=p_, dst=dst: nc.scalar.copy(out=dst, in_=p_[:]), [t_p], [t_xsT])
                        else:
                            DVE(lambda p_=p_, dst=dst: nc.vector.tensor_copy(out=dst, in_=p_[:]), [t_p], [t_xsT])

            gather(0)
            transp(0)
            for e in range(NE):
                xsT_, t_xsT = xsT[e % 2]
                hT_, t_hT = hT[0]
                if e + 1 < NE:
                    gather(e + 1)
                usrc = w_up[l, e].rearrange("(kc kp) n -> kp kc n", kp=128)
                for hf in range(2):
                    gs_, t_gs = gs[cnt["gs"] % 2]
                    cnt["gs"] += 1
                    for part in range(2):
                        w_, t_w = wu[cnt["wu"] % NWU]
                        cnt["wu"] += 1
                        c0 = part * 1024 + hf * 512
                        dma_pool(w_[:], usrc[:, :, c0:c0 + 512], [], [t_w])
                        for j4 in range(4):
                            fc = hf * 4 + j4
                            p_, t_p = pu[cnt["pu"] % 3]
                            cnt["pu"] += 1
                            for kc in range(KC):
                                PE(lambda p_=p_, w_=w_, j4=j4, kc=kc, xsT_=xsT_: nc.tensor.matmul(
                                    p_[:], lhsT=w_[:, kc, j4 * 128:(j4 + 1) * 128], rhs=xsT_[:, kc, :],
                                    start=(kc == 0), stop=(kc == KC - 1)), [t_w, t_xsT], [t_p])
                            bcol = bupT[:, e * 16 + part * 8 + fc:e * 16 + part * 8 + fc + 1]
                            if part == 0:
                                sg_, t_sg = sg[cnt["sg"] % 2]
                                cnt["sg"] += 1
                                DVE(lambda p_=p_, gs_=gs_, j4=j4, bcol=bcol: nc.vector.tensor_scalar(
                                    out=gs_[:, j4, :], in0=p_[:], scalar1=bcol, scalar2=7.0, op0=ALU.add,
                                    op1=ALU.min), [t_p, t_bup], [t_gs])
                                ACT(lambda sg_=sg_, gs_=gs_, j4=j4: nc.scalar.activation(
                                    out=sg_[:], in_=gs_[:, j4, :], func=AF.Sigmoid, scale=1.702), [t_gs], [t_sg])
                                DVE(lambda sg_=sg_, gs_=gs_, j4=j4: nc.vector.tensor_tensor(
                                    out=gs_[:, j4, :], in0=gs_[:, j4, :], in1=sg_[:], op=ALU.mult),
                                    [t_gs, t_sg], [t_gs])
                            else:
                                l_, t_l = ln_[cnt["sg"] % 2]
                                cnt["sg"] += 1
                                DVE(lambda p_=p_, l_=l_, bcol=bcol: nc.vector.tensor_scalar(
                                    out=l_[:], in0=p_[:], scalar1=bcol, scalar2=7.0, op0=ALU.add, op1=ALU.min),
                                    [t_p, t_bup], [t_l])
                                DVE(lambda l_=l_: nc.vector.tensor_scalar(
                                    out=l_[:], in0=l_[:], scalar1=-7.0, scalar2=1.0, op0=ALU.max, op1=ALU.add),
                                    [t_l], [t_l])
                                DVE(lambda l_=l_, gs_=gs_, j4=j4, fc=fc, hT_=hT_: nc.vector.tensor_tensor(
                                    out=hT_[:, fc, :], in0=l_[:], in1=gs_[:, j4, :], op=ALU.mult),
                                    [t_l, t_gs], [t_hT])
                if e + 1 < NE:
                    transp(e + 1)
                dsrc = w_dn[l, e].rearrange("(fc fp) n -> fp fc n", fp=128)
                for nb in range(4):
                    w_, t_w = wd[cnt["wd"] % NWD]
                    cnt["wd"] += 1
                    dma_pool(w_[:], dsrc[:, :, nb * 512:(nb + 1) * 512], [], [t_w])
                    for j in range(3):
                        p_, t_p = pd[cnt["pd"] % 3]
                        cnt["pd"] += 1
                        for fc in range(8):
                            PE(lambda p_=p_, fc=fc, j=j, w_=w_, hT_=hT_: nc.tensor.matmul(
                                p_[:], lhsT=hT_[:, fc, j * 128:(j + 1) * 128], rhs=w_[:, fc, :],
                                start=(fc == 0), stop=(fc == 7)), [t_hT, t_w], [t_p])
                        yt, t_yt = yo[cnt["yo"] % 4]
                        cnt["yo"] += 1
                        if cnt["yo"] % 2 == 0:
                            ACT(lambda p_=p_, yt=yt: nc.scalar.copy(out=yt[:], in_=p_[:]), [t_p], [t_yt])
                        else:
                            DVE(lambda p_=p_, yt=yt: nc.vector.tensor_copy(out=yt[:], in_=p_[:]), [t_p], [t_yt])
                        dma_sp(ys_d[e * CAP:(e + 1) * CAP, :].rearrange("(p j) n -> p j n", j=3)[:, j, nb * 512:(nb + 1) * 512], yt[:],
                               [t_yt], [DT("ys")])
            S_.barrier()

        with contextlib.ExitStack() as se:
            rows, t_rows = sbt(se, "rowsE", [128, 2, D])
            dma_sp(rows[:], rowsBig_d[l][:, 3:5, :], [], [t_rows])
            bd, t_bd = sbt(se, "bd", [32, D])
            dma_sp(bd[:], b_dn[l], [], [t_bd])
            yg = [[sbt(se, "yg%d_%d" % (i, k_), [128, D]) for k_ in range(4)] for i in range(2)]
            xi2 = [sbt(se, "xi2%d" % i, [128, D]) for i in range(2)]
            st_small = {"stats": sbt(se, "statsE", [128, 4, 6]), "mv": sbt(se, "mvE", [128, 4])}
            pb = [pst(se, "pbE%d" % i, [128, 512]) for i in range(4)]
            ipb = 0
            dst_d = out_d if last else xa_d
            t_dst = DT("out") if last else DT("xa")
            def fetchE(tt):
                xi, t_xi = xi2[tt % 2]
                dma_sp(xi[:], x1_d[tt * 128:(tt + 1) * 128, :], [DT("x1")], [t_xi])
                for k_ in range(4):
                    y_, t_y = yg[tt % 2][k_]
                    S_.dma("pool", lambda y_=y_, tt=tt, k_=k_: nc.gpsimd.indirect_dma_start(
                        out=y_[:, :], out_offset=None, in_=ys_d[:, :],
                        in_offset=bass.IndirectOffsetOnAxis(ap=sloti[:, tt, k_:k_ + 1], axis=0)),
                        [t_slotis[tt], DT("ys")], [t_y])

            fetchE(0)
            for tt in range(NT):
                xi, t_xi = xi2[tt % 2]
                if tt + 1 < NT:
                    fetchE(tt + 1)
                for nb in range(4):
                    p_, t_p = pb[ipb % 4]
                    ipb += 1
                    PE(lambda p_=p_, tt=tt, nb=nb: nc.tensor.matmul(p_[:], lhsT=GT[:, tt, :],
                                                                    rhs=bd[:, nb * 512:(nb + 1) * 512],
                                                                    start=True, stop=True), [t_GT, t_bd], [t_p])
                    sl = slice(nb * 512, (nb + 1) * 512)
                    DVE(lambda p_=p_, xi=xi, sl=sl: nc.vector.scalar_tensor_tensor(
                        out=xi[:, sl], in0=xi[:, sl], scalar=ALPHA, in1=p_[:], op0=ALU.mult, op1=ALU.add),
                        [t_xi, t_p], [t_xi])
                for k_ in range(4):
                    y_, t_y = yg[tt % 2][k_]
                    DVE(lambda y_=y_, xi=xi, tt=tt, k_=k_: nc.vector.scalar_tensor_tensor(
                        out=xi[:], in0=y_[:], scalar=gates[:, tt, k_:k_ + 1], in1=xi[:], op0=ALU.mult,
                        op1=ALU.add), [t_y, t_gates, t_xi], [t_xi])
                ln_tile(st_small, xi, t_xi, rows[:, 0, :], rows[:, 1, :], t_rows, "e")
                dma_sp(dst_d[tt * 128:(tt + 1) * 128, :], xi[:], [t_xi], [t_dst])
            S_.barrier()
    S_.barrier()
    es.close()
    return nc


def _consts():
    import ml_dtypes
    bf = ml_dtypes.bfloat16
    p = np.arange(128)
    d = p % 64
    inv_freq = (1.0 / (np.float32(10000.0) ** (np.arange(0, 64, 2, dtype=np.float32) / np.float32(64)))).astype(np.float32)
    ang = (np.arange(S, dtype=np.float32)[:, None] * inv_freq[None, :]).astype(np.float32)
    cos = np.cos(ang).astype(np.float32)
    sin = np.sin(ang).astype(np.float32)
    cosT = np.ascontiguousarray(cos[:, d % 32].T)
    sgn = np.where(d < 32, -1.0, 1.0).astype(np.float32)
    sinT = np.ascontiguousarray(sin[:, d % 32].T * sgn[:, None]).astype(np.float32)
    partner = 64 * (p // 64) + ((p % 64) + 32) % 64
    permR = np.zeros((128, 128), np.float32)
    permR[partner, p] = 1.0
    kk = np.arange(128)[:, None]
    qq = np.arange(128)[None, :]
    mprev = np.tile((kk > qq).astype(np.float32), (1, 8)).astype(bf)
    mcur = np.tile((kk <= qq).astype(np.float32), (1, 8)).astype(bf)
    triu = (kk < qq).astype(np.float32).astype(bf)
    iota = np.zeros((128, 64), np.float32)
    iota[:, 0:32] = np.arange(32)[None, :]
    iota[:, 32:64] = np.arange(32)[None, :] * CAP
    tokid = (np.arange(NT)[None, :] * 128 + p[:, None]).astype(np.int32)
    return {
        "c_identf": np.eye(128, dtype=np.float32), "c_identb": np.eye(128, dtype=np.float32).astype(bf),
        "c_cos": cosT, "c_sin": sinT, "c_permR": permR, "c_mprev": mprev, "c_mcur": mcur, "c_triu": triu,
        "c_onesb": np.ones((128, 128), np.float32).astype(bf), "c_iota": iota, "c_tokid": tokid,
        "c_zero": np.zeros((128, 96), np.int32),
    }


def _prep(inp, layers):
    ls = list(layers)
    f = lambda k: np.asarray(inp[k])
    pq = np.arange(1024)
    cq, pp = pq // 128, pq % 128
    perm_q = (np.where(pp < 64, cq, 8 + cq) * 64 + (pp % 64))
    perm = np.concatenate([perm_q, np.arange(1024, DIN)])
    w_in = np.ascontiguousarray(f("w_in")[ls][:, :, perm])
    b_in = f("b_in")[ls][:, perm]
    Ln = len(ls)
    colsA = np.zeros((Ln, 128, NA), np.float32)
    col = lambda v: v.reshape(Ln, -1, 128).transpose(0, 2, 1)
    colsA[:, :, CA_BQ:CA_BQ + 8] = col(b_in[:, 0:1024])
    colsA[:, :, CA_BK:CA_BK + 1] = col(b_in[:, 1024:1152])
    colsA[:, :, CA_BUX:CA_BUX + 8] = col(b_in[:, 1280:2304])
    colsA[:, :, CA_BUG:CA_BUG + 8] = col(b_in[:, 2304:3328])
    cw = f("conv_w")[ls]
    for k in range(4):
        colsA[:, :, CA_CW + k * 8:CA_CW + (k + 1) * 8] = col(cw[:, k, :])
    colsA[:, :, CA_CB:CA_CB + 8] = col(f("conv_b")[ls])
    colsA[:, :, CA_BA:CA_BA + 8] = col(f("lru_b_a")[ls])
    colsA[:, :, CA_BXG:CA_BXG + 8] = col(f("lru_b_x")[ls])
    colsA[:, :, CA_LAM:CA_LAM + 8] = col(f("lru_lambda")[ls])
    colsA[:, :, CA_GA:CA_GA + 8] = col(f("g_attn")[ls])
    colsA[:, :, CA_GL:CA_GL + 8] = col(f("g_lru")[ls])
    rowsB = np.zeros((Ln, 128, NB), np.float32)
    rowsB[:, :, RB_BV:RB_BV + 128] = b_in[:, None, 1152:1280]
    rowsB[:, :, RB_BR:RB_BR + 32] = f("b_router")[ls][:, None, :]
    rowsB[:, :, RB_SK:RB_SK + 16] = f("attn_sinks")[ls][:, None, :]
    rowsBig = np.zeros((Ln, 128, 5, D), np.float32)
    for i, k in enumerate(["b_out", "ln1_g", "ln1_b", "ln2_g", "ln2_b"]):
        rowsBig[:, :, i, :] = f(k)[ls][:, None, :]

    def bdiag(w):
        w = w[ls]
        o = np.zeros((Ln, 128, 8, 128), np.float32)
        for c in range(8):
            o[:, 0:64, c, 0:64] = w[:, 2 * c]
            o[:, 64:128, c, 64:128] = w[:, 2 * c + 1]
        return o
    bup = f("b_up")[ls]
    bupT = np.ascontiguousarray(bup.reshape(Ln, NE, 16, 128).transpose(0, 3, 1, 2).reshape(Ln, 128, NE * 16))
    m = {
        "w_in": w_in, "colsA": colsA, "rowsB": rowsB, "rowsBig": rowsBig,
        "wab": bdiag(f("lru_w_a")), "wxb": bdiag(f("lru_w_x")),
        "w_out": np.ascontiguousarray(f("w_out")[ls]), "w_router": np.ascontiguousarray(f("w_router")[ls]),
        "w_up": np.ascontiguousarray(f("w_up")[ls]), "bupT": bupT,
        "w_down": np.ascontiguousarray(f("w_down")[ls]), "b_down": np.ascontiguousarray(f("b_down")[ls]),
    }
    m.update(_consts())
    return m


_NC_CACHE = {}


def _get_nc(nl):
    if nl not in _NC_CACHE:
        _NC_CACHE[nl] = build(nl)
    return _NC_CACHE[nl]


FUSED = True


def kernel(**inputs):
    x = np.asarray(inputs["x"], dtype=np.float32)
    B = x.shape[0]
    if FUSED:
        nc = _get_nc(DEPTH)
        shared = _prep(inputs, range(DEPTH))
        in_maps = [dict(shared, x=np.ascontiguousarray(x[b])) for b in range(B)]
        res = run_bass_kernel_spmd(nc, in_maps, core_ids=list(range(B)))
        return np.stack([np.asarray(r["out"]) for r in res.results], axis=0).astype(np.float32)
    cur = [np.ascontiguousarray(x[b]) for b in range(B)]
    for l in range(DEPTH):
        nc = _get_nc(1)
        shared = _prep(inputs, [l])
        in_maps = [dict(shared, x=cur[b]) for b in range(B)]
        res = run_bass_kernel_spmd(nc, in_maps, core_ids=list(range(B)))
        cur = [np.ascontiguousarray(np.asarray(r["out"], dtype=np.float32)) for r in res.results]
    return np.stack(cur, axis=0).astype(np.float32)
```

```python
import contextlib
import numpy as np
import concourse.bass as bass
import concourse.mybir as mybir
from concourse.bass_utils import run_bass_kernel_spmd

F32 = mybir.dt.float32
BF16 = mybir.dt.bfloat16
I32 = mybir.dt.int32
U32 = mybir.dt.uint32
AF = mybir.ActivationFunctionType
ALU = mybir.AluOpType
AX = mybir.AxisListType

D = 2048
S = 2048
NT = 16
KC = 16
DIN = 3328
NE = 32
CAP = 384
NSLOT = NE * CAP
DEPTH = 4
ALPHA = float((2 * DEPTH) ** 0.25)
LN_EPS = 1e-5
RMS_EPS = 1e-6
GELU_NATIVE = False

CA_BQ = 0
CA_BK = 8
CA_BUX = 9
CA_BUG = 17
CA_CW = 25
CA_CB = 57
CA_BA = 65
CA_BXG = 73
CA_LAM = 81
CA_GA = 89
CA_GL = 97
NA = 105
RB_BV = 0
RB_BR = 128
RB_SK = 160
NB = 176


class Tk:
    __slots__ = ("w", "r")

    def __init__(self):
        self.w = None
        self.r = {}


class Sched:
    NDS = 24

    def __init__(self, nc, es):
        self.nc = nc
        self.eng = {"pe": nc.tensor, "act": nc.scalar, "dve": nc.vector, "pool": nc.gpsimd, "sp": nc.sync}
        self.semobj = {}
        for k in self.eng:
            self.semobj[k] = es.enter_context(nc.semaphore("s_" + k))
        self.nds = {"sp": 24, "pool": 32, "act": 4}
        self.dcnt = {}
        self.dnext = {}
        for q, n in self.nds.items():
            for i in range(n):
                self.semobj[("d", q, i)] = es.enter_context(nc.semaphore("d%s%d" % (q, i)))
                self.dcnt[(q, i)] = 0
            self.dnext[q] = 0
        self.cnt = {k: 0 for k in self.eng}
        self.seen = {k: {} for k in self.eng}

    def _wait(self, e, key, val):
        if self.seen[e].get(key, 0) >= val:
            return
        self.seen[e][key] = val
        self.eng[e].wait_ge(self.semobj[key], val)

    def _deps(self, e, reads, writes, is_dma):
        for t in reads:
            if t.w is not None:
                self._wait(e, t.w[0], t.w[1])
        for t in writes:
            if t.w is not None and (is_dma or t.w[0] != e):
                self._wait(e, t.w[0], t.w[1])
            for key, val in t.r.items():
                if is_dma or key != e:
                    self._wait(e, key, val)

    def _mark(self, ev, reads, writes):
        for t in reads:
            if t.r.get(ev[0], 0) < ev[1]:
                t.r[ev[0]] = ev[1]
        for t in writes:
            t.w = ev
            t.r = {}

    def op(self, e, fn, reads=(), writes=()):
        self._deps(e, reads, writes, False)
        ins = fn()
        self.cnt[e] += 1
        ins.then_inc(self.semobj[e], 1)
        self._mark((e, self.cnt[e]), reads, writes)

    def dma(self, q, fn, reads=(), writes=()):
        i = self.dnext[q]
        self.dnext[q] = (i + 1) % self.nds[q]
        key = ("d", q, i)
        if self.dcnt[(q, i)] > 0:
            self._wait(q, key, 16 * self.dcnt[(q, i)])
        self._deps(q, reads, writes, True)
        ins = fn()
        self.dcnt[(q, i)] += 1
        ins.then_inc(self.semobj[key], 16)
        self._mark((key, 16 * self.dcnt[(q, i)]), reads, writes)

    def barrier(self):
        evs = [(e, c) for e, c in self.cnt.items() if c > 0]
        evs += [(("d", q, i), 16 * c) for (q, i), c in self.dcnt.items() if c > 0]
        for e in self.eng:
            for key, val in evs:
                if key != e:
                    self._wait(e, key, val)


def build(nlayers, dbg=()):
    nc = bass.Bass("TRN2", target_bir_lowering=False)
    L = nlayers

    def din(name, shape, dt=F32):
        return nc.dram_tensor(name, list(shape), dt, kind="ExternalInput").ap()

    def dscr(name, shape, dt=F32):
        kind = "ExternalOutput" if name in dbg else "Internal"
        return nc.dram_tensor(name, list(shape), dt, kind=kind).ap()

    x_in = din("x", [S, D])
    w_in = din("w_in", [L, D, DIN])
    colsA_d = din("colsA", [L, 128, NA])
    rowsB_d = din("rowsB", [L, 128, NB])
    rowsBig_d = din("rowsBig", [L, 128, 5, D])
    wab_d = din("wab", [L, 128, 8, 128])
    wxb_d = din("wxb", [L, 128, 8, 128])
    w_out = din("w_out", [L, D, D])
    w_r = din("w_router", [L, D, NE])
    w_up = din("w_up", [L, NE, D, 2 * 1024])
    bupT_d = din("bupT", [L, 128, NE * 16])
    w_dn = din("w_down", [L, NE, 1024, D])
    b_dn = din("b_down", [L, NE, D])
    c_identf = din("c_identf", [128, 128])
    c_identb = din("c_identb", [128, 128], BF16)
    c_cos = din("c_cos", [128, S])
    c_sin = din("c_sin", [128, S])
    c_permR = din("c_permR", [128, 128])
    c_mprev = din("c_mprev", [128, 1024], BF16)
    c_mcur = din("c_mcur", [128, 1024], BF16)
    c_triu = din("c_triu", [128, 128], BF16)
    c_onesb = din("c_onesb", [128, 128], BF16)
    c_iota = din("c_iota", [128, 64])
    c_tokid = din("c_tokid", [128, NT], I32)
    c_zero = din("c_zero", [128, 96], I32)
    out_d = nc.dram_tensor("out", [S, D], F32, kind="ExternalOutput").ap()

    qT_d = dscr("qT_d", [9, 128, S], BF16)
    V_d = dscr("V_d", [128, NT * 2 * 65], BF16)
    mT_d = dscr("mT_d", [16, 128, S], BF16)
    xa_d = dscr("xa_d", [S, D])
    x1_d = dscr("x1_d", [S, D])
    x1b_d = dscr("x1b_d", [S, D], BF16)
    ys_d = dscr("ys_d", [NSLOT, D])
    stok_d = dscr("stok_d", [NSLOT, 1], I32)
    dbg_d = dscr("dbg_d", [128, 4096])

    es = contextlib.ExitStack()
    S_ = Sched(nc, es)

    uniq = [0]

    def sbt(st, name, shape, dt=F32):
        uniq[0] += 1
        return st.enter_context(nc.sbuf_tensor("sb%d_%s" % (uniq[0], name), list(shape), dt)), Tk()

    def pst(st, name, shape, dt=F32):
        uniq[0] += 1
        return st.enter_context(nc.psum_tensor("ps%d_%s" % (uniq[0], name), list(shape), dt)), Tk()

    PE = lambda fn, r, w: S_.op("pe", fn, r, w)
    ACT = lambda fn, r, w: S_.op("act", fn, r, w)
    DVE = lambda fn, r, w: S_.op("dve", fn, r, w)
    POOL = lambda fn, r, w: S_.op("pool", fn, r, w)
    dramT = {}

    def DT(ap_name):
        if ap_name not in dramT:
            dramT[ap_name] = Tk()
        return dramT[ap_name]

    def dma_sp(out, in_, r, w):
        S_.dma("sp", lambda: nc.sync.dma_start(out=out, in_=in_), r, w)

    def dma_pool(out, in_, r, w):
        S_.dma("pool", lambda: nc.gpsimd.dma_start(out=out, in_=in_), r, w)

    identf, t_identf = sbt(es, "identf", [128, 128])
    identb, t_identb = sbt(es, "identb", [128, 128], BF16)
    onesb, t_onesb = sbt(es, "onesb", [128, 128], BF16)
    triu, t_triu = sbt(es, "triu", [128, 128], BF16)
    iota, t_iota = sbt(es, "iota", [128, 64])
    tokid, t_tokid = sbt(es, "tokid", [128, NT], I32)
    onesf, t_onesf = sbt(es, "onesf", [128, 2])
    colsA, t_colsA = sbt(es, "colsA", [128, NA])
    rowsB, t_rowsB = sbt(es, "rowsB", [128, NB])
    c12, t_c12 = sbt(es, "c12", [128, 16])
    rstdl, t_rstdl = sbt(es, "rstdl", [128, NT])
    gates, t_gates = sbt(es, "gates", [128, NT, 4])
    sloti, t_sloti = sbt(es, "sloti", [128, NT, 4], I32)
    t_slotis = [Tk() for _ in range(NT)]
    GT, t_GT = sbt(es, "GT", [32, NT, 128])
    exps, t_exps = sbt(es, "exps", [128, 16])

    dma_sp(identf[:], c_identf, [], [t_identf])
    dma_sp(identb[:], c_identb, [], [t_identb])
    dma_sp(onesb[:], c_onesb, [], [t_onesb])
    dma_sp(triu[:], c_triu, [], [t_triu])
    dma_sp(iota[:], c_iota, [], [t_iota])
    dma_sp(tokid[:], c_tokid, [], [t_tokid])
    DVE(lambda: nc.vector.memset(onesf[:], 1.0), [], [t_onesf])
    with contextlib.ExitStack() as st0:
        zt, t_zt = sbt(st0, "zt", [128, 96], I32)
        dma_sp(zt[:], c_zero, [], [t_zt])
        dma_sp(stok_d.rearrange("(p j) o -> p (j o)", p=128), zt[:], [t_zt], [DT("stok")])
        S_.barrier()

    def ln_tile(st_small, res, t_res, grow, brow, t_rows, tagname):
        stats, t_stats = st_small["stats"]
        mv, t_mv = st_small["mv"]
        for j in range(4):
            DVE(lambda j=j: nc.vector.bn_stats(out=stats[:, j, :], in_=res[:, j * 512:(j + 1) * 512]),
                [t_res], [t_stats])
        DVE(lambda: nc.vector.bn_aggr(out=mv[:, 0:2], in_=stats[:]), [t_stats], [t_mv])
        ACT(lambda: nc.scalar.activation(out=mv[:, 2:3], in_=mv[:, 1:2], func=AF.Sqrt, bias=LN_EPS), [t_mv], [t_mv])
        DVE(lambda: nc.vector.reciprocal(out=mv[:, 2:3], in_=mv[:, 2:3]), [t_mv], [t_mv])
        DVE(lambda: nc.vector.scalar_tensor_tensor(out=mv[:, 3:4], in0=mv[:, 0:1], scalar=-1.0, in1=mv[:, 2:3],
                                                   op0=ALU.mult, op1=ALU.mult), [t_mv], [t_mv])
        ACT(lambda: nc.scalar.activation(out=res[:], in_=res[:], func=AF.Identity, scale=mv[:, 2:3],
                                         bias=mv[:, 3:4]), [t_res, t_mv], [t_res])
        POOL(lambda: nc.gpsimd.tensor_tensor(out=res[:], in0=res[:], in1=grow, op=ALU.mult),
             [t_res, t_rows], [t_res])
        POOL(lambda: nc.gpsimd.tensor_tensor(out=res[:], in0=res[:], in1=brow, op=ALU.add),
             [t_res, t_rows], [t_res])

    for l in range(L):
        xsrc = x_in if l == 0 else xa_d
        t_xsrc = DT("x_in") if l == 0 else DT("xa")
        last = (l == L - 1)
        dma_sp(colsA[:], colsA_d[l], [], [t_colsA])
        dma_sp(rowsB[:], rowsB_d[l], [], [t_rowsB])
        ACT(lambda: nc.scalar.activation(out=c12[:, 0:8], in_=colsA[:, CA_LAM:CA_LAM + 8], func=AF.Exp, scale=-1.0),
            [t_colsA], [t_c12])
        ACT(lambda: nc.scalar.activation(out=c12[:, 0:8], in_=c12[:, 0:8], func=AF.Ln, bias=1.0),
            [t_c12], [t_c12])
        DVE(lambda: nc.vector.tensor_scalar(out=c12[:, 8:16], in0=c12[:, 0:8], scalar1=-16.0, scalar2=None,
                                            op0=ALU.mult), [t_c12], [t_c12])
        DVE(lambda: nc.vector.tensor_scalar(out=c12[:, 0:8], in0=c12[:, 0:8], scalar1=-8.0, scalar2=None,
                                            op0=ALU.mult), [t_c12], [t_c12])
        ACT(lambda: nc.scalar.activation(out=exps[:], in_=rowsB[:, RB_SK:RB_SK + 16], func=AF.Exp),
            [t_rowsB], [t_exps])

        with contextlib.ExitStack() as sa:
            xT, t_xT = sbt(sa, "xT", [128, KC, S], BF16)
            wring = [sbt(sa, "wr%d" % i, [128, KC, 512], BF16) for i in range(2)]
            wcnt = [0]
            with contextlib.ExitStack() as s1:
                xin = [sbt(s1, "xin%d" % i, [128, D]) for i in range(2)]
                tp = [pst(s1, "tp%d" % i, [128, 4, 128]) for i in range(2)]
                k = 0
                for tt in range(NT):
                    xi, t_xi = xin[tt % 2]
                    dma_sp(xi[:], xsrc[tt * 128:(tt + 1) * 128, :], [t_xsrc], [t_xi])
                    for g in range(4):
                        p_, t_p = tp[k % 2]
                        for j in range(4):
                            kc = g * 4 + j
                            PE(lambda kc=kc, j=j, p_=p_, xi=xi: nc.tensor.transpose(
                                out=p_[:, j, :], in_=xi[:, kc * 128:(kc + 1) * 128], identity=identf[:]),
                               [t_xi, t_identf], [t_p])
                        dst = xT[:, g * 4:(g + 1) * 4, tt * 128:(tt + 1) * 128]
                        if k % 2 == 0:
                            ACT(lambda p_=p_, dst=dst: nc.scalar.copy(out=dst, in_=p_[:]), [t_p], [t_xT])
                        else:
                            DVE(lambda p_=p_, dst=dst: nc.vector.tensor_copy(out=dst, in_=p_[:]), [t_p], [t_xT])
                        k += 1
                S_.barrier()

            def wload(colspecs):
                wt, t_wt = wring[wcnt[0] % 2]
                wcnt[0] += 1
                src = w_in[l].rearrange("(kc kp) n -> kp kc n", kp=128)
                for (d0, s0, n) in colspecs:
                    dma_pool(wt[:, :, d0:d0 + n], src[:, :, s0:s0 + n], [], [t_wt])
                return wt, t_wt

            with contextlib.ExitStack() as s1:
                cosT, t_cos = sbt(s1, "cosT", [128, S])
                sinT, t_sin = sbt(s1, "sinT", [128, S])
                permR, t_permR = sbt(s1, "permR", [128, 128])
                dma_sp(cosT[:], c_cos, [], [t_cos])
                dma_sp(sinT[:], c_sin, [], [t_sin])
                dma_sp(permR[:], c_permR, [], [t_permR])
                qf = [sbt(s1, "qf%d" % i, [128, 512]) for i in range(2)]
                t1 = [sbt(s1, "t1%d" % i, [128, 512]) for i in range(2)]
                qo = [sbt(s1, "qo%d" % i, [128, S], BF16) for i in range(2)]
                Vt, t_V = sbt(s1, "Vt", [128, NT * 2 * 65], BF16)
                pa = [pst(s1, "pa%d" % i, [128, 512]) for i in range(3)]
                pr = [pst(s1, "pr%d" % i, [128, 512]) for i in range(2)]
                pv = [pst(s1, "pv%d" % i, [128, 128]) for i in range(2)]
                DVE(lambda: nc.vector.memset(Vt[:], 1.0), [], [t_V])
                it = 0
                for g in range(3):
                    ncols = 512 if g < 2 else 256
                    wt, t_wt = wload([(0, g * 512, ncols)])
                    nch = 4 if g < 2 else 1
                    for j in range(nch):
                        ch = g * 4 + j
                        bcol = colsA[:, CA_BQ + ch:CA_BQ + ch + 1]
                        qo_, t_qo = qo[ch % 2]
                        for tb in range(4):
                            p_, t_p = pa[it % 3]
                            r_, t_r = pr[it % 2]
                            qf_, t_qf = qf[it % 2]
                            t1_, t_t1 = t1[it % 2]
                            it += 1
                            for kc in range(KC):
                                PE(lambda kc=kc, p_=p_, wt=wt, j=j, tb=tb: nc.tensor.matmul(
                                    p_[:], lhsT=wt[:, kc, j * 128:(j + 1) * 128],
                                    rhs=xT[:, kc, tb * 512:(tb + 1) * 512], start=(kc == 0), stop=(kc == KC - 1)),
                                   [t_wt, t_xT], [t_p])
                            ACT(lambda p_=p_, qf_=qf_, bcol=bcol: nc.scalar.activation(
                                out=qf_[:], in_=p_[:], func=AF.Identity, bias=bcol), [t_p, t_colsA], [t_qf])
                            PE(lambda r_=r_, qf_=qf_: nc.tensor.matmul(r_[:], lhsT=permR[:], rhs=qf_[:],
                                                                       start=True, stop=True),
                               [t_permR, t_qf], [t_r])
                            sl = slice(tb * 512, (tb + 1) * 512)
                            DVE(lambda t1_=t1_, qf_=qf_, sl=sl: nc.vector.tensor_tensor(
                                out=t1_[:], in0=qf_[:], in1=cosT[:, sl], op=ALU.mult), [t_qf, t_cos], [t_t1])
                            DVE(lambda qf_=qf_, r_=r_, sl=sl: nc.vector.tensor_tensor(
                                out=qf_[:], in0=r_[:], in1=sinT[:, sl], op=ALU.mult), [t_r, t_sin], [t_qf])
                            DVE(lambda qo_=qo_, t1_=t1_, qf_=qf_, sl=sl: nc.vector.tensor_tensor(
                                out=qo_[:, sl], in0=t1_[:], in1=qf_[:], op=ALU.add), [t_t1, t_qf], [t_qo])
                        dma_sp(qT_d[ch], qo_[:], [t_qo], [DT("qT")])
                    if g == 2:
                        for tt in range(NT):
                            p_, t_p = pv[tt % 2]
                            for kc in range(KC):
                                PE(lambda kc=kc, p_=p_, tt=tt, wt=wt: nc.tensor.matmul(
                                    p_[:], lhsT=xT[:, kc, tt * 128:(tt + 1) * 128], rhs=wt[:, kc, 128:256],
                                    start=(kc == 0), stop=(kc == KC - 1)), [t_wt, t_xT], [t_p])
                            for hk in range(2):
                                o0 = (tt * 2 + hk) * 65
                                DVE(lambda p_=p_, hk=hk, o0=o0: nc.vector.tensor_tensor(
                                    out=Vt[:, o0:o0 + 64], in0=p_[:, hk * 64:(hk + 1) * 64],
                                    in1=rowsB[:, RB_BV + hk * 64:RB_BV + (hk + 1) * 64], op=ALU.add),
                                    [t_p, t_rowsB], [t_V])
                        dma_sp(V_d, Vt[:], [t_V], [DT("V")])
                S_.barrier()

            with contextlib.ExitStack() as s2:
                wab, t_wab = sbt(s2, "wab", [128, 8, 128], BF16)
                wxb, t_wxb = sbt(s2, "wxb", [128, 8, 128], BF16)
                dma_pool(wab[:], wab_d[l], [], [t_wab])
                dma_pool(wxb[:], wxb_d[l], [], [t_wxb])
                uxp, t_uxp = sbt(s2, "uxp", [128, S + 3])
                u, t_u = sbt(s2, "u", [128, S])
                ub, t_ub = sbt(s2, "ub", [128, S], BF16)
                r_, t_r = sbt(s2, "r", [128, S])
                M_, t_M = sbt(s2, "M", [128, S])
                ig, t_ig = sbt(s2, "ig", [128, S])
                gel, t_gel = sbt(s2, "gel", [128, S])
                h_, t_h = sbt(s2, "hscan", [128, S])
                gtmp, t_gtmp = sbt(s2, "gtmp", [128, S])
                mtl, t_mtl = sbt(s2, "mtl", [128, S], BF16)
                sqacc, t_sq = sbt(s2, "sqacc", [128, S])
                pa = [pst(s2, "pb%d" % i, [128, 512]) for i in range(4)]
                pg = [pst(s2, "pg%d" % i, [128, 512]) for i in range(3)]
                pss, t_pss = pst(s2, "pss", [128, NT])
                DVE(lambda: nc.vector.memset(uxp[:, 0:3], 0.0), [], [t_uxp])
                ia = 0
                igc = 0
                wnext = wload([(0, 1280, 128), (128, 2304, 128)])
                for c in range(8):
                    wt, t_wt = wnext
                    if c + 1 < 8:
                        wnext = wload([(0, 1280 + (c + 1) * 128, 128), (128, 2304 + (c + 1) * 128, 128)])
                    for tb in range(4):
                        p_, t_p = pa[ia % 4]
                        ia += 1
                        for kc in range(KC):
                            PE(lambda kc=kc, p_=p_, tb=tb, wt=wt: nc.tensor.matmul(
                                p_[:], lhsT=wt[:, kc, 0:128], rhs=xT[:, kc, tb * 512:(tb + 1) * 512],
                                start=(kc == 0), stop=(kc == KC - 1)), [t_wt, t_xT], [t_p])
                        ACT(lambda p_=p_, tb=tb, c=c: nc.scalar.activation(
                            out=uxp[:, 3 + tb * 512:3 + (tb + 1) * 512], in_=p_[:], func=AF.Identity,
                            bias=colsA[:, CA_BUX + c:CA_BUX + c + 1]), [t_p, t_colsA], [t_uxp])
                    for tb in range(4):
                        p_, t_p = pa[ia % 4]
                        ia += 1
                        for kc in range(KC):
                            PE(lambda kc=kc, p_=p_, tb=tb, wt=wt: nc.tensor.matmul(
                                p_[:], lhsT=wt[:, kc, 128:256], rhs=xT[:, kc, tb * 512:(tb + 1) * 512],
                                start=(kc == 0), stop=(kc == KC - 1)), [t_wt, t_xT], [t_p])
                        ACT(lambda p_=p_, tb=tb, c=c: nc.scalar.activation(
                            out=gel[:, tb * 512:(tb + 1) * 512], in_=p_[:], func=AF.Identity,
                            bias=colsA[:, CA_BUG + c:CA_BUG + c + 1]), [t_p, t_colsA], [t_gel])
                    cw = lambda k_, c=c: colsA[:, CA_CW + k_ * 8 + c:CA_CW + k_ * 8 + c + 1]
                    DVE(lambda c=c, cw=cw: nc.vector.tensor_scalar(
                        out=u[:], in0=uxp[:, 3:S + 3], scalar1=cw(3), scalar2=colsA[:, CA_CB + c:CA_CB + c + 1],
                        op0=ALU.mult, op1=ALU.add), [t_uxp, t_colsA], [t_u])
                    for k_ in (2, 1, 0):
                        DVE(lambda k_=k_, cw=cw: nc.vector.scalar_tensor_tensor(
                            out=u[:], in0=uxp[:, k_:k_ + S], scalar=cw(k_), in1=u[:], op0=ALU.mult, op1=ALU.add),
                            [t_uxp, t_colsA, t_u], [t_u])
                    DVE(lambda: nc.vector.tensor_copy(out=ub[:], in_=u[:]), [t_u], [t_ub])
                    ACT(lambda: nc.scalar.activation(out=gtmp[:], in_=gel[:], func=AF.Square), [t_gel], [t_gtmp])
                    DVE(lambda: nc.vector.tensor_scalar(out=gtmp[:], in0=gtmp[:], scalar1=0.044715, scalar2=1.0,
                                                        op0=ALU.mult, op1=ALU.add), [t_gtmp], [t_gtmp])
                    DVE(lambda: nc.vector.tensor_tensor(out=gtmp[:], in0=gtmp[:], in1=gel[:], op=ALU.mult),
                        [t_gtmp, t_gel], [t_gtmp])
                    ACT(lambda: nc.scalar.activation(out=gtmp[:], in_=gtmp[:], func=AF.Sigmoid, scale=1.5957691216),
                        [t_gtmp], [t_gtmp])
                    DVE(lambda: nc.vector.tensor_tensor(out=gel[:], in0=gel[:], in1=gtmp[:], op=ALU.mult),
                        [t_gel, t_gtmp], [t_gel])
                    for (wb, t_wb, dst, t_dst, bc) in ((wab, t_wab, r_, t_r, CA_BA), (wxb, t_wxb, ig, t_ig, CA_BXG)):
                        for tb in range(4):
                            p_, t_p = pg[igc % 3]
                            igc += 1
                            PE(lambda p_=p_, wb=wb, tb=tb, c=c: nc.tensor.matmul(
                                p_[:], lhsT=wb[:, c, :], rhs=ub[:, tb * 512:(tb + 1) * 512], start=True, stop=True),
                               [t_wb, t_ub], [t_p])
                            ACT(lambda p_=p_, dst=dst, tb=tb, bc=bc, c=c: nc.scalar.activation(
                                out=dst[:, tb * 512:(tb + 1) * 512], in_=p_[:], func=AF.Sigmoid,
                                bias=colsA[:, bc + c:bc + c + 1]), [t_p, t_colsA], [t_dst])
                    ACT(lambda c=c: nc.scalar.activation(out=M_[:], in_=r_[:], func=AF.Exp,
                                                         scale=c12[:, 8 + c:9 + c]), [t_r, t_c12], [t_M])
                    ACT(lambda c=c: nc.scalar.activation(out=r_[:], in_=r_[:], func=AF.Exp,
                                                         scale=c12[:, c:c + 1]), [t_r, t_c12], [t_r])
                    ACT(lambda: nc.scalar.activation(out=M_[:], in_=M_[:], func=AF.Sqrt, scale=-1.0, bias=1.0),
                        [t_M], [t_M])
                    DVE(lambda: nc.vector.tensor_tensor(out=ig[:], in0=ig[:], in1=u[:], op=ALU.mult),
                        [t_ig, t_u], [t_ig])
                    DVE(lambda: nc.vector.tensor_tensor(out=ig[:], in0=ig[:], in1=M_[:], op=ALU.mult),
                        [t_ig, t_M], [t_ig])
                    DVE(lambda: nc.vector.tensor_tensor_scan(out=h_[:], data0=r_[:], data1=ig[:], initial=0.0,
                                                             op0=ALU.mult, op1=ALU.add), [t_r, t_ig], [t_h])
                    DVE(lambda: nc.vector.tensor_tensor(out=gel[:], in0=gel[:], in1=h_[:], op=ALU.mult),
                        [t_gel, t_h], [t_gel])
                    ACT(lambda c=c: nc.scalar.activation(out=mtl[:], in_=gel[:], func=AF.Identity,
                                                         scale=colsA[:, CA_GL + c:CA_GL + c + 1]),
                        [t_gel, t_colsA], [t_mtl])
                    dma_sp(mT_d[8 + c], mtl[:], [t_mtl], [DT("mT")])
                    if c == 0:
                        ACT(lambda: nc.scalar.activation(out=sqacc[:], in_=gel[:], func=AF.Square), [t_gel], [t_sq])
                    else:
                        ACT(lambda: nc.scalar.activation(out=gtmp[:], in_=gel[:], func=AF.Square),
                            [t_gel], [t_gtmp])
                        POOL(lambda: nc.gpsimd.tensor_tensor(out=sqacc[:], in0=sqacc[:], in1=gtmp[:], op=ALU.add),
                             [t_sq, t_gtmp], [t_sq])
                for tt in range(NT):
                    PE(lambda tt=tt: nc.tensor.matmul(pss[:, tt:tt + 1], lhsT=sqacc[:, tt * 128:(tt + 1) * 128],
                                                      rhs=onesf[:, 0:1], start=True, stop=True),
                       [t_sq, t_onesf], [t_pss])
                ACT(lambda: nc.scalar.activation(out=rstdl[:], in_=pss[:], func=AF.Sqrt, scale=1.0 / 1024,
                                                 bias=RMS_EPS), [t_pss], [t_rstdl])
                DVE(lambda: nc.vector.reciprocal(out=rstdl[:], in_=rstdl[:]), [t_rstdl], [t_rstdl])
                S_.barrier()
        S_.barrier()

        with contextlib.ExitStack() as sbk:
            qT, t_qT = sbt(sbk, "qT", [128, 9, S], BF16)
            Vt, t_V = sbt(sbk, "VtB", [128, NT * 2 * 65], BF16)
            mprev, t_mprev = sbt(sbk, "mprev", [128, 1024], BF16)
            mcur, t_mcur = sbt(sbk, "mcur", [128, 1024], BF16)
            mTa, t_mTa = sbt(sbk, "mTa", [128, 8, S], BF16)
            dma_sp(qT[:], qT_d.rearrange("c p t -> p c t"), [DT("qT")], [t_qT])
            dma_sp(Vt[:], V_d, [DT("V")], [t_V])
            dma_sp(mprev[:], c_mprev, [], [t_mprev])
            dma_sp(mcur[:], c_mcur, [], [t_mcur])
            E = [sbt(sbk, "E%d" % i, [128, 1024], BF16) for i in range(8)]
            at = [sbt(sbk, "at%d" % i, [128, 1024]) for i in range(2)]
            atb = [sbt(sbk, "atb%d" % i, [128, 1024], BF16) for i in range(2)]
            small = [sbt(sbk, "sm%d" % i, [128, 32]) for i in range(2)]
            psc = [pst(sbk, "psc%d" % i, [128, 512]) for i in range(4)]
            po = [pst(sbk, "po%d" % i, [128, 4, 65]) for i in range(2)]
            ptr, t_ptr = pst(sbk, "ptr", [128, 8, 128], BF16)
            cB = {"ie": 0, "isc": 0}
            EsAll = {}

            def stS(qb):
                kbs = ([qb - 1] if qb > 0 else []) + [qb]
                for hk in range(2):
                    Es = []
                    for kb in kbs:
                        E_, t_E = E[cB["ie"] % 8]
                        cB["ie"] += 1
                        for half in range(2):
                            p_, t_p = psc[cB["isc"] % 4]
                            cB["isc"] += 1
                            PE(lambda p_=p_, hk=hk, kb=kb, half=half, qb=qb: nc.tensor.matmul(
                                p_[:], lhsT=qT[hk * 64:(hk + 1) * 64, 8, kb * 128:(kb + 1) * 128],
                                rhs=qT[hk * 64:(hk + 1) * 64, half * 4:(half + 1) * 4, qb * 128:(qb + 1) * 128],
                                start=True, stop=True), [t_qT], [t_p])
                            ACT(lambda p_=p_, E_=E_, half=half: nc.scalar.activation(
                                out=E_[:, half * 512:(half + 1) * 512], in_=p_[:], func=AF.Exp, scale=0.125),
                                [t_p], [t_E])
                        msk, t_msk = (mcur, t_mcur) if kb == qb else (mprev, t_mprev)
                        POOL(lambda E_=E_, msk=msk: nc.gpsimd.tensor_tensor(out=E_[:], in0=E_[:], in1=msk[:],
                                                                            op=ALU.mult), [t_E, t_msk], [t_E])
                        Es.append((E_, t_E, kb))
                    EsAll[(qb, hk)] = Es

            def stP(qb):
                at_, t_at = at[qb % 2]
                atb_, t_atb = atb[qb % 2]
                sm, t_sm = small[qb % 2]
                for hk in range(2):
                    Es = EsAll.pop((qb, hk))
                    for hh in range(2):
                        o_, t_o = po[hh]
                        for g4 in range(4):
                            g = hh * 4 + g4
                            for i_, (E_, t_E, kb) in enumerate(Es):
                                v0 = (kb * 2 + hk) * 65
                                PE(lambda o_=o_, g4=g4, g=g, E_=E_, v0=v0, i_=i_, n=len(Es): nc.tensor.matmul(
                                    o_[:, g4, :], lhsT=E_[:, g * 128:(g + 1) * 128], rhs=Vt[:, v0:v0 + 65],
                                    start=(i_ == 0), stop=(i_ == n - 1)), [t_E, t_V], [t_o])
                        h0 = hk * 8 + hh * 4
                        DVE(lambda o_=o_, sm=sm, h0=h0: nc.vector.tensor_tensor(
                            out=sm[:, h0:h0 + 4], in0=o_[:, :, 64], in1=exps[:, h0:h0 + 4], op=ALU.add),
                            [t_o, t_exps], [t_sm])
                        DVE(lambda sm=sm, h0=h0: nc.vector.reciprocal(out=sm[:, h0:h0 + 4], in_=sm[:, h0:h0 + 4]),
                            [t_sm], [t_sm])
                        for g4 in range(4):
                            h = h0 + g4
                            DVE(lambda o_=o_, g4=g4, h=h, at_=at_, sm=sm: nc.vector.tensor_scalar(
                                out=at_[:, h * 64:(h + 1) * 64], in0=o_[:, g4, 0:64], scalar1=sm[:, h:h + 1],
                                scalar2=None, op0=ALU.mult), [t_o, t_sm], [t_at])
                ACT(lambda atb_=atb_, at_=at_, sm=sm: nc.scalar.activation(
                    out=atb_[:], in_=at_[:], func=AF.Square, accum_out=sm[:, 16:17]), [t_at], [t_atb, t_sm])
                ACT(lambda sm=sm: nc.scalar.activation(out=sm[:, 17:18], in_=sm[:, 16:17], func=AF.Sqrt,
                                                       scale=1.0 / 1024, bias=RMS_EPS), [t_sm], [t_sm])
                DVE(lambda sm=sm: nc.vector.reciprocal(out=sm[:, 17:18], in_=sm[:, 17:18]), [t_sm], [t_sm])
                DVE(lambda atb_=atb_, at_=at_, sm=sm: nc.vector.tensor_scalar(
                    out=atb_[:], in0=at_[:], scalar1=sm[:, 17:18], scalar2=None, op0=ALU.mult),
                    [t_at, t_sm, t_atb], [t_atb])

            def stT(qb):
                atb_, t_atb = atb[qb % 2]
                for c in range(8):
                    PE(lambda c=c, atb_=atb_: nc.tensor.transpose(out=ptr[:, c, :], in_=atb_[:, c * 128:(c + 1) * 128],
                                                                  identity=identb[:]), [t_atb, t_identb], [t_ptr])
                for c in range(8):
                    if c % 2 == 0:
                        ACT(lambda c=c, qb=qb: nc.scalar.activation(
                            out=mTa[:, c, qb * 128:(qb + 1) * 128], in_=ptr[:, c, :], func=AF.Identity,
                            scale=colsA[:, CA_GA + c:CA_GA + c + 1]), [t_ptr, t_colsA], [t_mTa])
                    else:
                        DVE(lambda c=c, qb=qb: nc.vector.tensor_scalar(
                            out=mTa[:, c, qb * 128:(qb + 1) * 128], in0=ptr[:, c, :],
                            scalar1=colsA[:, CA_GA + c:CA_GA + c + 1], scalar2=None, op0=ALU.mult),
                            [t_ptr, t_colsA], [t_mTa])

            stS(0)
            for qb in range(NT):
                if qb + 1 < NT:
                    stS(qb + 1)
                stP(qb)
                if qb > 0:
                    stT(qb - 1)
            stT(NT - 1)
            dma_sp(mT_d[0:8].rearrange("c p t -> p c t"), mTa[:], [t_mTa], [DT("mT")])
            S_.barrier()

        with contextlib.ExitStack() as sc:
            wo, t_wo = sbt(sc, "wo", [128, KC, D], BF16)
            wsrc = w_out[l].rearrange("(kc kp) n -> kp kc n", kp=128)
            for nb in range(4):
                dma_pool(wo[:, :, nb * 512:(nb + 1) * 512], wsrc[:, :, nb * 512:(nb + 1) * 512], [], [t_wo])
            wr32, t_wr = sbt(sc, "wr32", [128, KC, NE])
            dma_sp(wr32[:], w_r[l].rearrange("(kc kp) e -> kp kc e", kp=128), [], [t_wr])
            rows, t_rows = sbt(sc, "rowsC", [128, 3, D])
            dma_sp(rows[:], rowsBig_d[l][:, 0:3, :], [], [t_rows])
            mTr = [sbt(sc, "mTr%d" % i, [128, KC, 512], BF16) for i in range(2)]
            xin = [sbt(sc, "xinC%d" % i, [128, D]) for i in range(2)]
            res = [sbt(sc, "resC%d" % i, [128, D]) for i in range(2)]
            x1b = [sbt(sc, "x1b%d" % i, [128, D], BF16) for i in range(2)]
            x1T, t_x1T = sbt(sc, "x1T", [128, KC, 128])
            lg, t_lg = sbt(sc, "lg", [128, NE])
            maskb, t_maskb = sbt(sc, "maskb", [128, NT, NE], BF16)
            idxf, t_idxf = sbt(sc, "idxf", [128, NT, 4])
            st_small = {"stats": sbt(sc, "stats", [128, 4, 6]), "mv": sbt(sc, "mv", [128, 4])}
            top8, t_top8 = sbt(sc, "top8", [128, 8])
            idx8, t_idx8 = sbt(sc, "idx8", [128, 8], U32)
            sm, t_sm = sbt(sc, "smC", [128, 16])
            G, t_G = sbt(sc, "G", [128, NE])
            oh, t_oh = sbt(sc, "oh", [128, NE])
            posC, t_posC = sbt(sc, "posC", [128, NE])
            slotf, t_slotf = sbt(sc, "slotf", [128, NT, 4])
            pya = [pst(sc, "pya%d" % i, [128, 512]) for i in range(2)]
            pyl = [pst(sc, "pyl%d" % i, [128, 512]) for i in range(2)]
            ptp = [pst(sc, "ptpC%d" % i, [128, 4, 128]) for i in range(2)]
            plg_all, t_plg = pst(sc, "plg", [128, 96])
            plg = plg_all[:, 0:32]
            ppos = plg_all[:, 32:96]
            t_ppos = t_plg
            runc, t_runc = sbt(sc, "runc", [128, NE])
            DVE(lambda: nc.vector.memset(runc[:], 0.0), [], [t_runc])
            pgt, t_pgt = pst(sc, "pgt", [32, 128])
            cc = {"iy": 0, "itp": 0, "mt": None}

            def ldC(tt):
                xi, t_xi = xin[tt % 2]
                dma_sp(xi[:], xsrc[tt * 128:(tt + 1) * 128, :], [t_xsrc], [t_xi])
                if tt % 4 == 0:
                    mt_, t_mt_ = mTr[(tt // 4) % 2]
                    dma_sp(mt_[:], mT_d.rearrange("c p t -> p c t")[:, :, tt * 128:tt * 128 + 512],
                           [DT("mT")], [t_mt_])

            def stage1(tt):
                if tt % 4 == 0:
                    cc["mt"] = mTr[(tt // 4) % 2]
                mt, t_mt = cc["mt"]
                xi, t_xi = xin[tt % 2]
                rs, t_rs = res[tt % 2]
                xb_, t_xb = x1b[tt % 2]
                DVE(lambda xi=xi: nc.vector.scalar_tensor_tensor(out=xi[:], in0=xi[:], scalar=ALPHA, in1=rows[:, 0, :],
                                                                 op0=ALU.mult, op1=ALU.add), [t_xi, t_rows], [t_xi])
                tl = (tt % 4) * 128
                for nb in range(4):
                    a_, t_a = pya[cc["iy"] % 2]
                    l_, t_l = pyl[cc["iy"] % 2]
                    cc["iy"] += 1
                    for kc in range(8):
                        PE(lambda kc=kc, a_=a_, mt=mt, tl=tl, nb=nb: nc.tensor.matmul(
                            a_[:], lhsT=mt[:, kc, tl:tl + 128], rhs=wo[:, kc, nb * 512:(nb + 1) * 512],
                            start=(kc == 0), stop=(kc == 7)), [t_mt, t_wo], [t_a])
                    for kc in range(8, 16):
                        PE(lambda kc=kc, l_=l_, mt=mt, tl=tl, nb=nb: nc.tensor.matmul(
                            l_[:], lhsT=mt[:, kc, tl:tl + 128], rhs=wo[:, kc, nb * 512:(nb + 1) * 512],
                            start=(kc == 8), stop=(kc == 15)), [t_mt, t_wo], [t_l])
                    sl = slice(nb * 512, (nb + 1) * 512)
                    DVE(lambda a_=a_, rs=rs, xi=xi, sl=sl: nc.vector.tensor_tensor(
                        out=rs[:, sl], in0=a_[:], in1=xi[:, sl], op=ALU.add), [t_a, t_xi], [t_rs])
                    DVE(lambda l_=l_, rs=rs, sl=sl, tt=tt: nc.vector.scalar_tensor_tensor(
                        out=rs[:, sl], in0=l_[:], scalar=rstdl[:, tt:tt + 1], in1=rs[:, sl],
                        op0=ALU.mult, op1=ALU.add), [t_l, t_rstdl, t_rs], [t_rs])

            def stage2(tt):
                rs, t_rs = res[tt % 2]
                xb_, t_xb = x1b[tt % 2]
                ln_tile(st_small, rs, t_rs, rows[:, 1, :], rows[:, 2, :], t_rows, "c")
                dma_sp(x1_d[tt * 128:(tt + 1) * 128, :], rs[:], [t_rs], [DT("x1")])
                ACT(lambda xb_=xb_, rs=rs: nc.scalar.copy(out=xb_[:], in_=rs[:]), [t_rs], [t_xb])
                dma_sp(x1b_d[tt * 128:(tt + 1) * 128, :], xb_[:], [t_xb], [DT("x1b")])

            def stage2b(tt):
                rs, t_rs = res[tt % 2]
                for g in range(4):
                    p_, t_p = ptp[cc["itp"] % 2]
                    cc["itp"] += 1
                    for j in range(4):
                        kc = g * 4 + j
                        PE(lambda kc=kc, j=j, p_=p_, rs=rs: nc.tensor.transpose(
                            out=p_[:, j, :], in_=rs[:, kc * 128:(kc + 1) * 128], identity=identf[:]),
                           [t_rs, t_identf], [t_p])
                    if g % 2 == 0:
                        ACT(lambda p_=p_, g=g: nc.scalar.copy(out=x1T[:, g * 4:(g + 1) * 4, :], in_=p_[:]),
                            [t_p], [t_x1T])
                    else:
                        DVE(lambda p_=p_, g=g: nc.vector.tensor_copy(out=x1T[:, g * 4:(g + 1) * 4, :], in_=p_[:]),
                            [t_p], [t_x1T])
                for kc in range(KC):
                    PE(lambda kc=kc: nc.tensor.matmul(plg, lhsT=x1T[:, kc, :], rhs=wr32[:, kc, :],
                                                      start=(kc == 0), stop=(kc == KC - 1)),
                       [t_x1T, t_wr], [t_plg])
                DVE(lambda: nc.vector.tensor_tensor(out=lg[:], in0=plg, in1=rowsB[:, RB_BR:RB_BR + NE],
                                                    op=ALU.add), [t_plg, t_rowsB], [t_lg])
                DVE(lambda: nc.vector.max(out=top8[:], in_=lg[:]), [t_lg], [t_top8])
                DVE(lambda: nc.vector.max_index(out=idx8[:], in_max=top8[:], in_values=lg[:]),
                    [t_top8, t_lg], [t_idx8])
                DVE(lambda: nc.vector.tensor_scalar(out=sm[:, 0:1], in0=top8[:, 0:1], scalar1=-1.0, scalar2=None,
                                                    op0=ALU.mult), [t_top8], [t_sm])
                ACT(lambda: nc.scalar.activation(out=sm[:, 4:8], in_=top8[:, 0:4], func=AF.Exp, bias=sm[:, 0:1],
                                                 accum_out=sm[:, 1:2]), [t_top8, t_sm], [t_sm])
                DVE(lambda: nc.vector.reciprocal(out=sm[:, 2:3], in_=sm[:, 1:2]), [t_sm], [t_sm])
                DVE(lambda tt=tt: nc.vector.tensor_scalar(out=gates[:, tt, :], in0=sm[:, 4:8], scalar1=sm[:, 2:3],
                                                          scalar2=None, op0=ALU.mult), [t_sm], [t_gates])
                DVE(lambda tt=tt: nc.vector.tensor_scalar(out=maskb[:, tt, :], in0=lg[:], scalar1=top8[:, 3:4],
                                                          scalar2=None, op0=ALU.is_ge), [t_lg, t_top8], [t_maskb])
                DVE(lambda tt=tt: nc.vector.tensor_copy(out=idxf[:, tt, :], in_=idx8[:, 0:4]), [t_idx8], [t_idxf])
                for k_ in range(4):
                    dst, t_dst = (G, t_G) if k_ == 0 else (oh, t_oh)
                    DVE(lambda k_=k_, tt=tt, dst=dst: nc.vector.tensor_scalar(
                        out=dst[:], in0=iota[:, 0:32], scalar1=idxf[:, tt, k_:k_ + 1],
                        scalar2=gates[:, tt, k_:k_ + 1], op0=ALU.is_equal, op1=ALU.mult),
                        [t_iota, t_idxf, t_gates], [t_dst])
                    if k_ > 0:
                        DVE(lambda: nc.vector.tensor_tensor(out=G[:], in0=G[:], in1=oh[:], op=ALU.add),
                            [t_G, t_oh], [t_G])
                PE(lambda: nc.tensor.transpose(out=pgt[:], in_=G[:], identity=identf[:]), [t_G, t_identf], [t_pgt])
                ACT(lambda tt=tt: nc.scalar.copy(out=GT[:, tt, :], in_=pgt[:]), [t_pgt], [t_GT])
                PE(lambda tt=tt: nc.tensor.matmul(ppos[:, 0:32], lhsT=triu[:], rhs=maskb[:, tt, :], start=True,
                                                  stop=True), [t_triu, t_maskb], [t_ppos])
                PE(lambda tt=tt: nc.tensor.matmul(ppos[:, 32:64], lhsT=onesb[:], rhs=maskb[:, tt, :], start=True,
                                                  stop=True), [t_onesb, t_maskb], [t_ppos])
                DVE(lambda: nc.vector.tensor_tensor(out=posC[:], in0=ppos[:, 0:32], in1=runc[:], op=ALU.add),
                    [t_ppos, t_runc], [t_posC])
                DVE(lambda: nc.vector.scalar_tensor_tensor(out=posC[:], in0=posC[:], scalar=float(CAP - 1),
                                                           in1=iota[:, 32:64], op0=ALU.min, op1=ALU.add),
                    [t_posC, t_iota], [t_posC])
                DVE(lambda: nc.vector.tensor_tensor(out=runc[:], in0=runc[:], in1=ppos[:, 32:64], op=ALU.add),
                    [t_ppos, t_runc], [t_runc])
                for k_ in range(4):
                    DVE(lambda k_=k_, tt=tt: nc.vector.scalar_tensor_tensor(
                        out=oh[:], in0=iota[:, 0:32], scalar=idxf[:, tt, k_:k_ + 1], in1=posC[:],
                        op0=ALU.is_equal, op1=ALU.mult), [t_iota, t_idxf, t_posC], [t_oh])
                    DVE(lambda k_=k_, tt=tt: nc.vector.reduce_sum(out=slotf[:, tt, k_:k_ + 1], in_=oh[:], axis=AX.X),
                        [t_oh], [t_slotf])
                DVE(lambda tt=tt: nc.vector.tensor_copy(out=sloti[:, tt, :], in_=slotf[:, tt, :]),
                    [t_slotf], [t_slotis[tt]])
                for k_ in range(4):
                    S_.dma("pool", lambda tt=tt, k_=k_: nc.gpsimd.indirect_dma_start(
                        out=stok_d[:, :], out_offset=bass.IndirectOffsetOnAxis(ap=sloti[:, tt, k_:k_ + 1], axis=0),
                        in_=tokid[:, tt:tt + 1], in_offset=None), [t_slotis[tt], t_tokid], [DT("stok")])

            ldC(0)
            stage1(0)
            for tt in range(NT):
                if tt + 1 < NT:
                    ldC(tt + 1)
                stage2(tt)
                if tt + 1 < NT:
                    stage1(tt + 1)
                stage2b(tt)
            S_.barrier()

        with contextlib.ExitStack() as sd:
            bupT, t_bup = sbt(sd, "bupT", [128, NE * 16])
            dma_sp(bupT[:], bupT_d[l], [], [t_bup])
            sidx_all, _ = sbt(sd, "sidx", [128, NE, 3], I32)
            t_sidx = [Tk() for _ in range(NE)]
            for e in range(NE):
                dma_sp(sidx_all[:, e, :], stok_d[e * CAP:(e + 1) * CAP, :].rearrange("(p j) o -> p (j o)", p=128),
                       [DT("stok")], [t_sidx[e]])
            xs = [[sbt(sd, "xs%d_%d" % (i, j), [128, D], BF16) for j in range(3)] for i in range(2)]
            xsT = [sbt(sd, "xsT%d" % i, [128, KC, CAP], BF16) for i in range(2)]
            NWU = 4
            NWD = 4
            wu = [sbt(sd, "wu%d" % i, [128, KC, 512], BF16) for i in range(NWU)]
            wd = [sbt(sd, "wd%d" % i, [128, 8, 512], BF16) for i in range(NWD)]
            gs = [sbt(sd, "gs%d" % i, [128, 4, CAP]) for i in range(2)]
            sg = [sbt(sd, "sg%d" % i, [128, CAP]) for i in range(2)]
            ln_ = [sbt(sd, "ln%d" % i, [128, CAP]) for i in range(2)]
            hT = [sbt(sd, "hT%d" % i, [128, 8, CAP], BF16) for i in range(1)]
            yo = [sbt(sd, "yo%d" % i, [128, 512]) for i in range(4)]
            ptr_ = [pst(sd, "ptrD%d" % i, [128, 8, 128], BF16) for i in range(2)]
            pu = [pst(sd, "pu%d" % i, [128, CAP]) for i in range(3)]
            pd = [pst(sd, "pd%d" % i, [128, 512]) for i in range(3)]
            cnt = {"wu": 0, "wd": 0, "tr": 0, "pu": 0, "pd": 0, "gs": 0, "sg": 0, "yo": 0}

            def gather(e):
                t_si = t_sidx[e]
                for j in range(3):
                    xs_, t_xs = xs[e % 2][j]
                    S_.dma("pool", lambda xs_=xs_, e=e, j=j: nc.gpsimd.indirect_dma_start(
                        out=xs_[:, :], out_offset=None, in_=x1b_d[:, :],
                        in_offset=bass.IndirectOffsetOnAxis(ap=sidx_all[:, e, j:j + 1], axis=0)),
                        [t_si, DT("x1b")], [t_xs])

            def transp(e):
                xsT_, t_xsT = xsT[e % 2]
                for j in range(3):
                    xs_, t_xs = xs[e % 2][j]
                    for g in range(2):
                        p_, t_p = ptr_[cnt["tr"] % 2]
                        cnt["tr"] += 1
                        for c in range(8):
                            kc = g * 8 + c
                            PE(lambda p_=p_, c=c, kc=kc, xs_=xs_: nc.tensor.transpose(
                                out=p_[:, c, :], in_=xs_[:, kc * 128:(kc + 1) * 128], identity=identb[:]),
                               [t_xs, t_identb], [t_p])
                        dst = xsT_[:, g * 8:(g + 1) * 8, j * 128:(j + 1) * 128]
                        if cnt["tr"] % 2 == 0:
                            ACT(lambda p_=p_, dst=dst: nc.scalar.copy(out=dst, in_=p_[:]), [t_p], [t_xsT])
                        else:
                            DVE(lambda p_=p_, dst=dst: nc.vector.tensor_copy(out=dst, in_=p_[:]), [t_p], [t_xsT])

            gather(0)
            transp(0)
            for e in range(NE):
                xsT_, t_xsT = xsT[e % 2]
                hT_, t_hT = hT[0]
                if e + 1 < NE:
                    gather(e + 1)
                usrc = w_up[l, e].rearrange("(kc kp) n -> kp kc n", kp=128)
                for hf in range(2):
                    gs_, t_gs = gs[cnt["gs"] % 2]
                    cnt["gs"] += 1
                    for part in range(2):
                        w_, t_w = wu[cnt["wu"] % NWU]
                        cnt["wu"] += 1
                        c0 = part * 1024 + hf * 512
                        dma_pool(w_[:], usrc[:, :, c0:c0 + 512], [], [t_w])
                        for j4 in range(4):
                            fc = hf * 4 + j4
                            p_, t_p = pu[cnt["pu"] % 3]
                            cnt["pu"] += 1
                            for kc in range(KC):
                                PE(lambda p_=p_, w_=w_, j4=j4, kc=kc, xsT_=xsT_: nc.tensor.matmul(
                                    p_[:], lhsT=w_[:, kc, j4 * 128:(j4 + 1) * 128], rhs=xsT_[:, kc, :],
                                    start=(kc == 0), stop=(kc == KC - 1)), [t_w, t_xsT], [t_p])
                            bcol = bupT[:, e * 16 + part * 8 + fc:e * 16 + part * 8 + fc + 1]
                            if part == 0:
                                sg_, t_sg = sg[cnt["sg"] % 2]
                                cnt["sg"] += 1
                                DVE(lambda p_=p_, gs_=gs_, j4=j4, bcol=bcol: nc.vector.tensor_scalar(
                                    out=gs_[:, j4, :], in0=p_[:], scalar1=bcol, scalar2=7.0, op0=ALU.add,
                                    op1=ALU.min), [t_p, t_bup], [t_gs])
                                ACT(lambda sg_=sg_, gs_=gs_, j4=j4: nc.scalar.activation(
                                    out=sg_[:], in_=gs_[:, j4, :], func=AF.Sigmoid, scale=1.702), [t_gs], [t_sg])
                                DVE(lambda sg_=sg_, gs_=gs_, j4=j4: nc.vector.tensor_tensor(
                                    out=gs_[:, j4, :], in0=gs_[:, j4, :], in1=sg_[:], op=ALU.mult),
                                    [t_gs, t_sg], [t_gs])
                            else:
                                l_, t_l = ln_[cnt["sg"] % 2]
                                cnt["sg"] += 1
                                DVE(lambda p_=p_, l_=l_, bcol=bcol: nc.vector.tensor_scalar(
                                    out=l_[:], in0=p_[:], scalar1=bcol, scalar2=7.0, op0=ALU.add, op1=ALU.min),
                                    [t_p, t_bup], [t_l])
                                DVE(lambda l_=l_: nc.vector.tensor_scalar(
                                    out=l_[:], in0=l_[:], scalar1=-7.0, scalar2=1.0, op0=ALU.max, op1=ALU.add),
                                    [t_l], [t_l])
                                DVE(lambda l_=l_, gs_=gs_, j4=j4, fc=fc, hT_=hT_: nc.vector.tensor_tensor(
                                    out=hT_[:, fc, :], in0=l_[:], in1=gs_[:, j4, :], op=ALU.mult),
                                    [t_l, t_gs], [t_hT])
                if e + 1 < NE:
                    transp(e + 1)
                dsrc = w_dn[l, e].rearrange("(fc fp) n -> fp fc n", fp=128)
                for nb in range(4):
                    w_, t_w = wd[cnt["wd"] % NWD]
                    cnt["wd"] += 1
                    dma_pool(w_[:], dsrc[:, :, nb * 512:(nb + 1) * 512], [], [t_w])
                    for j in range(3):
                        p_, t_p = pd[cnt["pd"] % 3]
                        cnt["pd"] += 1
                        for fc in range(8):
                            PE(lambda p_=p_, fc=fc, j=j, w_=w_, hT_=hT_: nc.tensor.matmul(
                                p_[:], lhsT=hT_[:, fc, j * 128:(j + 1) * 128], rhs=w_[:, fc, :],
                                start=(fc == 0), stop=(fc == 7)), [t_hT, t_w], [t_p])
                        yt, t_yt = yo[cnt["yo"] % 4]
                        cnt["yo"] += 1
                        if cnt["yo"] % 2 == 0:
                            ACT(lambda p_=p_, yt=yt: nc.scalar.copy(out=yt[:], in_=p_[:]), [t_p], [t_yt])
                        else:
                            DVE(lambda p_=p_, yt=yt: nc.vector.tensor_copy(out=yt[:], in_=p_[:]), [t_p], [t_yt])
                        dma_sp(ys_d[e * CAP:(e + 1) * CAP, :].rearrange("(p j) n -> p j n", j=3)[:, j, nb * 512:(nb + 1) * 512], yt[:],
                               [t_yt], [DT("ys")])
            S_.barrier()

        with contextlib.ExitStack() as se:
            rows, t_rows = sbt(se, "rowsE", [128, 2, D])
            dma_sp(rows[:], rowsBig_d[l][:, 3:5, :], [], [t_rows])
            bd, t_bd = sbt(se, "bd", [32, D])
            dma_sp(bd[:], b_dn[l], [], [t_bd])
            yg = [[sbt(se, "yg%d_%d" % (i, k_), [128, D]) for k_ in range(4)] for i in range(2)]
            xi2 = [sbt(se, "xi2%d" % i, [128, D]) for i in range(2)]
            st_small = {"stats": sbt(se, "statsE", [128, 4, 6]), "mv": sbt(se, "mvE", [128, 4])}
            pb = [pst(se, "pbE%d" % i, [128, 512]) for i in range(4)]
            pacc = [pst(se, "paccE%d" % i, [128, 512]) for i in range(4)]
            dg = [[sbt(se, "dg%d_%d" % (i, k_), [128, 128]) for k_ in range(4)] for i in range(2)]
            ipb = 0
            dst_d = out_d if last else xa_d
            t_dst = DT("out") if last else DT("xa")
            def fetchE(tt):
                xi, t_xi = xi2[tt % 2]
                dma_sp(xi[:], x1_d[tt * 128:(tt + 1) * 128, :], [DT("x1")], [t_xi])
                for k_ in range(4):
                    y_, t_y = yg[tt % 2][k_]
                    S_.dma("pool", lambda y_=y_, tt=tt, k_=k_: nc.gpsimd.indirect_dma_start(
                        out=y_[:, :], out_offset=None, in_=ys_d[:, :],
                        in_offset=bass.IndirectOffsetOnAxis(ap=sloti[:, tt, k_:k_ + 1], axis=0)),
                        [t_slotis[tt], DT("ys")], [t_y])

            fetchE(0)
            for tt in range(NT):
                xi, t_xi = xi2[tt % 2]
                if tt + 1 < NT:
                    fetchE(tt + 1)
                for nb in range(4):
                    p_, t_p = pb[ipb % 4]
                    ipb += 1
                    PE(lambda p_=p_, tt=tt, nb=nb: nc.tensor.matmul(p_[:], lhsT=GT[:, tt, :],
                                                                    rhs=bd[:, nb * 512:(nb + 1) * 512],
                                                                    start=True, stop=True), [t_GT, t_bd], [t_p])
                    sl = slice(nb * 512, (nb + 1) * 512)
                    DVE(lambda p_=p_, xi=xi, sl=sl: nc.vector.scalar_tensor_tensor(
                        out=xi[:, sl], in0=xi[:, sl], scalar=ALPHA, in1=p_[:], op0=ALU.mult, op1=ALU.add),
                        [t_xi, t_p], [t_xi])
                dgs = dg[tt % 2]
                for k_ in range(4):
                    d_, t_d = dgs[k_]
                    ACT(lambda d_=d_, tt=tt, k_=k_: nc.scalar.activation(
                        out=d_[:], in_=identf[:], func=AF.Identity, scale=gates[:, tt, k_:k_ + 1]),
                        [t_identf, t_gates], [t_d])
                for nb in range(4):
                    p_, t_p = pacc[nb]
                    sl = slice(nb * 512, (nb + 1) * 512)
                    for k_ in range(4):
                        y_, t_y = yg[tt % 2][k_]
                        d_, t_d = dgs[k_]
                        PE(lambda p_=p_, d_=d_, y_=y_, sl=sl, k_=k_: nc.tensor.matmul(
                            p_[:], lhsT=d_[:], rhs=y_[:, sl], start=(k_ == 0), stop=(k_ == 3)),
                           [t_d, t_y], [t_p])
                    DVE(lambda p_=p_, xi=xi, sl=sl: nc.vector.tensor_tensor(
                        out=xi[:, sl], in0=xi[:, sl], in1=p_[:], op=ALU.add), [t_xi, t_p], [t_xi])
                ln_tile(st_small, xi, t_xi, rows[:, 0, :], rows[:, 1, :], t_rows, "e")
                dma_sp(dst_d[tt * 128:(tt + 1) * 128, :], xi[:], [t_xi], [t_dst])
            S_.barrier()
    S_.barrier()
    es.close()
    return nc


def _consts():
    import ml_dtypes
    bf = ml_dtypes.bfloat16
    p = np.arange(128)
    d = p % 64
    inv_freq = (1.0 / (np.float32(10000.0) ** (np.arange(0, 64, 2, dtype=np.float32) / np.float32(64)))).astype(np.float32)
    ang = (np.arange(S, dtype=np.float32)[:, None] * inv_freq[None, :]).astype(np.float32)
    cos = np.cos(ang).astype(np.float32)
    sin = np.sin(ang).astype(np.float32)
    cosT = np.ascontiguousarray(cos[:, d % 32].T)
    sgn = np.where(d < 32, -1.0, 1.0).astype(np.float32)
    sinT = np.ascontiguousarray(sin[:, d % 32].T * sgn[:, None]).astype(np.float32)
    partner = 64 * (p // 64) + ((p % 64) + 32) % 64
    permR = np.zeros((128, 128), np.float32)
    permR[partner, p] = 1.0
    kk = np.arange(128)[:, None]
    qq = np.arange(128)[None, :]
    mprev = np.tile((kk > qq).astype(np.float32), (1, 8)).astype(bf)
    mcur = np.tile((kk <= qq).astype(np.float32), (1, 8)).astype(bf)
    triu = (kk < qq).astype(np.float32).astype(bf)
    iota = np.zeros((128, 64), np.float32)
    iota[:, 0:32] = np.arange(32)[None, :]
    iota[:, 32:64] = np.arange(32)[None, :] * CAP
    tokid = (np.arange(NT)[None, :] * 128 + p[:, None]).astype(np.int32)
    return {
        "c_identf": np.eye(128, dtype=np.float32), "c_identb": np.eye(128, dtype=np.float32).astype(bf),
        "c_cos": cosT, "c_sin": sinT, "c_permR": permR, "c_mprev": mprev, "c_mcur": mcur, "c_triu": triu,
        "c_onesb": np.ones((128, 128), np.float32).astype(bf), "c_iota": iota, "c_tokid": tokid,
        "c_zero": np.zeros((128, 96), np.int32),
    }


def _prep(inp, layers):
    ls = list(layers)
    f = lambda k: np.asarray(inp[k])
    pq = np.arange(1024)
    cq, pp = pq // 128, pq % 128
    perm_q = (np.where(pp < 64, cq, 8 + cq) * 64 + (pp % 64))
    perm = np.concatenate([perm_q, np.arange(1024, DIN)])
    w_in = np.ascontiguousarray(f("w_in")[ls][:, :, perm])
    b_in = f("b_in")[ls][:, perm]
    Ln = len(ls)
    colsA = np.zeros((Ln, 128, NA), np.float32)
    col = lambda v: v.reshape(Ln, -1, 128).transpose(0, 2, 1)
    colsA[:, :, CA_BQ:CA_BQ + 8] = col(b_in[:, 0:1024])
    colsA[:, :, CA_BK:CA_BK + 1] = col(b_in[:, 1024:1152])
    colsA[:, :, CA_BUX:CA_BUX + 8] = col(b_in[:, 1280:2304])
    colsA[:, :, CA_BUG:CA_BUG + 8] = col(b_in[:, 2304:3328])
    cw = f("conv_w")[ls]
    for k in range(4):
        colsA[:, :, CA_CW + k * 8:CA_CW + (k + 1) * 8] = col(cw[:, k, :])
    colsA[:, :, CA_CB:CA_CB + 8] = col(f("conv_b")[ls])
    colsA[:, :, CA_BA:CA_BA + 8] = col(f("lru_b_a")[ls])
    colsA[:, :, CA_BXG:CA_BXG + 8] = col(f("lru_b_x")[ls])
    colsA[:, :, CA_LAM:CA_LAM + 8] = col(f("lru_lambda")[ls])
    colsA[:, :, CA_GA:CA_GA + 8] = col(f("g_attn")[ls])
    colsA[:, :, CA_GL:CA_GL + 8] = col(f("g_lru")[ls])
    rowsB = np.zeros((Ln, 128, NB), np.float32)
    rowsB[:, :, RB_BV:RB_BV + 128] = b_in[:, None, 1152:1280]
    rowsB[:, :, RB_BR:RB_BR + 32] = f("b_router")[ls][:, None, :]
    rowsB[:, :, RB_SK:RB_SK + 16] = f("attn_sinks")[ls][:, None, :]
    rowsBig = np.zeros((Ln, 128, 5, D), np.float32)
    for i, k in enumerate(["b_out", "ln1_g", "ln1_b", "ln2_g", "ln2_b"]):
        rowsBig[:, :, i, :] = f(k)[ls][:, None, :]

    def bdiag(w):
        w = w[ls]
        o = np.zeros((Ln, 128, 8, 128), np.float32)
        for c in range(8):
            o[:, 0:64, c, 0:64] = w[:, 2 * c]
            o[:, 64:128, c, 64:128] = w[:, 2 * c + 1]
        return o
    bup = f("b_up")[ls]
    bupT = np.ascontiguousarray(bup.reshape(Ln, NE, 16, 128).transpose(0, 3, 1, 2).reshape(Ln, 128, NE * 16))
    m = {
        "w_in": w_in, "colsA": colsA, "rowsB": rowsB, "rowsBig": rowsBig,
        "wab": bdiag(f("lru_w_a")), "wxb": bdiag(f("lru_w_x")),
        "w_out": np.ascontiguousarray(f("w_out")[ls]), "w_router": np.ascontiguousarray(f("w_router")[ls]),
        "w_up": np.ascontiguousarray(f("w_up")[ls]), "bupT": bupT,
        "w_down": np.ascontiguousarray(f("w_down")[ls]), "b_down": np.ascontiguousarray(f("b_down")[ls]),
    }
    m.update(_consts())
    return m


_NC_CACHE = {}


def _get_nc(nl):
    if nl not in _NC_CACHE:
        _NC_CACHE[nl] = build(nl)
    return _NC_CACHE[nl]


FUSED = True


def kernel(**inputs):
    x = np.asarray(inputs["x"], dtype=np.float32)
    B = x.shape[0]
    if FUSED:
        nc = _get_nc(DEPTH)
        shared = _prep(inputs, range(DEPTH))
        in_maps = [dict(shared, x=np.ascontiguousarray(x[b])) for b in range(B)]
        res = run_bass_kernel_spmd(nc, in_maps, core_ids=list(range(B)))
        return np.stack([np.asarray(r["out"]) for r in res.results], axis=0).astype(np.float32)
    cur = [np.ascontiguousarray(x[b]) for b in range(B)]
    for l in range(DEPTH):
        nc = _get_nc(1)
        shared = _prep(inputs, [l])
        in_maps = [dict(shared, x=cur[b]) for b in range(B)]
        res = run_bass_kernel_spmd(nc, in_maps, core_ids=list(range(B)))
        cur = [np.ascontiguousarray(np.asarray(r["out"], dtype=np.float32)) for r in res.results]
    return np.stack(cur, axis=0).astype(np.float32)
```
